# Optimizing a Trainium2 kernel written in Bass

```python
import jax, jax.numpy as jnp
from jax import lax
import numpy as np

D_MODEL = 1024
BATCH = 4
SEQ = 8192
DEPTH = 4

HEAD_DIM = 64
HEADS_MLA = 4
HEADS_DIL = 8
HEADS_DSA = 4
D_MIX = (HEADS_MLA + HEADS_DIL + HEADS_DSA) * HEAD_DIM
Q_LORA = 256
KV_LORA = 128
MLA_NOPE = 64
MLA_ROPE = 32
MLA_V = HEAD_DIM
DIL_PAIRS = ((128, 1), (512, 4), (2048, 16))
IDX_HEADS = 8
IDX_DIM = 32
DSA_TOPK = 256
D_FF = 2816
CONV_WIDTH = 3
ROPE_THETA = 500000.0
ROT_FRAC = 4
BLOCK = 128
NORM_EPS = 1e-6
DIL_PAD = 16 * BLOCK
IN_SPLITS = (Q_LORA, KV_LORA, MLA_ROPE,
             3 * HEADS_DIL * HEAD_DIM,
             3 * HEADS_DSA * HEAD_DIM,
             IDX_HEADS * IDX_DIM, IDX_DIM, IDX_HEADS)
N_IN = sum(IN_SPLITS)

kernel_name = "hybrid_mla_dilated_dsa_convffn"


def rms_norm(x, g):
    xf = x.astype(jnp.float32)
    y = xf * lax.rsqrt(jnp.mean(xf * xf, axis=-1, keepdims=True) + NORM_EPS)
    return (y * g.astype(jnp.float32)).astype(x.dtype)


def rope_tables(seq, dim):
    inv = jnp.power(jnp.float32(ROPE_THETA), -jnp.arange(0, dim, 2, dtype=jnp.float32) / dim)
    ang = jnp.arange(seq, dtype=jnp.float32)[:, None] * inv[None, :]
    return jnp.cos(ang), jnp.sin(ang)


def apply_rope(x, cos, sin):
    h = x.shape[-1] // 2
    xf = x.astype(jnp.float32)
    x1, x2 = xf[..., :h], xf[..., h:]
    c, s = cos[None, :, None, :], sin[None, :, None, :]
    return jnp.concatenate([x1 * c - x2 * s, x2 * c + x1 * s], axis=-1).astype(x.dtype)


def partial_rope(x, cos, sin):
    r = 2 * cos.shape[-1]
    return jnp.concatenate([apply_rope(x[..., :r], cos, sin), x[..., r:]], axis=-1)


def mla_attention(c_q, c_kv, k_pe_raw, g_q, w_uq, g_kv, w_ukv, cos_a, sin_a):
    B, S, _ = c_q.shape
    q = (rms_norm(c_q, g_q) @ w_uq).reshape(B, S, HEADS_MLA, MLA_NOPE + MLA_ROPE)
    q_nope = q[..., :MLA_NOPE]
    q_pe = apply_rope(q[..., MLA_NOPE:], cos_a, sin_a)
    kv = (rms_norm(c_kv, g_kv) @ w_ukv).reshape(B, S, HEADS_MLA, MLA_NOPE + MLA_V)
    k_nope, v = kv[..., :MLA_NOPE], kv[..., MLA_NOPE:]
    k_pe = apply_rope(k_pe_raw[:, :, None, :], cos_a, sin_a)[:, :, 0]
    scale = (MLA_NOPE + MLA_ROPE) ** -0.5
    nb = S // BLOCK
    qn_b = q_nope.reshape(B, nb, BLOCK, HEADS_MLA, MLA_NOPE).transpose(1, 0, 2, 3, 4)
    qp_b = q_pe.reshape(B, nb, BLOCK, HEADS_MLA, MLA_ROPE).transpose(1, 0, 2, 3, 4)
    kpos = jnp.arange(S)

    def block(args):
        qn, qp, i = args
        s = (jnp.einsum('bqhe,bshe->bhqs', qn, k_nope).astype(jnp.float32)
             + jnp.einsum('bqhr,bsr->bhqs', qp, k_pe).astype(jnp.float32)) * scale
        qpos = i * BLOCK + jnp.arange(BLOCK)
        s = jnp.where((kpos[None, :] <= qpos[:, None])[None, None], s, -jnp.inf)
        p = jax.nn.softmax(s, axis=-1).astype(v.dtype)
        return jnp.einsum('bhqs,bshe->bqhe', p, v)

    o = lax.map(block, (qn_b, qp_b, jnp.arange(nb)))
    return o.transpose(1, 0, 2, 3, 4).reshape(B, S, HEADS_MLA, MLA_V)


def dilated_window(q, k, v, dilation, n_win):
    B, Sp, H, Dh = q.shape
    n = Sp // dilation
    nb = n // BLOCK

    def stride_split(t):
        return t.reshape(B, n, dilation, H, Dh).transpose(0, 2, 1, 3, 4)

    qs, ks, vs = stride_split(q), stride_split(k), stride_split(v)
    qb = qs.reshape(B, dilation, nb, BLOCK, H, Dh)

    def band(t):
        tp = jnp.pad(t, ((0, 0), (0, 0), (BLOCK, 0), (0, 0), (0, 0)))
        prev = tp[:, :, :n].reshape(B, dilation, nb, BLOCK, H, Dh)
        return jnp.concatenate([prev, t.reshape(B, dilation, nb, BLOCK, H, Dh)], axis=3)

    kb, vb = band(ks), band(vs)
    s = jnp.einsum('bdnqhe,bdnkhe->bdnhqk', qb, kb).astype(jnp.float32) * (Dh ** -0.5)
    qi = jnp.arange(BLOCK)[None, :, None]
    kj = jnp.arange(2 * BLOCK)[None, None, :]
    blk = jnp.arange(nb)[:, None, None]
    dist = BLOCK + qi - kj
    valid = (dist >= 0) & (dist <= n_win) & ((blk > 0) | (kj >= BLOCK))
    s = jnp.where(valid[None, None, :, None], s, -jnp.inf)
    m = jnp.max(s, axis=-1, keepdims=True)
    p = jnp.exp(s - m)
    l = jnp.sum(p, axis=-1, keepdims=True)
    o = jnp.einsum('bdnhqk,bdnkhe->bdnqhe', p / l, vb.astype(jnp.float32))
    o = o.reshape(B, dilation, n, H, Dh).transpose(0, 2, 1, 3, 4).reshape(B, Sp, H, Dh)

    def stat_back(t):
        t = t[..., 0].transpose(0, 1, 2, 4, 3).reshape(B, dilation, n, H)
        return t.transpose(0, 2, 1, 3).reshape(B, Sp, H)

    return o, stat_back(m), stat_back(l)


def dilated_attention(q, k, v):
    B, S, H, Dh = q.shape
    Sp = -(-S // DIL_PAD) * DIL_PAD
    pad = ((0, 0), (0, Sp - S), (0, 0), (0, 0))
    qp, kp, vp = jnp.pad(q, pad), jnp.pad(k, pad), jnp.pad(v, pad)
    os_, ms, ls = [], [], []
    for window, dil in DIL_PAIRS:
        o, m, l = dilated_window(qp, kp, vp, dil, window // dil)
        os_.append(o); ms.append(m); ls.append(l)
    m_all = jnp.stack(ms)
    a = jnp.stack(ls) * jnp.exp(m_all - jnp.max(m_all, axis=0))
    o = jnp.einsum('pbsh,pbshe->bshe', a, jnp.stack(os_)) / jnp.sum(a, axis=0)[..., None]
    return o[:, :S].astype(q.dtype)


def dsa_attention(q, k, v, q_idx, k_idx, w_idx):
    B, S, H, Dh = q.shape
    k_sel = min(DSA_TOPK, S // 4)
    nb = S // BLOCK
    idx_scale = (IDX_HEADS * IDX_DIM) ** -0.5
    kpos = jnp.arange(S)

    def to_blocks(t):
        return t.reshape((B, nb, BLOCK) + t.shape[2:]).swapaxes(0, 1)

    def block(args):
        qb, qib, wib, i = args
        qpos = i * BLOCK + jnp.arange(BLOCK)
        causal = kpos[None, :] <= qpos[:, None]
        dots = jnp.einsum('bqhd,bsd->bqhs', qib, k_idx).astype(jnp.float32)
        score = jnp.einsum('bqhs,bqh->bqs', jax.nn.relu(dots), wib.astype(jnp.float32) * idx_scale)
        score = jnp.where(causal[None], score, -jnp.inf)
        _, sel = lax.top_k(score, k_sel)
        flat = sel.reshape(B, BLOCK * k_sel)
        kg = jax.vmap(lambda a, j: a[j])(k, flat).reshape(B, BLOCK, k_sel, H, Dh)
        vg = jax.vmap(lambda a, j: a[j])(v, flat).reshape(B, BLOCK, k_sel, H, Dh)
        s = jnp.einsum('bqhe,bqkhe->bhqk', qb, kg).astype(jnp.float32) * (Dh ** -0.5)
        ok = sel <= qpos[None, :, None]
        s = jnp.where(ok[:, None], s, -jnp.inf)
        p = jax.nn.softmax(s, axis=-1).astype(v.dtype)
        return jnp.einsum('bhqk,bqkhe->bqhe', p, vg)

    o = lax.map(block, (to_blocks(q), to_blocks(q_idx), to_blocks(w_idx), jnp.arange(nb)))
    return o.swapaxes(0, 1).reshape(B, S, H, Dh)


def causal_dwconv(u, w, b):
    K = w.shape[0]
    S = u.shape[1]
    up = jnp.pad(u, ((0, 0), (K - 1, 0), (0, 0)))
    y = up[:, 0:S] * w[0]
    for j in range(1, K):
        y = y + up[:, j:j + S] * w[j]
    return y + b


def setup_inputs(seed: int = 0) -> dict:
    key = jax.random.key(seed)
    ks = jax.random.split(key, 16)
    f32 = jnp.float32

    def nrm(k, shape, scale):
        return jax.random.normal(k, shape, f32) * scale

    def gain(k, shape):
        return 1.0 + 0.02 * jax.random.normal(k, shape, f32)

    L = DEPTH
    return {
        "x": nrm(ks[0], (BATCH, SEQ, D_MODEL), 1.0),
        "g_attn": gain(ks[1], (L, D_MODEL)),
        "w_in": nrm(ks[2], (L, D_MODEL, N_IN), D_MODEL ** -0.5),
        "g_q_lat": gain(ks[3], (L, Q_LORA)),
        "w_uq": nrm(ks[4], (L, Q_LORA, HEADS_MLA * (MLA_NOPE + MLA_ROPE)), Q_LORA ** -0.5),
        "g_kv_lat": gain(ks[5], (L, KV_LORA)),
        "w_ukv": nrm(ks[6], (L, KV_LORA, HEADS_MLA * (MLA_NOPE + MLA_V)), KV_LORA ** -0.5),
        "w_o": nrm(ks[7], (L, D_MIX, D_MODEL), D_MIX ** -0.5),
        "g_ffn": gain(ks[8], (L, D_MODEL)),
        "w_up": nrm(ks[9], (L, D_MODEL, 2 * D_FF), D_MODEL ** -0.5),
        "conv_w": nrm(ks[10], (L, CONV_WIDTH, 2 * D_FF), CONV_WIDTH ** -0.5),
        "conv_b": nrm(ks[11], (L, 2 * D_FF), 0.01),
        "w_down": nrm(ks[12], (L, D_FF, D_MODEL), D_FF ** -0.5),
        "g_final": gain(ks[13], (D_MODEL,)),
    }


def reference(x, g_attn, w_in, g_q_lat, w_uq, g_kv_lat, w_ukv, w_o,
              g_ffn, w_up, conv_w, conv_b, w_down, g_final):
    B, S, _ = x.shape
    cos_a, sin_a = rope_tables(S, MLA_ROPE)
    cos_h, sin_h = rope_tables(S, HEAD_DIM // ROT_FRAC)
    cos_i, sin_i = rope_tables(S, IDX_DIM // ROT_FRAC)
    offsets = np.cumsum(IN_SPLITS)[:-1].tolist()
    for l in range(DEPTH):
        h = rms_norm(x, g_attn[l])
        z = h @ w_in[l]
        c_q, c_kv, k_pe, z_dil, z_dsa, z_qi, z_ki, z_wi = jnp.split(z, offsets, axis=-1)

        o_a = mla_attention(c_q, c_kv, k_pe, g_q_lat[l], w_uq[l], g_kv_lat[l], w_ukv[l], cos_a, sin_a)

        qkv_b = z_dil.reshape(B, S, 3, HEADS_DIL, HEAD_DIM)
        o_b = dilated_attention(partial_rope(qkv_b[:, :, 0], cos_h, sin_h),
                                partial_rope(qkv_b[:, :, 1], cos_h, sin_h),
                                qkv_b[:, :, 2])

        qkv_c = z_dsa.reshape(B, S, 3, HEADS_DSA, HEAD_DIM)
        q_idx = partial_rope(z_qi.reshape(B, S, IDX_HEADS, IDX_DIM), cos_i, sin_i)
        k_idx = partial_rope(z_ki[:, :, None, :], cos_i, sin_i)[:, :, 0]
        o_c = dsa_attention(partial_rope(qkv_c[:, :, 0], cos_h, sin_h),
                            partial_rope(qkv_c[:, :, 1], cos_h, sin_h),
                            qkv_c[:, :, 2], q_idx, k_idx, z_wi)

        mix = jnp.concatenate([o_a.reshape(B, S, -1), o_b.reshape(B, S, -1),
                               o_c.reshape(B, S, -1)], axis=-1)
        x = x + mix @ w_o[l]

        h = rms_norm(x, g_ffn[l])
        u = causal_dwconv(h @ w_up[l], conv_w[l], conv_b[l])
        gate, up = u[..., :D_FF], u[..., D_FF:]
        x = x + (jax.nn.silu(gate) * up) @ w_down[l]
    return rms_norm(x, g_final)
```

```python
import numpy as np
import ml_dtypes
from contextlib import ExitStack
import concourse.bass as bass
import concourse.mybir as mybir
from concourse.bass_utils import run_bass_kernel_spmd

F32 = mybir.dt.float32
BF16 = mybir.dt.bfloat16
AF = mybir.ActivationFunctionType
ALU = mybir.AluOpType
AX = mybir.AxisListType

EPS = 1e-6
NEG = -1.0e30
D = 1024
NIN = 3016
DFF = 2816
TOPK = 256
NBIS = 17


class Res:
    __slots__ = ("name", "w", "r", "ordered")

    def __init__(self, name, ordered=True):
        self.name = name
        self.w = {}
        self.r = {}
        self.ordered = ordered


class Eng:
    def __init__(self, name, eng, sem):
        self.name = name
        self.eng = eng
        self.sem = sem
        self.n = 0
        self.waited = {}
        self.dsem = []
        self.dval = []
        self.di = 0


class Sched:
    K = 12

    def __init__(self, nc, es):
        self.nc = nc
        self.E = {}
        for name, e in (("pe", nc.tensor), ("act", nc.scalar), ("dve", nc.vector),
                        ("pool", nc.gpsimd), ("sp", nc.sync)):
            self.E[name] = Eng(name, e, es.enter_context(nc.semaphore("p_" + name)))
        for q in ("sp", "pool", "act"):
            E = self.E[q]
            for i in range(self.K):
                E.dsem.append(es.enter_context(nc.semaphore("d_%s%d" % (q, i))))
                E.dval.append(0)
        self.ninst = 0

    def _need(self, reads, writes):
        need = {}

        def add(d):
            for k, sv in d.items():
                if k not in need or need[k][1] < sv[1]:
                    need[k] = sv
        for r in reads:
            add(r.w)
        for w in writes:
            add(w.r)
            if w.ordered:
                add(w.w)
        return need

    def _emit_waits(self, E, need, is_dma):
        for k, (s, v) in need.items():
            if s is E.sem and not is_dma and E.name == "pe":
                continue
            if E.waited.get(k, 0) >= v:
                continue
            E.eng.wait_ge(s, v)
            E.waited[k] = v

    def _mark(self, tok, reads, writes):
        k = id(tok[0])
        for r in reads:
            if r.r.get(k, (None, 0))[1] < tok[1]:
                r.r[k] = tok
        for w in writes:
            if w.ordered:
                w.w = {k: tok}
                w.r = {}
            else:
                w.w[k] = tok

    def op(self, eng, fn, reads=(), writes=()):
        E = self.E[eng]
        self._emit_waits(E, self._need(reads, writes), False)
        inst = fn(E.eng)
        E.n += 1
        inst.then_inc(E.sem, 1)
        self._mark((E.sem, E.n), reads, writes)
        self.ninst += 1

    def dma(self, q, out, in_, reads=(), writes=()):
        E = self.E[q]
        self._emit_waits(E, self._need(reads, writes), True)
        i = E.di % self.K
        E.di += 1
        s = E.dsem[i]
        pv = E.dval[i]
        if pv > 0 and E.waited.get(id(s), 0) < pv:
            E.eng.wait_ge(s, pv)
            E.waited[id(s)] = pv
        E.eng.dma_start(out=out, in_=in_).then_inc(s, 16)
        E.dval[i] = pv + 16
        self._mark((s, pv + 16), reads, writes)
        self.ninst += 1

    def barrier(self):
        for E in self.E.values():
            for O in self.E.values():
                if O is not E and O.n > 0 and E.waited.get(id(O.sem), 0) < O.n:
                    E.eng.wait_ge(O.sem, O.n)
                    E.waited[id(O.sem)] = O.n
                for s, v in zip(O.dsem, O.dval):
                    if v > 0 and E.waited.get(id(s), 0) < v:
                        E.eng.wait_ge(s, v)
                        E.waited[id(s)] = v


class Ring:
    def __init__(self, items):
        self.items = items
        self.i = 0

    def next(self):
        it = self.items[self.i % len(self.items)]
        self.i += 1
        return it


def build(S=8192, L=4, dbg=False, stop_after=None, skip=()):
    NT = S // 512
    NB = S // 128
    nc = bass.Bass("TRN2", target_bir_lowering=False)

    def din(name, shape, dt=F32):
        return nc.dram_tensor(name, list(shape), dt, kind="ExternalInput").ap()

    def dscr(name, shape, dt):
        return nc.dram_tensor(name, list(shape), dt, kind=("ExternalOutput" if dbg else "Internal")).ap()

    x_in = din("x", [S, D])
    w_in = din("w_in", [L, D, NIN])
    w_uq = din("w_uq", [L, 256, 384])
    w_ukv = din("w_ukv", [L, 128, 512])
    w_o = din("w_o", [L, D, D])
    w_up = din("w_up", [L, D, 2 * DFF])
    w_down = din("w_down", [L, DFF, D])
    gA_d = din("gA", [128, L * 8])
    gF_d = din("gF", [128, L * 8])
    gq_d = din("gq", [128, L * 2])
    gkv_d = din("gkv", [128, L])
    cw_d = din("cw", [128, L * 3 * 44])
    cb_d = din("cb", [128, L * 44])
    gfin_d = din("gfin", [128, D])
    c_ident = din("c_ident", [128, 128], BF16)
    c_ones = din("c_ones", [128, 128], BF16)
    c_sel = din("c_sel", [128, 64])
    c_band = din("c_band", [128, 256], BF16)
    c_negtri = din("c_negtri", [128, 128])
    c_pw2 = din("c_pw2", [128, NBIS + 1])
    tab64 = din("tab64", [2, 128, S])
    tabidx = din("tabidx", [2, 128, S])
    tabmla = din("tabmla", [2, 96, S])
    y_out = nc.dram_tensor("y", [S, D], F32, kind="ExternalOutput").ap()

    xres0 = dscr("xres0", [S, D], F32)
    xres1 = dscr("xres1", [S, D], F32)
    QTm = dscr("QTm", [4, 96, S], BF16)
    KTm = dscr("KTm", [4, 96, S], BF16)
    Vm = dscr("Vm", [S, 256], BF16)
    QTd = dscr("QTd", [4, 128, S], BF16)
    KTd = dscr("KTd", [4, 128, S], BF16)
    Vd = dscr("Vd", [S, 512], BF16)
    QTs = dscr("QTs", [2, 128, S], BF16)
    KTs = dscr("KTs", [2, 128, S], BF16)
    Vs = dscr("Vs", [S, 256], BF16)
    QIT = dscr("QIT", [3, 96, S], BF16)
    KIT = dscr("KIT", [128, S], BF16)
    WI = dscr("WI", [S, 8], F32)
    mixT = dscr("mixT", [D, S], BF16)
    actT = dscr("actT", [DFF, S], BF16)

    ges = ExitStack()
    with ges:
        SC = Sched(nc, ges)
        R_x = Res("xres0", ordered=False)
        R_x1 = Res("xres1", ordered=False)
        R_proj = Res("proj", ordered=False)
        R_mix = Res("mixT", ordered=False)
        R_act = Res("actT", ordered=False)
        R_y = Res("y", ordered=False)
        R_const = Res("const")

        def gsb(name, shape, dt):
            return ges.enter_context(nc.sbuf_tensor("g_" + name, shape, dt))

        PS = []
        for i in range(8):
            t = ges.enter_context(nc.psum_tensor("ps%d" % i, [128, 512], F32))
            PS.append((t, Res("ps%d" % i)))

        gA = gsb("gA", [128, L * 8], F32)
        gF = gsb("gF", [128, L * 8], F32)
        gq = gsb("gq", [128, L * 2], F32)
        gkv = gsb("gkv", [128, L], F32)
        cw = gsb("cw", [128, L * 3 * 44], F32)
        cb = gsb("cb", [128, L * 44], F32)
        ident = gsb("ident", [128, 128], BF16)
        ones = gsb("ones", [128, 128], BF16)
        sel = gsb("sel", [128, 64], F32)
        band = gsb("band", [128, 256], BF16)
        negtri = gsb("negtri", [128, 128], F32)
        pw2 = gsb("pw2", [128, NBIS + 1], F32)
        for dst, src in ((gA, gA_d), (gF, gF_d), (gq, gq_d), (gkv, gkv_d), (cw, cw_d), (cb, cb_d),
                         (ident, c_ident), (ones, c_ones), (sel, c_sel), (band, c_band), (negtri, c_negtri), (pw2, c_pw2)):
            SC.dma("sp", dst[:], src[:, :], reads=(), writes=(R_const,))
        R_const.ordered = False

        def mm(out, lhsT, rhs, start, stop, reads, writes):
            SC.op("pe", lambda e: e.matmul(out, lhsT=lhsT, rhs=rhs, start=start, stop=stop), reads, writes)

        def tr(out, in_, reads, writes):
            SC.op("pe", lambda e: e.transpose(out, in_, ident[:]), reads, writes)

        def act(out, in_, func, reads, writes, scale=1.0, bias=None, accum_out=None):
            kw = {}
            if bias is not None:
                kw["bias"] = bias
            if accum_out is not None:
                kw["accum_out"] = accum_out
            SC.op("act", lambda e: e.activation(out=out, in_=in_, func=func, scale=scale, **kw), reads, writes)

        def ts(eng, out, in0, s1, s2, op0, op1, reads, writes, accum_out=None):
            kw = {}
            if accum_out is not None:
                kw["accum_out"] = accum_out
            if op1 is None:
                SC.op(eng, lambda e: e.tensor_scalar(out=out, in0=in0, scalar1=s1, scalar2=None, op0=op0, **kw),
                      reads, writes)
            else:
                SC.op(eng, lambda e: e.tensor_scalar(out=out, in0=in0, scalar1=s1, scalar2=s2, op0=op0, op1=op1, **kw),
                      reads, writes)

        def tt(eng, out, in0, in1, op, reads, writes):
            SC.op(eng, lambda e: e.tensor_tensor(out=out, in0=in0, in1=in1, op=op), reads, writes)

        def stt(out, in0, scalar, in1, op0, op1, reads, writes):
            SC.op("dve", lambda e: e.scalar_tensor_tensor(out=out, in0=in0, scalar=scalar, in1=in1, op0=op0, op1=op1),
                  reads, writes)

        def cp(eng, out, in_, reads, writes):
            if eng == "act":
                SC.op("act", lambda e: e.activation(out=out, in_=in_, func=AF.Copy), reads, writes)
            else:
                SC.op(eng, lambda e: e.tensor_copy(out=out, in_=in_), reads, writes)

        def mset(eng, ap, val, writes):
            SC.op(eng, lambda e: e.memset(ap, val), (), writes)

        uid = [0]
        dq = ["sp", "pool"]
        dqi = [0]

        def dma(out, in_, reads, writes, q=None):
            if q is None:
                q = dq[dqi[0] % 2]
                dqi[0] += 1
            SC.dma(q, out, in_, reads, writes)

        def rms_rows(es, xt, R_xt, hb, R_hb, name):
            pass

        def phase_A(l, xsrc):
            uid[0] += 1
            with ExitStack() as es:
                def sb(name, shape, dt):
                    return es.enter_context(nc.sbuf_tensor(name + "_A_%d" % uid[0], shape, dt))
                Win = sb("Win", [128, 8, NIN], BF16)
                WinR = sb("WinR", [128, 8, 1920], BF16)
                Wki = sb("Wki", [128, 8, 128], BF16)
                Wkp = sb("Wkp", [128, 8, 96], BF16)
                WkpR = sb("WkpR", [128, 8, 96], BF16)
                Wuq = sb("Wuq", [128, 2, 384], BF16)
                WuqR = sb("WuqR", [128, 2, 384], BF16)
                Wukv = sb("Wukv", [128, 512], BF16)
                Wkk = sb("Wkk", [128, 4, 96], BF16)
                Wv = sb("Wv", [128, 256], BF16)
                R_W = Res("W_A")
                stg = Ring([(sb("stg%d" % i, [128, 1508], F32), Res("stg%d" % i)) for i in range(2)])
                for k in range(8):
                    for hf in range(2):
                        st, R_st = stg.next()
                        dma(st[:], w_in[l, 128 * k:128 * k + 128, 1508 * hf:1508 * hf + 1508], (), (R_st,))
                        ts("dve", Win[:, k, 1508 * hf:1508 * hf + 1508], st[:], gA[:, l * 8 + k:l * 8 + k + 1], None,
                           ALU.mult, None, (R_st, R_const), (R_W,))
                for c in range(2):
                    st, R_st = stg.next()
                    dma(st[:, 0:384], w_uq[l, 128 * c:128 * c + 128, :], (), (R_st,))
                    ts("dve", Wuq[:, c, :], st[:, 0:384], gq[:, l * 2 + c:l * 2 + c + 1], None, ALU.mult, None,
                       (R_st, R_const), (R_W,))
                st, R_st = stg.next()
                dma(st[:, 0:512], w_ukv[l, :, :], (), (R_st,))
                ts("dve", Wukv[:], st[:, 0:512], gkv[:, l:l + 1], None, ALU.mult, None, (R_st, R_const), (R_W,))
                mset("pool", WinR[:], 0.0, (R_W,))
                mset("pool", Wkp[:], 0.0, (R_W,))
                mset("pool", WkpR[:], 0.0, (R_W,))
                mset("pool", WuqR[:], 0.0, (R_W,))
                mset("pool", Wkk[:], 0.0, (R_W,))
                RW = (R_W,)
                for k in range(8):
                    for base, nh, rb in ((416, 8, 0), (928, 8, 512), (1952, 4, 1024), (2208, 4, 1280)):
                        src = Win[:, k, base:base + nh * 64].rearrange("p (h e) -> p h e", e=64)
                        dst = WinR[:, k, rb:rb + nh * 64].rearrange("p (h e) -> p h e", e=64)
                        ts("dve", dst[:, :, 0:8], src[:, :, 8:16], -1.0, None, ALU.mult, None, RW, RW)
                        cp("dve", dst[:, :, 8:16], src[:, :, 0:8], RW, RW)
                    src = Win[:, k, 2720:2976].rearrange("p (h e) -> p h e", e=32)
                    dst = WinR[:, k, 1536:1792].rearrange("p (h e) -> p h e", e=32)
                    ts("dve", dst[:, :, 0:4], src[:, :, 4:8], -1.0, None, ALU.mult, None, RW, RW)
                    cp("dve", dst[:, :, 4:8], src[:, :, 0:4], RW, RW)
                    for r in range(4):
                        cp("dve", Wki[:, k, 32 * r:32 * r + 32], Win[:, k, 2976:3008], RW, RW)
                        ts("dve", WinR[:, k, 1792 + 32 * r:1792 + 32 * r + 4], Win[:, k, 2980:2984], -1.0, None,
                           ALU.mult, None, RW, RW)
                        cp("dve", WinR[:, k, 1792 + 32 * r + 4:1792 + 32 * r + 8], Win[:, k, 2976:2980], RW, RW)
                    cp("dve", Wkp[:, k, 64:96], Win[:, k, 384:416], RW, RW)
                    ts("dve", WkpR[:, k, 64:80], Win[:, k, 400:416], -1.0, None, ALU.mult, None, RW, RW)
                    cp("dve", WkpR[:, k, 80:96], Win[:, k, 384:400], RW, RW)
                for c in range(2):
                    src = Wuq[:, c, :].rearrange("p (h e) -> p h e", e=96)
                    dst = WuqR[:, c, :].rearrange("p (h e) -> p h e", e=96)
                    ts("dve", dst[:, :, 64:80], src[:, :, 80:96], -1.0, None, ALU.mult, None, RW, RW)
                    cp("dve", dst[:, :, 80:96], src[:, :, 64:80], RW, RW)
                ukv = Wukv[:].rearrange("p (h t e) -> p h t e", t=2, e=64)
                cp("dve", Wkk[:, :, 0:64], ukv[:, :, 0, :], RW, RW)
                cp("dve", Wv[:].rearrange("p (h e) -> p h e", e=64), ukv[:, :, 1, :], RW, RW)

                xt_r = Ring([(sb("xt%d" % i, [128, D], F32), Res("xt%d" % i)) for i in range(2)])
                junk = sb("junk", [128, D], BF16)
                R_junk = Res("junk")
                hb_r = Ring([(sb("hb%d" % i, [128, D], BF16), Res("hb%d" % i)) for i in range(2)])
                st_r = Ring([(sb("st%d" % i, [128, 4], F32), Res("st%d" % i)) for i in range(2)])
                hT_r = Ring([(sb("hT%d" % i, [128, 8, 512], BF16), Res("hT%d" % i)) for i in range(2)])
                tb_r = Ring([(sb("tb%d" % i, [128, 6, 512], F32), Res("tb%d" % i)) for i in range(2)])
                t1_r = Ring([(sb("t1_%d" % i, [128, 512], F32), Res("t1_%d" % i)) for i in range(2)])
                t2_r = Ring([(sb("t2_%d" % i, [128, 512], F32), Res("t2_%d" % i)) for i in range(2)])
                ob_r = Ring([(sb("ob%d" % i, [128, 512], BF16), Res("ob%d" % i)) for i in range(4)])
                sq_r = Ring([(sb("sq%d" % i, [128, 512], BF16), Res("sq%d" % i)) for i in range(2)])
                rs_r = Ring([(sb("rs%d" % i, [128, 512], F32), Res("rs%d" % i)) for i in range(2)])
                cqn = sb("cqn", [128, 2, 512], BF16)
                R_cqn = Res("cqn")
                ckvn = sb("ckvn", [128, 512], BF16)
                R_ckvn = Res("ckvn")
                vo_r = Ring([(sb("vo%d" % i, [128, 1024], BF16), Res("vo%d" % i)) for i in range(2)])
                wo_r = Ring([(sb("wo%d" % i, [128, 8], F32), Res("wo%d" % i)) for i in range(2)])
                bank = Ring(PS)

                def rope_out(rows, pz, R_pz, pr, R_pr, tC, tS, R_tb, dst_ap):
                    t1, R_t1 = t1_r.next()
                    t2, R_t2 = t2_r.next()
                    ob, R_ob = ob_r.next()
                    tt("dve", t1[0:rows, :], pz[0:rows, :], tC, ALU.mult, (R_pz, R_tb), (R_t1,))
                    tt("dve", t2[0:rows, :], pr[0:rows, :], tS, ALU.mult, (R_pr, R_tb), (R_t2,))
                    tt("pool", ob[0:rows, :], t1[0:rows, :], t2[0:rows, :], ALU.add, (R_t1, R_t2), (R_ob,))
                    dma(dst_ap, ob[0:rows, :], (R_ob,), (R_proj,))

                for ti in range(NT):
                    t0 = 512 * ti
                    hT, R_hT = hT_r.next()
                    for b in range(4):
                        xt, R_xt = xt_r.next()
                        hb, R_hb = hb_r.next()
                        stt_, R_st_ = st_r.next()
                        dma(xt[:], xsrc[t0 + 128 * b:t0 + 128 * b + 128, :], (R_x,), (R_xt,))
                        act(junk[:], xt[:], AF.Square, (R_xt,), (R_junk, R_st_), accum_out=stt_[:, 0:1])
                        act(stt_[:, 1:2], stt_[:, 0:1], AF.Sqrt, (R_st_,), (R_st_,), scale=1.0 / D, bias=EPS)
                        SC.op("dve", lambda e, o=stt_[:, 2:3], i=stt_[:, 1:2]: e.reciprocal(out=o, in_=i), (R_st_,), (R_st_,))
                        ts("dve", hb[:], xt[:], stt_[:, 2:3], None, ALU.mult, None, (R_xt, R_st_), (R_hb,))
                        pt_, R_pt = bank.next()
                        ptb = pt_[:, :].bitcast(BF16)
                        for k in range(8):
                            tr(ptb[:, 128 * k:128 * k + 128], hb[:, 128 * k:128 * k + 128], (R_hb, R_const), (R_pt,))
                        cp("act", hT[:, :, 128 * b:128 * b + 128], ptb.rearrange("p (k t) -> p k t", t=128),
                           (R_pt,), (R_hT,))
                    tb, R_tb = tb_r.next()
                    dma(tb[:, 0, :], tab64[0, :, t0:t0 + 512], (), (R_tb,))
                    dma(tb[:, 1, :], tab64[1, :, t0:t0 + 512], (), (R_tb,))
                    dma(tb[:, 2, :], tabidx[0, :, t0:t0 + 512], (), (R_tb,))
                    dma(tb[:, 3, :], tabidx[1, :, t0:t0 + 512], (), (R_tb,))
                    dma(tb[0:96, 4, :], tabmla[0, :, t0:t0 + 512], (), (R_tb,))
                    dma(tb[0:96, 5, :], tabmla[1, :, t0:t0 + 512], (), (R_tb,))
                    blocks = []
                    for i in range(4):
                        blocks.append((Win, 416 + 128 * i, WinR, 128 * i, 0, QTd[i, :, t0:t0 + 512], 128))
                    for i in range(4):
                        blocks.append((Win, 928 + 128 * i, WinR, 512 + 128 * i, 0, KTd[i, :, t0:t0 + 512], 128))
                    for i in range(2):
                        blocks.append((Win, 1952 + 128 * i, WinR, 1024 + 128 * i, 0, QTs[i, :, t0:t0 + 512], 128))
                    for i in range(2):
                        blocks.append((Win, 2208 + 128 * i, WinR, 1280 + 128 * i, 0, KTs[i, :, t0:t0 + 512], 128))
                    for i in range(3):
                        nr = 96 if i < 2 else 64
                        blocks.append((Win, 2720 + 96 * i, WinR, 1536 + 96 * i, 2, QIT[i, 0:nr, t0:t0 + 512], nr))
                    blocks.append((Wki, 0, WinR, 1792, 2, KIT[:, t0:t0 + 512], 128))
                    for (Wz, cz, Wr, cr, tbi, dst, nr) in blocks:
                        pz, R_pz = bank.next()
                        pr, R_pr = bank.next()
                        for k in range(8):
                            mm(pz[0:nr, :], Wz[:, k, cz:cz + nr], hT[:, k, :], k == 0, k == 7, (R_W, R_hT), (R_pz,))
                        for k in range(8):
                            mm(pr[0:nr, :], Wr[:, k, cr:cr + nr], hT[:, k, :], k == 0, k == 7, (R_W, R_hT), (R_pr,))
                        rope_out(nr, pz, R_pz, pr, R_pr, tb[0:nr, tbi, :], tb[0:nr, tbi + 1, :], R_tb, dst)
                    pq0, R_pq0 = bank.next()
                    pq1, R_pq1 = bank.next()
                    pkv, R_pkv = bank.next()
                    for (pp, R_pp, c0) in ((pq0, R_pq0, 0), (pq1, R_pq1, 128), (pkv, R_pkv, 256)):
                        for k in range(8):
                            mm(pp[:, :], Win[:, k, c0:c0 + 128], hT[:, k, :], k == 0, k == 7, (R_W, R_hT), (R_pp,))
                    pss, R_pss = bank.next()
                    sqs = []
                    for (pp, R_pp) in ((pq0, R_pq0), (pq1, R_pq1)):
                        sq, R_sq = sq_r.next()
                        act(sq[:], pp[:, :], AF.Square, (R_pp,), (R_sq,))
                        sqs.append((sq, R_sq))
                    for i, (sq, R_sq) in enumerate(sqs):
                        mm(pss[:, :], ones[:], sq[:], i == 0, i == 1, (R_const, R_sq), (R_pss,))
                    rs, R_rs = rs_r.next()
                    act(rs[:], pss[:, :], AF.Sqrt, (R_pss,), (R_rs,), scale=1.0 / 256, bias=EPS)
                    SC.op("dve", lambda e, o=rs[:], i=rs[:]: e.reciprocal(out=o, in_=i), (R_rs,), (R_rs,))
                    tt("dve", cqn[:, 0, :], pq0[:, :], rs[:], ALU.mult, (R_pq0, R_rs), (R_cqn,))
                    tt("dve", cqn[:, 1, :], pq1[:, :], rs[:], ALU.mult, (R_pq1, R_rs), (R_cqn,))
                    pss, R_pss = bank.next()
                    sq, R_sq = sq_r.next()
                    act(sq[:], pkv[:, :], AF.Square, (R_pkv,), (R_sq,))
                    mm(pss[:, :], ones[:], sq[:], True, True, (R_const, R_sq), (R_pss,))
                    rs, R_rs = rs_r.next()
                    act(rs[:], pss[:, :], AF.Sqrt, (R_pss,), (R_rs,), scale=1.0 / 128, bias=EPS)
                    SC.op("dve", lambda e, o=rs[:], i=rs[:]: e.reciprocal(out=o, in_=i), (R_rs,), (R_rs,))
                    tt("dve", ckvn[:], pkv[:, :], rs[:], ALU.mult, (R_pkv, R_rs), (R_ckvn,))
                    for h in range(4):
                        pz, R_pz = bank.next()
                        pr, R_pr = bank.next()
                        for c in range(2):
                            mm(pz[0:96, :], Wuq[:, c, 96 * h:96 * h + 96], cqn[:, c, :], c == 0, c == 1,
                               (R_W, R_cqn), (R_pz,))
                        for c in range(2):
                            mm(pr[0:96, :], WuqR[:, c, 96 * h:96 * h + 96], cqn[:, c, :], c == 0, c == 1,
                               (R_W, R_cqn), (R_pr,))
                        rope_out(96, pz, R_pz, pr, R_pr, tb[0:96, 4, :], tb[0:96, 5, :], R_tb, QTm[h, :, t0:t0 + 512])
                    for h in range(4):
                        pz, R_pz = bank.next()
                        pr, R_pr = bank.next()
                        mm(pz[0:96, :], Wkk[:, h, :], ckvn[:], True, False, (R_W, R_ckvn), (R_pz,))
                        for k in range(8):
                            mm(pz[0:96, :], Wkp[:, k, :], hT[:, k, :], False, k == 7, (R_W, R_hT), (R_pz,))
                        for k in range(8):
                            mm(pr[0:96, :], WkpR[:, k, :], hT[:, k, :], k == 0, k == 7, (R_W, R_hT), (R_pr,))
                        rope_out(96, pz, R_pz, pr, R_pr, tb[0:96, 4, :], tb[0:96, 5, :], R_tb, KTm[h, :, t0:t0 + 512])
                    for b in range(4):
                        tsl = slice(128 * b, 128 * b + 128)
                        r0 = t0 + 128 * b
                        vo, R_vo = vo_r.next()
                        wo, R_wo = wo_r.next()
                        p1, R_p1 = bank.next()
                        for k in range(8):
                            mm(p1[:, :], hT[:, k, tsl], Win[:, k, 1440:1952], k == 0, k == 7, (R_W, R_hT), (R_p1,))
                        cp("act", vo[:, 0:512], p1[:, :], (R_p1,), (R_vo,))
                        p2, R_p2 = bank.next()
                        for k in range(8):
                            mm(p2[:, 0:256], hT[:, k, tsl], Win[:, k, 2464:2720], k == 0, k == 7, (R_W, R_hT), (R_p2,))
                        cp("act", vo[:, 512:768], p2[:, 0:256], (R_p2,), (R_vo,))
                        p3, R_p3 = bank.next()
                        for k in range(8):
                            mm(p3[:, 0:8], hT[:, k, tsl], Win[:, k, 3008:3016], k == 0, k == 7, (R_W, R_hT), (R_p3,))
                        cp("act", wo[:], p3[:, 0:8], (R_p3,), (R_wo,))
                        p4, R_p4 = bank.next()
                        mm(p4[:, 0:256], ckvn[:, tsl], Wv[:], True, True, (R_W, R_ckvn), (R_p4,))
                        cp("act", vo[:, 768:1024], p4[:, 0:256], (R_p4,), (R_vo,))
                        dma(Vd[r0:r0 + 128, :], vo[:, 0:512], (R_vo,), (R_proj,))
                        dma(Vs[r0:r0 + 128, :], vo[:, 512:768], (R_vo,), (R_proj,))
                        dma(Vm[r0:r0 + 128, :], vo[:, 768:1024], (R_vo,), (R_proj,))
                        dma(WI[r0:r0 + 128, :], wo[:], (R_wo,), (R_proj,))
                SC.barrier()


        def phase_B():
            scale = 96 ** -0.5
            uid[0] += 1
            with ExitStack() as es:
                def sb(name, shape, dt):
                    return es.enter_context(nc.sbuf_tensor(name + "_B_%d" % uid[0], shape, dt))
                KT = sb("KT", [96, S], BF16)
                QT = sb("QT", [96, S], BF16)
                Vx = sb("Vx", [128, NB, 66], BF16)
                R_K, R_Q, R_V = Res("K"), Res("Q"), Res("V")
                pt_r = Ring([(sb("pt%d" % i, [128, 512], BF16), Res("pt%d" % i)) for i in range(3)])
                os_r = Ring([(sb("os%d" % i, [128, 512], F32), Res("os%d" % i)) for i in range(2)])
                rr_r = Ring([(sb("rr%d" % i, [64, 512], F32), Res("rr%d" % i)) for i in range(2)])
                ob_r = Ring([(sb("ob%d" % i, [64, 512], BF16), Res("ob%d" % i)) for i in range(2)])
                s_r = Ring(PS[0:4])
                od_r = Ring(PS[4:6])
                pd_r = Ring(PS[6:8])
                mset("pool", Vx[:, :, 64:66], 1.0, (R_V,))
                for h in range(4):
                    dma(KT[:], KTm[h, :, :], (R_proj,), (R_K,), q="sp")
                    dma(QT[:], QTm[h, :, :], (R_proj,), (R_Q,), q="sp")
                    dma(Vx[:, :, 0:64], Vm[:, 64 * h:64 * h + 64].rearrange("(j p) e -> p j e", p=128),
                        (R_proj,), (R_V,), q="sp")
                    for g in range(NT):
                        od, R_od = od_r.next()
                        nj = 4 * g + 4
                        for j in range(nj):
                            jj = j - 4 * g
                            c0 = 128 * jj if jj > 0 else 0
                            ps, R_ps = s_r.next()
                            mm(ps[:, c0:512], KT[:, 128 * j:128 * j + 128], QT[:, 512 * g + c0:512 * g + 512],
                               True, True, (R_K, R_Q), (R_ps,))
                            pt, R_pt = pt_r.next()
                            act(pt[:, c0:512], ps[:, c0:512], AF.Exp, (R_ps,), (R_pt,), scale=scale)
                            if jj >= 0:
                                tt("dve", pt[:, 128 * jj:128 * jj + 128], pt[:, 128 * jj:128 * jj + 128],
                                   band[:, 0:128], ALU.mult, (R_pt, R_const), (R_pt,))
                            mm(od[0:65, c0:512], Vx[:, j, 0:65], pt[:, c0:512], j == 0, j == nj - 1,
                               (R_V, R_pt), (R_od,))
                        osb, R_os = os_r.next()
                        cp("act", osb[0:65, :], od[0:65, :], (R_od,), (R_os,))
                        pd, R_pd = pd_r.next()
                        mm(pd[0:64, :], sel[0:65, :], osb[0:65, :], True, True, (R_const, R_os), (R_pd,))
                        rr, R_rr = rr_r.next()
                        SC.op("dve", lambda e, o=rr[:, :], i=pd[0:64, :]: e.reciprocal(out=o, in_=i), (R_pd,), (R_rr,))
                        ob, R_ob = ob_r.next()
                        tt("dve", ob[:, :], osb[0:64, :], rr[:, :], ALU.mult, (R_os, R_rr), (R_ob,))
                        dma(mixT[64 * h:64 * h + 64, 512 * g:512 * g + 512], ob[:, :], (R_ob,), (R_mix,))
                SC.barrier()

        def phase_C():
            scale = 64 ** -0.5
            uid[0] += 1
            with ExitStack() as es:
                def sb(name, shape, dt):
                    return es.enter_context(nc.sbuf_tensor(name + "_C_%d" % uid[0], shape, dt))
                KT = sb("KT", [128, S], BF16)
                QT = sb("QT", [128, S], BF16)
                R_K, R_Q = Res("K"), Res("Q")
                vx_r = Ring([(sb("Vx%d" % i, [128, NB, 66], BF16), Res("Vx%d" % i)) for i in range(2)])
                acc = sb("acc", [128, S], F32)
                R_acc = Res("acc")
                pt_r = Ring([(sb("pt%d" % i, [128, 256], BF16), Res("pt%d" % i)) for i in range(3)])
                rr_r = Ring([(sb("rr%d" % i, [64, 512], F32), Res("rr%d" % i)) for i in range(2)])
                ob_r = Ring([(sb("ob%d" % i, [64, 512], BF16), Res("ob%d" % i)) for i in range(2)])
                s_r = Ring(PS[0:4])
                od_b = PS[4:6]
                pd_r = Ring(PS[6:8])
                for (vx, R_vx) in vx_r.items:
                    mset("pool", vx[:, :, 64:66], 1.0, (R_vx,))
                for i in range(4):
                    dma(KT[:], KTd[i, :, :], (R_proj,), (R_K,), q="sp")
                    dma(QT[:], QTd[i, :, :], (R_proj,), (R_Q,), q="sp")
                    for hh in range(2):
                        h = 2 * i + hh
                        rows = slice(64 * hh, 64 * hh + 64)
                        for pi, d in enumerate((1, 4, 16)):
                            vx, R_vx = vx_r.next()
                            vsrc = Vd[:, 64 * h:64 * h + 64].rearrange("(n i d) e -> i n d e", i=128, d=d)
                            for r in range(d):
                                dma(vx[:, r:NB:d, 0:64], vsrc[:, :, r, :], (R_proj,), (R_vx,), q="sp")
                            nblk = S // (128 * d)
                            for r in range(d):
                                for m in range(nblk):
                                    blk = m * d + r
                                    ks = 128 * m * d + r
                                    nq = 256 if m + 1 < nblk else 128
                                    ps, R_ps = s_r.next()
                                    mm(ps[:, 0:nq], KT[rows, ks:ks + 127 * d + 1:d], QT[rows, ks:ks + (nq - 1) * d + 1:d],
                                       True, True, (R_K, R_Q), (R_ps,))
                                    pt, R_pt = pt_r.next()
                                    act(pt[:, 0:nq], ps[:, 0:nq], AF.Exp, (R_ps,), (R_pt,), scale=scale)
                                    tt("pool", pt[:, 0:nq], pt[:, 0:nq], band[:, 0:nq], ALU.mult, (R_pt, R_const), (R_pt,))
                                    od, R_od = od_b[m % 2]
                                    mm(od[0:65, 0:128], vx[:, blk, 0:65], pt[:, 0:128], m == 0, True,
                                       (R_vx, R_pt), (R_od,))
                                    dst = acc[0:65, ks:ks + 127 * d + 1:d]
                                    if pi == 0:
                                        cp("dve", dst, od[0:65, 0:128], (R_od,), (R_acc,))
                                    else:
                                        tt("dve", dst, dst, od[0:65, 0:128], ALU.add, (R_od, R_acc), (R_acc,))
                                    if m + 1 < nblk:
                                        od2, R_od2 = od_b[(m + 1) % 2]
                                        mm(od2[0:65, 0:128], vx[:, blk, 0:65], pt[:, 128:256], True, False,
                                           (R_vx, R_pt), (R_od2,))
                        for g in range(NT):
                            pd, R_pd = pd_r.next()
                            mm(pd[0:64, :], sel[0:65, :], acc[0:65, 512 * g:512 * g + 512], True, True,
                               (R_const, R_acc), (R_pd,))
                            rr, R_rr = rr_r.next()
                            SC.op("dve", lambda e, o=rr[:, :], i_=pd[0:64, :]: e.reciprocal(out=o, in_=i_), (R_pd,), (R_rr,))
                            ob, R_ob = ob_r.next()
                            tt("dve", ob[:, :], acc[0:64, 512 * g:512 * g + 512], rr[:, :], ALU.mult,
                               (R_acc, R_rr), (R_ob,))
                            dma(mixT[256 + 64 * h:256 + 64 * h + 64, 512 * g:512 * g + 512], ob[:, :], (R_ob,), (R_mix,))
                SC.barrier()

        def phase_D():
            scale = 64 ** -0.5
            uid[0] += 1
            with ExitStack() as es:
                def sb(name, shape, dt):
                    return es.enter_context(nc.sbuf_tensor(name + "_D_%d" % uid[0], shape, dt))
                KI = sb("KI", [128, S], BF16)
                KT = sb("KT", [128, 2, S], BF16)
                Vx = sb("Vx", [128, NB, 4, 66], BF16)
                WIt = sb("WIt", [128, NB, 8], F32)
                R_KI, R_K, R_V, R_WI = Res("KI"), Res("K"), Res("V"), Res("WI")
                Isc = sb("Isc", [128, S], F32)
                Msk = sb("Msk", [128, S], BF16)
                MskT = sb("MskT", [128, NB, 128], BF16)
                R_I, R_M, R_MT = Res("I"), Res("M"), Res("MT")
                qi_r = Ring([(sb("qi%d" % i, [96, 3, 128], BF16), Res("qi%d" % i)) for i in range(2)])
                qs_r = Ring([(sb("qs%d" % i, [128, 2, 128], BF16), Res("qs%d" % i)) for i in range(2)])
                rl_r = Ring([(sb("rl%d" % i, [128, 512], F32), Res("rl%d" % i)) for i in range(2)])
                pt_r = Ring([(sb("pt%d" % i, [128, 512], BF16), Res("pt%d" % i)) for i in range(3)])
                st_r = Ring([(sb("st%d" % i, [128, 8], F32), Res("st%d" % i)) for i in range(2)])
                wk_r = Ring([(sb("wk%d" % i, [128, NBIS + 1], F32), Res("wk%d" % i)) for i in range(2)])
                os_r = Ring([(sb("os%d" % i, [128, 128], F32), Res("os%d" % i)) for i in range(2)])
                rr_r = Ring([(sb("rr%d" % i, [64, 128], F32), Res("rr%d" % i)) for i in range(2)])
                ob_r = Ring([(sb("ob%d" % i, [64, 128], BF16), Res("ob%d" % i)) for i in range(2)])
                i_r = Ring(PS[0:3])
                t_b = PS[3]
                s_r = Ring(PS[4:6])
                od_b = PS[6]
                pd_b = PS[7]
                mset("pool", Vx[:, :, :, 64:66], 1.0, (R_V,))
                dma(KI[:], KIT[:, :], (R_proj,), (R_KI,), q="sp")
                for i in range(2):
                    dma(KT[:, i, :], KTs[i, :, :], (R_proj,), (R_K,), q="sp")
                for h in range(4):
                    dma(Vx[:, :, h, 0:64], Vs[:, 64 * h:64 * h + 64].rearrange("(j p) e -> p j e", p=128),
                        (R_proj,), (R_V,), q="sp")
                dma(WIt[:], WI.rearrange("(j p) h -> p j h", p=128), (R_proj,), (R_WI,), q="sp")
                for qb in range(NB):
                    nk = 128 * (qb + 1)
                    nch = (nk + 511) // 512
                    qi, R_qi = qi_r.next()
                    qs, R_qs = qs_r.next()
                    dma(qi[:, 0:2, :], QIT[0:2, :, 128 * qb:128 * qb + 128].rearrange("i p t -> p i t"), (R_proj,), (R_qi,))
                    dma(qi[0:64, 2, :], QIT[2, 0:64, 128 * qb:128 * qb + 128], (R_proj,), (R_qi,))
                    dma(qs[:], QTs[:, :, 128 * qb:128 * qb + 128].rearrange("i p t -> p i t"), (R_proj,), (R_qs,))
                    for c in range(nch):
                        ncol = min(512, nk - 512 * c)
                        cs = slice(512 * c, 512 * c + ncol)
                        for hh in range(8):
                            po = 32 * (hh % 3)
                            bi = hh // 3
                            ps, R_ps = i_r.next()
                            mm(ps[:, 0:ncol], qi[po:po + 32, bi, :], KI[po:po + 32, cs], True, True,
                               (R_qi, R_KI), (R_ps,))
                            rl, R_rl = rl_r.next()
                            act(rl[:, 0:ncol], ps[:, 0:ncol], AF.Relu, (R_ps,), (R_rl,))
                            if hh == 0:
                                ts("dve", Isc[:, cs], rl[:, 0:ncol], WIt[:, qb, 0:1], None, ALU.mult, None,
                                   (R_rl, R_WI), (R_I,))
                            else:
                                stt(Isc[:, cs], rl[:, 0:ncol], WIt[:, qb, hh:hh + 1], Isc[:, cs], ALU.mult, ALU.add,
                                    (R_rl, R_WI, R_I), (R_I,))
                    dg = slice(128 * qb, 128 * qb + 128)
                    tt("dve", Isc[:, dg], Isc[:, dg], negtri[:], ALU.add, (R_I, R_const), (R_I,))
                    if nk > TOPK:
                        st, R_st = st_r.next()
                        RS = (R_st,)
                        SC.op("dve", lambda e, o=st[:, 0:1], i_=Isc[:, 0:nk - 128]: e.tensor_reduce(out=o, in_=i_, axis=AX.X, op=ALU.min),
                              (R_I,), RS)
                        SC.op("dve", lambda e, o=st[:, 1:2], i_=Isc[:, 0:nk]: e.tensor_reduce(out=o, in_=i_, axis=AX.X, op=ALU.max),
                              (R_I,), RS)
                        tt("dve", st[:, 1:2], st[:, 1:2], st[:, 0:1], ALU.subtract, RS, RS)
                        wk, R_wk = wk_r.next()
                        ts("dve", wk[:], pw2[:], st[:, 1:2], None, ALU.mult, None, (R_st, R_const), (R_wk,))
                        stt(st[:, 2:3], st[:, 1:2], 0.5, st[:, 0:1], ALU.mult, ALU.add, RS, RS)
                        for it in range(NBIS):
                            ts("dve", Msk[:, 0:nk], Isc[:, 0:nk], st[:, 2:3], 0.0, ALU.is_ge, ALU.add,
                               (R_I, R_st), (R_M, R_st), accum_out=st[:, 3:4])
                            ts("dve", st[:, 4:5], st[:, 3:4], TOPK - 0.5, 0.5, ALU.is_ge, ALU.subtract, RS, RS)
                            stt(st[:, 2:3], st[:, 4:5], wk[:, it:it + 1], st[:, 2:3], ALU.mult, ALU.add,
                                (R_st, R_wk), RS)
                        stt(st[:, 0:1], wk[:, NBIS:NBIS + 1], -1.0, st[:, 2:3], ALU.mult, ALU.add, (R_st, R_wk), RS)
                        ts("dve", Msk[:, 0:nk], Isc[:, 0:nk], st[:, 0:1], None, ALU.is_ge, None, (R_I, R_st), (R_M,))
                    else:
                        ts("dve", Msk[:, 0:nk], Isc[:, 0:nk], -1.0e29, None, ALU.is_ge, None, (R_I,), (R_M,))
                    tb_, R_tb_ = t_b
                    tbb = tb_[:, :].bitcast(BF16)
                    for j0 in range(0, qb + 1, 8):
                        n8 = min(8, qb + 1 - j0)
                        for jj in range(n8):
                            j = j0 + jj
                            tr(tbb[:, 128 * jj:128 * jj + 128], Msk[:, 128 * j:128 * j + 128], (R_M, R_const), (R_tb_,))
                        cp("act", MskT[:, j0:j0 + n8, :], tbb[:, 0:128 * n8].rearrange("p (j t) -> p j t", t=128),
                           (R_tb_,), (R_MT,))
                    for h in range(4):
                        rows = slice(64 * (h % 2), 64 * (h % 2) + 64)
                        bi = h // 2
                        od, R_od = od_b
                        for c in range(nch):
                            nbc = min(4, qb + 1 - 4 * c)
                            ps, R_ps = s_r.next()
                            for jj in range(nbc):
                                j = 4 * c + jj
                                mm(ps[:, 128 * jj:128 * jj + 128], KT[rows, bi, 128 * j:128 * j + 128], qs[rows, bi, :],
                                   True, True, (R_K, R_qs), (R_ps,))
                            pt, R_pt = pt_r.next()
                            act(pt[:, 0:128 * nbc], ps[:, 0:128 * nbc], AF.Exp, (R_ps,), (R_pt,), scale=scale)
                            tt("pool", pt[:, 0:128 * nbc], pt[:, 0:128 * nbc],
                               MskT[:, 4 * c:4 * c + nbc, :].rearrange("p j t -> p (j t)"), ALU.mult,
                               (R_pt, R_MT), (R_pt,))
                            for jj in range(nbc):
                                j = 4 * c + jj
                                mm(od[0:65, 0:128], Vx[:, j, h, 0:65], pt[:, 128 * jj:128 * jj + 128], j == 0, j == qb,
                                   (R_V, R_pt), (R_od,))
                        osb, R_os = os_r.next()
                        cp("act", osb[0:65, :], od[0:65, 0:128], (R_od,), (R_os,))
                        pd, R_pd = pd_b
                        mm(pd[0:64, 0:128], sel[0:65, :], osb[0:65, :], True, True, (R_const, R_os), (R_pd,))
                        rr, R_rr = rr_r.next()
                        SC.op("dve", lambda e, o=rr[:, :], i_=pd[0:64, 0:128]: e.reciprocal(out=o, in_=i_), (R_pd,), (R_rr,))
                        ob, R_ob = ob_r.next()
                        tt("dve", ob[:, :], osb[0:64, :], rr[:, :], ALU.mult, (R_os, R_rr), (R_ob,))
                        dma(mixT[768 + 64 * h:768 + 64 * h + 64, 128 * qb:128 * qb + 128], ob[:, :], (R_ob,), (R_mix,))
                SC.barrier()

        def norm_T(es_rings, xt, R_xt, hT, R_hT, b, bank):
            junk, R_junk, hb_r, st_r = es_rings
            hb, R_hb = hb_r.next()
            stt_, R_st_ = st_r.next()
            act(junk[:], xt[:], AF.Square, (R_xt,), (R_junk, R_st_), accum_out=stt_[:, 0:1])
            act(stt_[:, 1:2], stt_[:, 0:1], AF.Sqrt, (R_st_,), (R_st_,), scale=1.0 / D, bias=EPS)
            SC.op("dve", lambda e, o=stt_[:, 2:3], i=stt_[:, 1:2]: e.reciprocal(out=o, in_=i), (R_st_,), (R_st_,))
            ts("dve", hb[:], xt[:], stt_[:, 2:3], None, ALU.mult, None, (R_xt, R_st_), (R_hb,))
            pt_, R_pt = bank.next()
            ptb = pt_[:, :].bitcast(BF16)
            for k in range(8):
                tr(ptb[:, 128 * k:128 * k + 128], hb[:, 128 * k:128 * k + 128], (R_hb, R_const), (R_pt,))
            cp("act", hT[:, :, 128 * b:128 * b + 128], ptb.rearrange("p (k t) -> p k t", t=128), (R_pt,), (R_hT,))

        def phase_E1(l, xsrc, R_xs):
            uid[0] += 1
            with ExitStack() as es:
                def sb(name, shape, dt):
                    return es.enter_context(nc.sbuf_tensor(name + "_E1_%d" % uid[0], shape, dt))
                Wo = sb("Wo", [128, 8, D], BF16)
                Wup = sb("Wup", [128, 8, 2 * DFF], BF16)
                R_W = Res("W_E1")
                stg = Ring([(sb("stg%d" % i, [128, 2048], F32), Res("stg%d" % i)) for i in range(2)])
                for k in range(8):
                    st, R_st = stg.next()
                    dma(st[:, 0:1024], w_o[l, 128 * k:128 * k + 128, :], (), (R_st,))
                    cp("act", Wo[:, k, :], st[:, 0:1024], (R_st,), (R_W,))
                    for (c0, cn) in ((0, 2048), (2048, 2048), (4096, 1536)):
                        st, R_st = stg.next()
                        dma(st[:, 0:cn], w_up[l, 128 * k:128 * k + 128, c0:c0 + cn], (), (R_st,))
                        ts("dve", Wup[:, k, c0:c0 + cn], st[:, 0:cn], gF[:, l * 8 + k:l * 8 + k + 1], None, ALU.mult, None,
                           (R_st, R_const), (R_W,))
                halo = sb("halo", [128, 44, 2], F32)
                R_halo = Res("halo")
                mset("pool", halo[:], 0.0, (R_halo,))
                mT_r = Ring([(sb("mT%d" % i, [128, 8, 512], BF16), Res("mT%d" % i)) for i in range(2)])
                hT_r = Ring([(sb("hT%d" % i, [128, 8, 512], BF16), Res("hT%d" % i)) for i in range(2)])
                xt_r = Ring([(sb("xt%d" % i, [128, D], F32), Res("xt%d" % i)) for i in range(2)])
                x1_r = Ring([(sb("x1%d" % i, [128, D], F32), Res("x1%d" % i)) for i in range(2)])
                junk = sb("junk", [128, D], BF16)
                R_junk = Res("junk")
                hb_r = Ring([(sb("hb%d" % i, [128, D], BF16), Res("hb%d" % i)) for i in range(2)])
                st_r = Ring([(sb("st%d" % i, [128, 4], F32), Res("st%d" % i)) for i in range(2)])
                ub_r = Ring([(sb("ub%d" % i, [128, 514], F32), Res("ub%d" % i)) for i in range(3)])
                y_r = Ring([(sb("y%d" % i, [128, 512], F32), Res("y%d" % i)) for i in range(4)])
                sg_r = Ring([(sb("sg%d" % i, [128, 512], F32), Res("sg%d" % i)) for i in range(2)])
                ab_r = Ring([(sb("ab%d" % i, [128, 512], BF16), Res("ab%d" % i)) for i in range(3)])
                bank = Ring(PS)
                rings = (junk, R_junk, hb_r, st_r)
                for ti in range(NT):
                    t0 = 512 * ti
                    mT, R_mT = mT_r.next()
                    hT, R_hT = hT_r.next()
                    dma(mT[:], mixT[:, t0:t0 + 512].rearrange("(k p) t -> p k t", p=128), (R_mix,), (R_mT,), q="sp")
                    for b in range(4):
                        r0 = t0 + 128 * b
                        xt, R_xt = xt_r.next()
                        x1, R_x1t = x1_r.next()
                        dma(xt[:], xsrc[r0:r0 + 128, :], (R_xs,), (R_xt,))
                        for hf in range(2):
                            ps, R_ps = bank.next()
                            for k in range(8):
                                mm(ps[:, :], mT[:, k, 128 * b:128 * b + 128], Wo[:, k, 512 * hf:512 * hf + 512],
                                   k == 0, k == 7, (R_mT, R_W), (R_ps,))
                            tt("dve", x1[:, 512 * hf:512 * hf + 512], ps[:, :], xt[:, 512 * hf:512 * hf + 512], ALU.add,
                               (R_ps, R_xt), (R_x1t,))
                        dma(xres1[r0:r0 + 128, :], x1[:], (R_x1t,), (R_x1,))
                        norm_T(rings, x1, R_x1t, hT, R_hT, b, bank)
                    for f in range(22):
                        ys = []
                        for fc in (f, 22 + f):
                            ps, R_ps = bank.next()
                            for k in range(8):
                                mm(ps[:, :], Wup[:, k, 128 * fc:128 * fc + 128], hT[:, k, :], k == 0, k == 7,
                                   (R_W, R_hT), (R_ps,))
                            ub, R_ub = ub_r.next()
                            cp("dve", ub[:, 0:2], halo[:, fc, :], (R_halo,), (R_ub,))
                            cp("act", ub[:, 2:514], ps[:, :], (R_ps,), (R_ub,))
                            cp("dve", halo[:, fc, :], ub[:, 512:514], (R_ub,), (R_halo,))
                            y, R_y_ = y_r.next()
                            ci = l * 132 + fc
                            ts("dve", y[:], ps[:, :], cw[:, ci + 88:ci + 89], cb[:, l * 44 + fc:l * 44 + fc + 1],
                               ALU.mult, ALU.add, (R_ps, R_const), (R_y_,))
                            stt(y[:], ub[:, 1:513], cw[:, ci + 44:ci + 45], y[:], ALU.mult, ALU.add, (R_ub, R_const, R_y_), (R_y_,))
                            stt(y[:], ub[:, 0:512], cw[:, ci:ci + 1], y[:], ALU.mult, ALU.add, (R_ub, R_const, R_y_), (R_y_,))
                            ys.append((y, R_y_))
                        sg, R_sg = sg_r.next()
                        act(sg[:], ys[0][0][:], AF.Silu, (ys[0][1],), (R_sg,))
                        ab, R_ab = ab_r.next()
                        tt("dve", ab[:], sg[:], ys[1][0][:], ALU.mult, (R_sg, ys[1][1]), (R_ab,))
                        dma(actT[128 * f:128 * f + 128, t0:t0 + 512], ab[:], (R_ab,), (R_act,))
                SC.barrier()

        def phase_E2(l, last):
            uid[0] += 1
            with ExitStack() as es:
                def sb(name, shape, dt):
                    return es.enter_context(nc.sbuf_tensor(name + "_E2_%d" % uid[0], shape, dt))
                Wdn = sb("Wdn", [128, 22, D], BF16)
                R_W = Res("W_E2")
                stg = Ring([(sb("stg%d" % i, [128, D], F32), Res("stg%d" % i)) for i in range(2)])
                for f in range(22):
                    st, R_st = stg.next()
                    dma(st[:], w_down[l, 128 * f:128 * f + 128, :], (), (R_st,))
                    cp("act" if f % 2 else "dve", Wdn[:, f, :], st[:], (R_st,), (R_W,))
                gfin = sb("gfin", [128, D], F32)
                R_gf = Res("gfin")
                if last:
                    dma(gfin[:], gfin_d[:, :], (), (R_gf,))
                aT_r = Ring([(sb("aT%d" % i, [128, 22, 512], BF16), Res("aT%d" % i)) for i in range(2)])
                xt_r = Ring([(sb("xt%d" % i, [128, D], F32), Res("xt%d" % i)) for i in range(2)])
                x2_r = Ring([(sb("x2%d" % i, [128, D], F32), Res("x2%d" % i)) for i in range(2)])
                yo_r = Ring([(sb("yo%d" % i, [128, D], F32), Res("yo%d" % i)) for i in range(2)])
                junk = sb("junk", [128, D], BF16)
                R_junk = Res("junk")
                st_r = Ring([(sb("st%d" % i, [128, 4], F32), Res("st%d" % i)) for i in range(2)])
                bank = Ring(PS)
                for ti in range(NT):
                    t0 = 512 * ti
                    aT, R_aT = aT_r.next()
                    dma(aT[:], actT[:, t0:t0 + 512].rearrange("(f p) t -> p f t", p=128), (R_act,), (R_aT,), q="sp")
                    for b in range(4):
                        r0 = t0 + 128 * b
                        xt, R_xt = xt_r.next()
                        x2, R_x2 = x2_r.next()
                        dma(xt[:], xres1[r0:r0 + 128, :], (R_x1,), (R_xt,))
                        for hf in range(2):
                            ps, R_ps = bank.next()
                            for f in range(22):
                                mm(ps[:, :], aT[:, f, 128 * b:128 * b + 128], Wdn[:, f, 512 * hf:512 * hf + 512],
                                   f == 0, f == 21, (R_aT, R_W), (R_ps,))
                            tt("dve", x2[:, 512 * hf:512 * hf + 512], ps[:, :], xt[:, 512 * hf:512 * hf + 512], ALU.add,
                               (R_ps, R_xt), (R_x2,))
                        if not last:
                            dma(xres0[r0:r0 + 128, :], x2[:], (R_x2,), (R_x,))
                        else:
                            stt_, R_st_ = st_r.next()
                            yo, R_yo = yo_r.next()
                            act(junk[:], x2[:], AF.Square, (R_x2,), (R_junk, R_st_), accum_out=stt_[:, 0:1])
                            act(stt_[:, 1:2], stt_[:, 0:1], AF.Sqrt, (R_st_,), (R_st_,), scale=1.0 / D, bias=EPS)
                            SC.op("dve", lambda e, o=stt_[:, 2:3], i=stt_[:, 1:2]: e.reciprocal(out=o, in_=i), (R_st_,), (R_st_,))
                            stt(yo[:], x2[:], stt_[:, 2:3], gfin[:], ALU.mult, ALU.mult, (R_x2, R_st_, R_gf), (R_yo,))
                            dma(y_out[r0:r0 + 128, :], yo[:], (R_yo,), (R_y,))
                SC.barrier()

        for l in range(L):
            xsrc = x_in if l == 0 else xres0
            phase_A(l, xsrc)
            if stop_after == "A":
                break
            if "B" not in skip:
                phase_B()
            if stop_after == "B":
                break
            if "C" not in skip:
                phase_C()
            if stop_after == "C":
                break
            if "D" not in skip:
                phase_D()
            if stop_after == "D":
                break
            phase_E1(l, xsrc, R_x)
            if stop_after == "E1":
                break
            phase_E2(l, l == L - 1)
        SC.barrier()
        print("instructions:", SC.ninst)
    return nc


def make_consts(S):
    bf = ml_dtypes.bfloat16
    c = {}
    c["c_ident"] = np.eye(128, dtype=np.float32).astype(bf)
    c["c_ones"] = np.ones((128, 128), np.float32).astype(bf)
    sel = np.zeros((128, 64), np.float32)
    sel[64, :] = 1.0
    c["c_sel"] = sel
    k = np.arange(128)[:, None]
    q = np.arange(128)[None, :]
    c["c_band"] = np.concatenate([(k <= q), (k >= q)], axis=1).astype(np.float32).astype(bf)
    c["c_negtri"] = np.where(q.T >= k.T, 0.0, NEG).astype(np.float32) if False else \
        np.where(np.arange(128)[None, :] <= np.arange(128)[:, None], 0.0, NEG).astype(np.float32)
    c["c_pw2"] = np.ascontiguousarray(np.broadcast_to(
        (2.0 ** -(np.arange(NBIS + 1, dtype=np.float32) + 1.0)).astype(np.float32)[None, :], (128, NBIS + 1)))
    t = np.arange(S, dtype=np.float32)

    def tables(dim):
        inv = np.power(np.float32(500000.0), -np.arange(0, dim, 2, dtype=np.float32) / np.float32(dim)).astype(np.float32)
        ang = (t[:, None] * inv[None, :]).astype(np.float32)
        return np.cos(ang).astype(np.float32).T, np.sin(ang).astype(np.float32).T
    ch, sh = tables(16)
    ci, si = tables(8)
    ca, sa = tables(32)
    C = np.ones((128, S), np.float32)
    Sn = np.zeros((128, S), np.float32)
    for hh in range(2):
        C[64 * hh:64 * hh + 8] = ch
        C[64 * hh + 8:64 * hh + 16] = ch
        Sn[64 * hh:64 * hh + 8] = sh
        Sn[64 * hh + 8:64 * hh + 16] = sh
    c["tab64"] = np.stack([C, Sn])
    C = np.ones((128, S), np.float32)
    Sn = np.zeros((128, S), np.float32)
    for hh in range(4):
        C[32 * hh:32 * hh + 4] = ci
        C[32 * hh + 4:32 * hh + 8] = ci
        Sn[32 * hh:32 * hh + 4] = si
        Sn[32 * hh + 4:32 * hh + 8] = si
    c["tabidx"] = np.stack([C, Sn])
    C = np.ones((96, S), np.float32)
    Sn = np.zeros((96, S), np.float32)
    C[64:80] = ca
    C[80:96] = ca
    Sn[64:80] = sa
    Sn[80:96] = sa
    c["tabmla"] = np.stack([C, Sn])
    return c


def layout_params(inp, L):
    f = np.float32
    o = {}
    o["gA"] = np.ascontiguousarray(np.asarray(inp["g_attn"], f).reshape(L, 8, 128).transpose(2, 0, 1).reshape(128, L * 8))
    o["gF"] = np.ascontiguousarray(np.asarray(inp["g_ffn"], f).reshape(L, 8, 128).transpose(2, 0, 1).reshape(128, L * 8))
    o["gq"] = np.ascontiguousarray(np.asarray(inp["g_q_lat"], f).reshape(L, 2, 128).transpose(2, 0, 1).reshape(128, L * 2))
    o["gkv"] = np.ascontiguousarray(np.asarray(inp["g_kv_lat"], f).reshape(L, 128).T)
    o["cw"] = np.ascontiguousarray(np.asarray(inp["conv_w"], f).reshape(L, 3, 44, 128).transpose(3, 0, 1, 2).reshape(128, L * 3 * 44))
    o["cb"] = np.ascontiguousarray(np.asarray(inp["conv_b"], f).reshape(L, 44, 128).transpose(2, 0, 1).reshape(128, L * 44))
    o["gfin"] = np.ascontiguousarray(np.broadcast_to(np.asarray(inp["g_final"], f).reshape(1, D), (128, D)))
    for k in ("w_in", "w_uq", "w_ukv", "w_o", "w_up", "w_down"):
        o[k] = np.ascontiguousarray(np.asarray(inp[k], f))
    return o


_CACHE = {}


def kernel(**inputs):
    x = np.asarray(inputs["x"], np.float32)
    B, S, _ = x.shape
    L = inputs["w_in"].shape[0]
    key = (S, L)
    if key not in _CACHE:
        _CACHE[key] = (build(S, L), make_consts(S))
    nc, consts = _CACHE[key]
    shared = layout_params(inputs, L)
    shared.update(consts)
    n = 8
    in_maps = []
    for c in range(n):
        m = dict(shared)
        m["x"] = np.ascontiguousarray(x[c % B])
        in_maps.append(m)
    res = run_bass_kernel_spmd(nc, in_maps, core_ids=list(range(n)))
    return np.stack([res.results[b]["y"] for b in range(B)], axis=0).astype(np.float32)
```

```python
import numpy as np
import ml_dtypes
from contextlib import ExitStack
import concourse.bass as bass
import concourse.mybir as mybir
from concourse.bass_utils import run_bass_kernel_spmd

F32 = mybir.dt.float32
BF16 = mybir.dt.bfloat16
AF = mybir.ActivationFunctionType
ALU = mybir.AluOpType
AX = mybir.AxisListType

EPS = 1e-6
NEG = -1.0e30
D = 1024
NIN = 3016
DFF = 2816
TOPK = 256
NBIS = 17


class Res:
    __slots__ = ("name", "w", "r", "ordered")

    def __init__(self, name, ordered=True):
        self.name = name
        self.w = {}
        self.r = {}
        self.ordered = ordered


class Eng:
    def __init__(self, name, eng, sem):
        self.name = name
        self.eng = eng
        self.sem = sem
        self.n = 0
        self.waited = {}
        self.dsem = []
        self.dval = []
        self.di = 0


class Sched:
    K = 12

    def __init__(self, nc, es):
        self.nc = nc
        self.E = {}
        for name, e in (("pe", nc.tensor), ("act", nc.scalar), ("dve", nc.vector),
                        ("pool", nc.gpsimd), ("sp", nc.sync)):
            self.E[name] = Eng(name, e, es.enter_context(nc.semaphore("p_" + name)))
        for q in ("sp", "pool", "act"):
            E = self.E[q]
            for i in range(self.K):
                E.dsem.append(es.enter_context(nc.semaphore("d_%s%d" % (q, i))))
                E.dval.append(0)
        self.ninst = 0

    def _need(self, reads, writes):
        need = {}

        def add(d):
            for k, sv in d.items():
                if k not in need or need[k][1] < sv[1]:
                    need[k] = sv
        for r in reads:
            add(r.w)
        for w in writes:
            add(w.r)
            if w.ordered:
                add(w.w)
        return need

    def _emit_waits(self, E, need, is_dma):
        for k, (s, v) in need.items():
            if s is E.sem and not is_dma and E.name == "pe":
                continue
            if E.waited.get(k, 0) >= v:
                continue
            E.eng.wait_ge(s, v)
            E.waited[k] = v

    def _mark(self, tok, reads, writes):
        k = id(tok[0])
        for r in reads:
            if r.r.get(k, (None, 0))[1] < tok[1]:
                r.r[k] = tok
        for w in writes:
            if w.ordered:
                w.w = {k: tok}
                w.r = {}
            else:
                w.w[k] = tok

    def op(self, eng, fn, reads=(), writes=()):
        E = self.E[eng]
        self._emit_waits(E, self._need(reads, writes), False)
        inst = fn(E.eng)
        E.n += 1
        inst.then_inc(E.sem, 1)
        self._mark((E.sem, E.n), reads, writes)
        self.ninst += 1

    def dma(self, q, out, in_, reads=(), writes=()):
        E = self.E[q]
        self._emit_waits(E, self._need(reads, writes), True)
        i = E.di % self.K
        E.di += 1
        s = E.dsem[i]
        pv = E.dval[i]
        if pv > 0 and E.waited.get(id(s), 0) < pv:
            E.eng.wait_ge(s, pv)
            E.waited[id(s)] = pv
        E.eng.dma_start(out=out, in_=in_).then_inc(s, 16)
        E.dval[i] = pv + 16
        self._mark((s, pv + 16), reads, writes)
        self.ninst += 1

    def barrier(self):
        for E in self.E.values():
            for O in self.E.values():
                if O is not E and O.n > 0 and E.waited.get(id(O.sem), 0) < O.n:
                    E.eng.wait_ge(O.sem, O.n)
                    E.waited[id(O.sem)] = O.n
                for s, v in zip(O.dsem, O.dval):
                    if v > 0 and E.waited.get(id(s), 0) < v:
                        E.eng.wait_ge(s, v)
                        E.waited[id(s)] = v


class Ring:
    def __init__(self, items):
        self.items = items
        self.i = 0

    def next(self):
        it = self.items[self.i % len(self.items)]
        self.i += 1
        return it


def build(S=8192, L=4, dbg=False, stop_after=None, skip=()):
    NT = S // 512
    NB = S // 128
    nc = bass.Bass("TRN2", target_bir_lowering=False)

    def din(name, shape, dt=F32):
        return nc.dram_tensor(name, list(shape), dt, kind="ExternalInput").ap()

    def dscr(name, shape, dt):
        return nc.dram_tensor(name, list(shape), dt, kind=("ExternalOutput" if dbg else "Internal")).ap()

    x_in = din("x", [S, D])
    w_in = din("w_in", [L, D, NIN])
    w_uq = din("w_uq", [L, 256, 384])
    w_ukv = din("w_ukv", [L, 128, 512])
    w_o = din("w_o", [L, D, D])
    w_up = din("w_up", [L, D, 2 * DFF])
    w_down = din("w_down", [L, DFF, D])
    gA_d = din("gA", [128, L * 8])
    gF_d = din("gF", [128, L * 8])
    gq_d = din("gq", [128, L * 2])
    gkv_d = din("gkv", [128, L])
    cw_d = din("cw", [128, L * 3 * 44])
    cb_d = din("cb", [128, L * 44])
    gfin_d = din("gfin", [128, D])
    c_ident = din("c_ident", [128, 128], BF16)
    c_ones = din("c_ones", [128, 128], BF16)
    c_sel = din("c_sel", [128, 64])
    c_band = din("c_band", [128, 256], BF16)
    c_negtri = din("c_negtri", [128, 128])
    c_pw2 = din("c_pw2", [128, NBIS + 1])
    tab64 = din("tab64", [2, 128, S])
    tabidx = din("tabidx", [2, 128, S])
    tabmla = din("tabmla", [2, 96, S])
    y_out = nc.dram_tensor("y", [S, D], F32, kind="ExternalOutput").ap()

    xres0 = dscr("xres0", [S, D], F32)
    xres1 = dscr("xres1", [S, D], F32)
    QTm = dscr("QTm", [4, 96, S], BF16)
    KTm = dscr("KTm", [4, 96, S], BF16)
    Vm = dscr("Vm", [S, 256], BF16)
    QTd = dscr("QTd", [4, 128, S], BF16)
    KTd = dscr("KTd", [4, 128, S], BF16)
    Vd = dscr("Vd", [S, 512], BF16)
    QTs = dscr("QTs", [2, 128, S], BF16)
    KTs = dscr("KTs", [2, 128, S], BF16)
    Vs = dscr("Vs", [S, 256], BF16)
    QIT = dscr("QIT", [3, 96, S], BF16)
    KIT = dscr("KIT", [128, S], BF16)
    WI = dscr("WI", [S, 8], F32)
    mixT = dscr("mixT", [D, S], BF16)
    actT = dscr("actT", [DFF, S], BF16)

    ges = ExitStack()
    with ges:
        SC = Sched(nc, ges)
        R_x = Res("xres0", ordered=False)
        R_x1 = Res("xres1", ordered=False)
        R_proj = Res("proj", ordered=False)
        R_mix = Res("mixT", ordered=False)
        R_act = Res("actT", ordered=False)
        R_y = Res("y", ordered=False)
        R_const = Res("const")

        def gsb(name, shape, dt):
            return ges.enter_context(nc.sbuf_tensor("g_" + name, shape, dt))

        PS = []
        for i in range(8):
            t = ges.enter_context(nc.psum_tensor("ps%d" % i, [128, 512], F32))
            PS.append((t, Res("ps%d" % i)))

        gA = gsb("gA", [128, L * 8], F32)
        gF = gsb("gF", [128, L * 8], F32)
        gq = gsb("gq", [128, L * 2], F32)
        gkv = gsb("gkv", [128, L], F32)
        cw = gsb("cw", [128, L * 3 * 44], F32)
        cb = gsb("cb", [128, L * 44], F32)
        ident = gsb("ident", [128, 128], BF16)
        ones = gsb("ones", [128, 128], BF16)
        sel = gsb("sel", [128, 64], F32)
        band = gsb("band", [128, 256], BF16)
        negtri = gsb("negtri", [128, 128], F32)
        pw2 = gsb("pw2", [128, NBIS + 1], F32)
        for dst, src in ((gA, gA_d), (gF, gF_d), (gq, gq_d), (gkv, gkv_d), (cw, cw_d), (cb, cb_d),
                         (ident, c_ident), (ones, c_ones), (sel, c_sel), (band, c_band), (negtri, c_negtri), (pw2, c_pw2)):
            SC.dma("sp", dst[:], src[:, :], reads=(), writes=(R_const,))
        R_const.ordered = False

        def mm(out, lhsT, rhs, start, stop, reads, writes):
            SC.op("pe", lambda e: e.matmul(out, lhsT=lhsT, rhs=rhs, start=start, stop=stop), reads, writes)

        def tr(out, in_, reads, writes):
            SC.op("pe", lambda e: e.transpose(out, in_, ident[:]), reads, writes)

        def act(out, in_, func, reads, writes, scale=1.0, bias=None, accum_out=None):
            kw = {}
            if bias is not None:
                kw["bias"] = bias
            if accum_out is not None:
                kw["accum_out"] = accum_out
            SC.op("act", lambda e: e.activation(out=out, in_=in_, func=func, scale=scale, **kw), reads, writes)

        def ts(eng, out, in0, s1, s2, op0, op1, reads, writes, accum_out=None):
            kw = {}
            if accum_out is not None:
                kw["accum_out"] = accum_out
            if op1 is None:
                SC.op(eng, lambda e: e.tensor_scalar(out=out, in0=in0, scalar1=s1, scalar2=None, op0=op0, **kw),
                      reads, writes)
            else:
                SC.op(eng, lambda e: e.tensor_scalar(out=out, in0=in0, scalar1=s1, scalar2=s2, op0=op0, op1=op1, **kw),
                      reads, writes)

        def tt(eng, out, in0, in1, op, reads, writes):
            SC.op(eng, lambda e: e.tensor_tensor(out=out, in0=in0, in1=in1, op=op), reads, writes)

        def stt(out, in0, scalar, in1, op0, op1, reads, writes):
            SC.op("dve", lambda e: e.scalar_tensor_tensor(out=out, in0=in0, scalar=scalar, in1=in1, op0=op0, op1=op1),
                  reads, writes)

        def cp(eng, out, in_, reads, writes):
            if eng == "act":
                SC.op("act", lambda e: e.activation(out=out, in_=in_, func=AF.Copy), reads, writes)
            else:
                SC.op(eng, lambda e: e.tensor_copy(out=out, in_=in_), reads, writes)

        def mset(eng, ap, val, writes):
            SC.op(eng, lambda e: e.memset(ap, val), (), writes)

        uid = [0]
        dq = ["sp", "pool"]
        dqi = [0]

        def dma(out, in_, reads, writes, q=None):
            if q is None:
                q = dq[dqi[0] % 2]
                dqi[0] += 1
            SC.dma(q, out, in_, reads, writes)

        def rms_rows(es, xt, R_xt, hb, R_hb, name):
            pass

        def phase_A(l, xsrc):
            uid[0] += 1
            with ExitStack() as es:
                def sb(name, shape, dt):
                    return es.enter_context(nc.sbuf_tensor(name + "_A_%d" % uid[0], shape, dt))
                Win = sb("Win", [128, 8, NIN], BF16)
                WinR = sb("WinR", [128, 8, 1920], BF16)
                Wki = sb("Wki", [128, 8, 128], BF16)
                Wkp = sb("Wkp", [128, 8, 96], BF16)
                WkpR = sb("WkpR", [128, 8, 96], BF16)
                Wuq = sb("Wuq", [128, 2, 384], BF16)
                WuqR = sb("WuqR", [128, 2, 384], BF16)
                Wukv = sb("Wukv", [128, 512], BF16)
                Wkk = sb("Wkk", [128, 4, 96], BF16)
                Wv = sb("Wv", [128, 256], BF16)
                R_W = Res("W_A")
                stg = Ring([(sb("stg%d" % i, [128, 1508], F32), Res("stg%d" % i)) for i in range(2)])
                for k in range(8):
                    for hf in range(2):
                        st, R_st = stg.next()
                        dma(st[:], w_in[l, 128 * k:128 * k + 128, 1508 * hf:1508 * hf + 1508], (), (R_st,))
                        ts("dve", Win[:, k, 1508 * hf:1508 * hf + 1508], st[:], gA[:, l * 8 + k:l * 8 + k + 1], None,
                           ALU.mult, None, (R_st, R_const), (R_W,))
                for c in range(2):
                    st, R_st = stg.next()
                    dma(st[:, 0:384], w_uq[l, 128 * c:128 * c + 128, :], (), (R_st,))
                    ts("dve", Wuq[:, c, :], st[:, 0:384], gq[:, l * 2 + c:l * 2 + c + 1], None, ALU.mult, None,
                       (R_st, R_const), (R_W,))
                st, R_st = stg.next()
                dma(st[:, 0:512], w_ukv[l, :, :], (), (R_st,))
                ts("dve", Wukv[:], st[:, 0:512], gkv[:, l:l + 1], None, ALU.mult, None, (R_st, R_const), (R_W,))
                mset("pool", WinR[:], 0.0, (R_W,))
                mset("pool", Wkp[:], 0.0, (R_W,))
                mset("pool", WkpR[:], 0.0, (R_W,))
                mset("pool", WuqR[:], 0.0, (R_W,))
                mset("pool", Wkk[:], 0.0, (R_W,))
                RW = (R_W,)
                for k in range(8):
                    for base, nh, rb in ((416, 8, 0), (928, 8, 512), (1952, 4, 1024), (2208, 4, 1280)):
                        src = Win[:, k, base:base + nh * 64].rearrange("p (h e) -> p h e", e=64)
                        dst = WinR[:, k, rb:rb + nh * 64].rearrange("p (h e) -> p h e", e=64)
                        ts("dve", dst[:, :, 0:8], src[:, :, 8:16], -1.0, None, ALU.mult, None, RW, RW)
                        cp("dve", dst[:, :, 8:16], src[:, :, 0:8], RW, RW)
                    src = Win[:, k, 2720:2976].rearrange("p (h e) -> p h e", e=32)
                    dst = WinR[:, k, 1536:1792].rearrange("p (h e) -> p h e", e=32)
                    ts("dve", dst[:, :, 0:4], src[:, :, 4:8], -1.0, None, ALU.mult, None, RW, RW)
                    cp("dve", dst[:, :, 4:8], src[:, :, 0:4], RW, RW)
                    for r in range(4):
                        cp("dve", Wki[:, k, 32 * r:32 * r + 32], Win[:, k, 2976:3008], RW, RW)
                        ts("dve", WinR[:, k, 1792 + 32 * r:1792 + 32 * r + 4], Win[:, k, 2980:2984], -1.0, None,
                           ALU.mult, None, RW, RW)
                        cp("dve", WinR[:, k, 1792 + 32 * r + 4:1792 + 32 * r + 8], Win[:, k, 2976:2980], RW, RW)
                    cp("dve", Wkp[:, k, 64:96], Win[:, k, 384:416], RW, RW)
                    ts("dve", WkpR[:, k, 64:80], Win[:, k, 400:416], -1.0, None, ALU.mult, None, RW, RW)
                    cp("dve", WkpR[:, k, 80:96], Win[:, k, 384:400], RW, RW)
                for c in range(2):
                    src = Wuq[:, c, :].rearrange("p (h e) -> p h e", e=96)
                    dst = WuqR[:, c, :].rearrange("p (h e) -> p h e", e=96)
                    ts("dve", dst[:, :, 64:80], src[:, :, 80:96], -1.0, None, ALU.mult, None, RW, RW)
                    cp("dve", dst[:, :, 80:96], src[:, :, 64:80], RW, RW)
                ukv = Wukv[:].rearrange("p (h t e) -> p h t e", t=2, e=64)
                cp("dve", Wkk[:, :, 0:64], ukv[:, :, 0, :], RW, RW)
                cp("dve", Wv[:].rearrange("p (h e) -> p h e", e=64), ukv[:, :, 1, :], RW, RW)

                xt_r = Ring([(sb("xt%d" % i, [128, D], F32), Res("xt%d" % i)) for i in range(2)])
                junk = sb("junk", [128, D], BF16)
                R_junk = Res("junk")
                hb_r = Ring([(sb("hb%d" % i, [128, D], BF16), Res("hb%d" % i)) for i in range(2)])
                st_r = Ring([(sb("st%d" % i, [128, 4], F32), Res("st%d" % i)) for i in range(2)])
                hT_r = Ring([(sb("hT%d" % i, [128, 8, 512], BF16), Res("hT%d" % i)) for i in range(2)])
                tb_r = Ring([(sb("tb%d" % i, [128, 6, 512], F32), Res("tb%d" % i)) for i in range(2)])
                t1_r = Ring([(sb("t1_%d" % i, [128, 512], F32), Res("t1_%d" % i)) for i in range(2)])
                t2_r = Ring([(sb("t2_%d" % i, [128, 512], F32), Res("t2_%d" % i)) for i in range(2)])
                ob_r = Ring([(sb("ob%d" % i, [128, 512], BF16), Res("ob%d" % i)) for i in range(4)])
                sq_r = Ring([(sb("sq%d" % i, [128, 512], BF16), Res("sq%d" % i)) for i in range(2)])
                rs_r = Ring([(sb("rs%d" % i, [128, 512], F32), Res("rs%d" % i)) for i in range(2)])
                cqn = sb("cqn", [128, 2, 512], BF16)
                R_cqn = Res("cqn")
                ckvn = sb("ckvn", [128, 512], BF16)
                R_ckvn = Res("ckvn")
                vo_r = Ring([(sb("vo%d" % i, [128, 1024], BF16), Res("vo%d" % i)) for i in range(2)])
                wo_r = Ring([(sb("wo%d" % i, [128, 8], F32), Res("wo%d" % i)) for i in range(2)])
                bank = Ring(PS)

                def rope_out(rows, pz, R_pz, pr, R_pr, tC, tS, R_tb, dst_ap):
                    t1, R_t1 = t1_r.next()
                    t2, R_t2 = t2_r.next()
                    ob, R_ob = ob_r.next()
                    tt("dve", t1[0:rows, :], pz[0:rows, :], tC, ALU.mult, (R_pz, R_tb), (R_t1,))
                    tt("dve", t2[0:rows, :], pr[0:rows, :], tS, ALU.mult, (R_pr, R_tb), (R_t2,))
                    tt("pool", ob[0:rows, :], t1[0:rows, :], t2[0:rows, :], ALU.add, (R_t1, R_t2), (R_ob,))
                    dma(dst_ap, ob[0:rows, :], (R_ob,), (R_proj,))

                for ti in range(NT):
                    t0 = 512 * ti
                    hT, R_hT = hT_r.next()
                    for b in range(4):
                        xt, R_xt = xt_r.next()
                        hb, R_hb = hb_r.next()
                        stt_, R_st_ = st_r.next()
                        dma(xt[:], xsrc[t0 + 128 * b:t0 + 128 * b + 128, :], (R_x,), (R_xt,))
                        act(junk[:], xt[:], AF.Square, (R_xt,), (R_junk, R_st_), accum_out=stt_[:, 0:1])
                        act(stt_[:, 1:2], stt_[:, 0:1], AF.Sqrt, (R_st_,), (R_st_,), scale=1.0 / D, bias=EPS)
                        SC.op("dve", lambda e, o=stt_[:, 2:3], i=stt_[:, 1:2]: e.reciprocal(out=o, in_=i), (R_st_,), (R_st_,))
                        ts("dve", hb[:], xt[:], stt_[:, 2:3], None, ALU.mult, None, (R_xt, R_st_), (R_hb,))
                        pt_, R_pt = bank.next()
                        ptb = pt_[:, :].bitcast(BF16)
                        for k in range(8):
                            tr(ptb[:, 128 * k:128 * k + 128], hb[:, 128 * k:128 * k + 128], (R_hb, R_const), (R_pt,))
                        cp("act", hT[:, :, 128 * b:128 * b + 128], ptb.rearrange("p (k t) -> p k t", t=128),
                           (R_pt,), (R_hT,))
                    tb, R_tb = tb_r.next()
                    dma(tb[:, 0, :], tab64[0, :, t0:t0 + 512], (), (R_tb,))
                    dma(tb[:, 1, :], tab64[1, :, t0:t0 + 512], (), (R_tb,))
                    dma(tb[:, 2, :], tabidx[0, :, t0:t0 + 512], (), (R_tb,))
                    dma(tb[:, 3, :], tabidx[1, :, t0:t0 + 512], (), (R_tb,))
                    dma(tb[0:96, 4, :], tabmla[0, :, t0:t0 + 512], (), (R_tb,))
                    dma(tb[0:96, 5, :], tabmla[1, :, t0:t0 + 512], (), (R_tb,))
                    blocks = []
                    for i in range(4):
                        blocks.append((Win, 416 + 128 * i, WinR, 128 * i, 0, QTd[i, :, t0:t0 + 512], 128))
                    for i in range(4):
                        blocks.append((Win, 928 + 128 * i, WinR, 512 + 128 * i, 0, KTd[i, :, t0:t0 + 512], 128))
                    for i in range(2):
                        blocks.append((Win, 1952 + 128 * i, WinR, 1024 + 128 * i, 0, QTs[i, :, t0:t0 + 512], 128))
                    for i in range(2):
                        blocks.append((Win, 2208 + 128 * i, WinR, 1280 + 128 * i, 0, KTs[i, :, t0:t0 + 512], 128))
                    for i in range(3):
                        nr = 96 if i < 2 else 64
                        blocks.append((Win, 2720 + 96 * i, WinR, 1536 + 96 * i, 2, QIT[i, 0:nr, t0:t0 + 512], nr))
                    blocks.append((Wki, 0, WinR, 1792, 2, KIT[:, t0:t0 + 512], 128))
                    for (Wz, cz, Wr, cr, tbi, dst, nr) in blocks:
                        pz, R_pz = bank.next()
                        pr, R_pr = bank.next()
                        for k in range(8):
                            mm(pz[0:nr, :], Wz[:, k, cz:cz + nr], hT[:, k, :], k == 0, k == 7, (R_W, R_hT), (R_pz,))
                        for k in range(8):
                            mm(pr[0:nr, :], Wr[:, k, cr:cr + nr], hT[:, k, :], k == 0, k == 7, (R_W, R_hT), (R_pr,))
                        rope_out(nr, pz, R_pz, pr, R_pr, tb[0:nr, tbi, :], tb[0:nr, tbi + 1, :], R_tb, dst)
                    pq0, R_pq0 = bank.next()
                    pq1, R_pq1 = bank.next()
                    pkv, R_pkv = bank.next()
                    for (pp, R_pp, c0) in ((pq0, R_pq0, 0), (pq1, R_pq1, 128), (pkv, R_pkv, 256)):
                        for k in range(8):
                            mm(pp[:, :], Win[:, k, c0:c0 + 128], hT[:, k, :], k == 0, k == 7, (R_W, R_hT), (R_pp,))
                    pss, R_pss = bank.next()
                    sqs = []
                    for (pp, R_pp) in ((pq0, R_pq0), (pq1, R_pq1)):
                        sq, R_sq = sq_r.next()
                        act(sq[:], pp[:, :], AF.Square, (R_pp,), (R_sq,))
                        sqs.append((sq, R_sq))
                    for i, (sq, R_sq) in enumerate(sqs):
                        mm(pss[:, :], ones[:], sq[:], i == 0, i == 1, (R_const, R_sq), (R_pss,))
                    rs, R_rs = rs_r.next()
                    act(rs[:], pss[:, :], AF.Sqrt, (R_pss,), (R_rs,), scale=1.0 / 256, bias=EPS)
                    SC.op("dve", lambda e, o=rs[:], i=rs[:]: e.reciprocal(out=o, in_=i), (R_rs,), (R_rs,))
                    tt("dve", cqn[:, 0, :], pq0[:, :], rs[:], ALU.mult, (R_pq0, R_rs), (R_cqn,))
                    tt("dve", cqn[:, 1, :], pq1[:, :], rs[:], ALU.mult, (R_pq1, R_rs), (R_cqn,))
                    pss, R_pss = bank.next()
                    sq, R_sq = sq_r.next()
                    act(sq[:], pkv[:, :], AF.Square, (R_pkv,), (R_sq,))
                    mm(pss[:, :], ones[:], sq[:], True, True, (R_const, R_sq), (R_pss,))
                    rs, R_rs = rs_r.next()
                    act(rs[:], pss[:, :], AF.Sqrt, (R_pss,), (R_rs,), scale=1.0 / 128, bias=EPS)
                    SC.op("dve", lambda e, o=rs[:], i=rs[:]: e.reciprocal(out=o, in_=i), (R_rs,), (R_rs,))
                    tt("dve", ckvn[:], pkv[:, :], rs[:], ALU.mult, (R_pkv, R_rs), (R_ckvn,))
                    for h in range(4):
                        pz, R_pz = bank.next()
                        pr, R_pr = bank.next()
                        for c in range(2):
                            mm(pz[0:96, :], Wuq[:, c, 96 * h:96 * h + 96], cqn[:, c, :], c == 0, c == 1,
                               (R_W, R_cqn), (R_pz,))
                        for c in range(2):
                            mm(pr[0:96, :], WuqR[:, c, 96 * h:96 * h + 96], cqn[:, c, :], c == 0, c == 1,
                               (R_W, R_cqn), (R_pr,))
                        rope_out(96, pz, R_pz, pr, R_pr, tb[0:96, 4, :], tb[0:96, 5, :], R_tb, QTm[h, :, t0:t0 + 512])
                    for h in range(4):
                        pz, R_pz = bank.next()
                        pr, R_pr = bank.next()
                        mm(pz[0:96, :], Wkk[:, h, :], ckvn[:], True, False, (R_W, R_ckvn), (R_pz,))
                        for k in range(8):
                            mm(pz[0:96, :], Wkp[:, k, :], hT[:, k, :], False, k == 7, (R_W, R_hT), (R_pz,))
                        for k in range(8):
                            mm(pr[0:96, :], WkpR[:, k, :], hT[:, k, :], k == 0, k == 7, (R_W, R_hT), (R_pr,))
                        rope_out(96, pz, R_pz, pr, R_pr, tb[0:96, 4, :], tb[0:96, 5, :], R_tb, KTm[h, :, t0:t0 + 512])
                    for b in range(4):
                        tsl = slice(128 * b, 128 * b + 128)
                        r0 = t0 + 128 * b
                        vo, R_vo = vo_r.next()
                        wo, R_wo = wo_r.next()
                        p1, R_p1 = bank.next()
                        for k in range(8):
                            mm(p1[:, :], hT[:, k, tsl], Win[:, k, 1440:1952], k == 0, k == 7, (R_W, R_hT), (R_p1,))
                        cp("act", vo[:, 0:512], p1[:, :], (R_p1,), (R_vo,))
                        p2, R_p2 = bank.next()
                        for k in range(8):
                            mm(p2[:, 0:256], hT[:, k, tsl], Win[:, k, 2464:2720], k == 0, k == 7, (R_W, R_hT), (R_p2,))
                        cp("act", vo[:, 512:768], p2[:, 0:256], (R_p2,), (R_vo,))
                        p3, R_p3 = bank.next()
                        for k in range(8):
                            mm(p3[:, 0:8], hT[:, k, tsl], Win[:, k, 3008:3016], k == 0, k == 7, (R_W, R_hT), (R_p3,))
                        cp("act", wo[:], p3[:, 0:8], (R_p3,), (R_wo,))
                        p4, R_p4 = bank.next()
                        mm(p4[:, 0:256], ckvn[:, tsl], Wv[:], True, True, (R_W, R_ckvn), (R_p4,))
                        cp("act", vo[:, 768:1024], p4[:, 0:256], (R_p4,), (R_vo,))
                        dma(Vd[r0:r0 + 128, :], vo[:, 0:512], (R_vo,), (R_proj,))
                        dma(Vs[r0:r0 + 128, :], vo[:, 512:768], (R_vo,), (R_proj,))
                        dma(Vm[r0:r0 + 128, :], vo[:, 768:1024], (R_vo,), (R_proj,))
                        dma(WI[r0:r0 + 128, :], wo[:], (R_wo,), (R_proj,))
                SC.barrier()


        def phase_B():
            scale = 96 ** -0.5
            uid[0] += 1
            with ExitStack() as es:
                def sb(name, shape, dt):
                    return es.enter_context(nc.sbuf_tensor(name + "_B_%d" % uid[0], shape, dt))
                KT = sb("KT", [96, S], BF16)
                QT = sb("QT", [96, S], BF16)
                Vx = sb("Vx", [128, NB, 66], BF16)
                R_K, R_Q, R_V = Res("K"), Res("Q"), Res("V")
                pt_r = Ring([(sb("pt%d" % i, [128, 512], BF16), Res("pt%d" % i)) for i in range(3)])
                os_r = Ring([(sb("os%d" % i, [128, 512], F32), Res("os%d" % i)) for i in range(2)])
                rr_r = Ring([(sb("rr%d" % i, [64, 512], F32), Res("rr%d" % i)) for i in range(2)])
                ob_r = Ring([(sb("ob%d" % i, [64, 512], BF16), Res("ob%d" % i)) for i in range(2)])
                s_r = Ring(PS[0:4])
                od_r = Ring(PS[4:6])
                pd_r = Ring(PS[6:8])
                mset("pool", Vx[:, :, 64:66], 1.0, (R_V,))
                for h in range(4):
                    dma(KT[:], KTm[h, :, :], (R_proj,), (R_K,), q="sp")
                    dma(QT[:], QTm[h, :, :], (R_proj,), (R_Q,), q="sp")
                    dma(Vx[:, :, 0:64], Vm[:, 64 * h:64 * h + 64].rearrange("(j p) e -> p j e", p=128),
                        (R_proj,), (R_V,), q="sp")
                    for g in range(NT):
                        od, R_od = od_r.next()
                        nj = 4 * g + 4
                        for j in range(nj):
                            jj = j - 4 * g
                            c0 = 128 * jj if jj > 0 else 0
                            ps, R_ps = s_r.next()
                            mm(ps[:, c0:512], KT[:, 128 * j:128 * j + 128], QT[:, 512 * g + c0:512 * g + 512],
                               True, True, (R_K, R_Q), (R_ps,))
                            pt, R_pt = pt_r.next()
                            act(pt[:, c0:512], ps[:, c0:512], AF.Exp, (R_ps,), (R_pt,), scale=scale)
                            if jj >= 0:
                                tt("dve", pt[:, 128 * jj:128 * jj + 128], pt[:, 128 * jj:128 * jj + 128],
                                   band[:, 0:128], ALU.mult, (R_pt, R_const), (R_pt,))
                            mm(od[0:65, c0:512], Vx[:, j, 0:65], pt[:, c0:512], j == 0, j == nj - 1,
                               (R_V, R_pt), (R_od,))
                        osb, R_os = os_r.next()
                        cp("act", osb[0:65, :], od[0:65, :], (R_od,), (R_os,))
                        pd, R_pd = pd_r.next()
                        mm(pd[0:64, :], sel[0:65, :], osb[0:65, :], True, True, (R_const, R_os), (R_pd,))
                        rr, R_rr = rr_r.next()
                        SC.op("dve", lambda e, o=rr[:, :], i=pd[0:64, :]: e.reciprocal(out=o, in_=i), (R_pd,), (R_rr,))
                        ob, R_ob = ob_r.next()
                        tt("dve", ob[:, :], osb[0:64, :], rr[:, :], ALU.mult, (R_os, R_rr), (R_ob,))
                        dma(mixT[64 * h:64 * h + 64, 512 * g:512 * g + 512], ob[:, :], (R_ob,), (R_mix,))
                SC.barrier()

        def phase_C():
            scale = 64 ** -0.5
            uid[0] += 1
            with ExitStack() as es:
                def sb(name, shape, dt):
                    return es.enter_context(nc.sbuf_tensor(name + "_C_%d" % uid[0], shape, dt))
                KT = sb("KT", [128, S], BF16)
                QT = sb("QT", [128, S], BF16)
                R_K, R_Q = Res("K"), Res("Q")
                vx_r = Ring([(sb("Vx%d" % i, [128, NB, 66], BF16), Res("Vx%d" % i)) for i in range(2)])
                acc = sb("acc", [128, S], F32)
                R_acc = Res("acc")
                pt_r = Ring([(sb("pt%d" % i, [128, 256], BF16), Res("pt%d" % i)) for i in range(3)])
                rr_r = Ring([(sb("rr%d" % i, [64, 512], F32), Res("rr%d" % i)) for i in range(2)])
                ob_r = Ring([(sb("ob%d" % i, [64, 512], BF16), Res("ob%d" % i)) for i in range(2)])
                s_r = Ring(PS[0:4])
                od_b = PS[4:6]
                pd_r = Ring(PS[6:8])
                for (vx, R_vx) in vx_r.items:
                    mset("pool", vx[:, :, 64:66], 1.0, (R_vx,))
                for i in range(4):
                    dma(KT[:], KTd[i, :, :], (R_proj,), (R_K,), q="sp")
                    dma(QT[:], QTd[i, :, :], (R_proj,), (R_Q,), q="sp")
                    for hh in range(2):
                        h = 2 * i + hh
                        rows = slice(64 * hh, 64 * hh + 64)
                        for pi, d in enumerate((1, 4, 16)):
                            vx, R_vx = vx_r.next()
                            vsrc = Vd[:, 64 * h:64 * h + 64].rearrange("(n i d) e -> i n d e", i=128, d=d)
                            for r in range(d):
                                dma(vx[:, r:NB:d, 0:64], vsrc[:, :, r, :], (R_proj,), (R_vx,), q="sp")
                            nblk = S // (128 * d)
                            for r in range(d):
                                for m in range(nblk):
                                    blk = m * d + r
                                    ks = 128 * m * d + r
                                    nq = 256 if m + 1 < nblk else 128
                                    ps, R_ps = s_r.next()
                                    mm(ps[:, 0:nq], KT[rows, ks:ks + 127 * d + 1:d], QT[rows, ks:ks + (nq - 1) * d + 1:d],
                                       True, True, (R_K, R_Q), (R_ps,))
                                    pt, R_pt = pt_r.next()
                                    act(pt[:, 0:nq], ps[:, 0:nq], AF.Exp, (R_ps,), (R_pt,), scale=scale)
                                    tt("pool", pt[:, 0:nq], pt[:, 0:nq], band[:, 0:nq], ALU.mult, (R_pt, R_const), (R_pt,))
                                    od, R_od = od_b[m % 2]
                                    mm(od[0:65, 0:128], vx[:, blk, 0:65], pt[:, 0:128], m == 0, True,
                                       (R_vx, R_pt), (R_od,))
                                    dst = acc[0:65, ks:ks + 127 * d + 1:d]
                                    if pi == 0:
                                        cp("dve", dst, od[0:65, 0:128], (R_od,), (R_acc,))
                                    else:
                                        tt("dve", dst, dst, od[0:65, 0:128], ALU.add, (R_od, R_acc), (R_acc,))
                                    if m + 1 < nblk:
                                        od2, R_od2 = od_b[(m + 1) % 2]
                                        mm(od2[0:65, 0:128], vx[:, blk, 0:65], pt[:, 128:256], True, False,
                                           (R_vx, R_pt), (R_od2,))
                        for g in range(NT):
                            pd, R_pd = pd_r.next()
                            mm(pd[0:64, :], sel[0:65, :], acc[0:65, 512 * g:512 * g + 512], True, True,
                               (R_const, R_acc), (R_pd,))
                            rr, R_rr = rr_r.next()
                            SC.op("dve", lambda e, o=rr[:, :], i_=pd[0:64, :]: e.reciprocal(out=o, in_=i_), (R_pd,), (R_rr,))
                            ob, R_ob = ob_r.next()
                            tt("dve", ob[:, :], acc[0:64, 512 * g:512 * g + 512], rr[:, :], ALU.mult,
                               (R_acc, R_rr), (R_ob,))
                            dma(mixT[256 + 64 * h:256 + 64 * h + 64, 512 * g:512 * g + 512], ob[:, :], (R_ob,), (R_mix,))
                SC.barrier()

        def phase_D():
            scale = 64 ** -0.5
            uid[0] += 1
            with ExitStack() as es:
                def sb(name, shape, dt):
                    return es.enter_context(nc.sbuf_tensor(name + "_D_%d" % uid[0], shape, dt))
                KI = sb("KI", [128, S], BF16)
                KT = sb("KT", [128, 2, S], BF16)
                Vx = sb("Vx", [128, NB, 4, 66], BF16)
                WIt = sb("WIt", [128, NB, 8], F32)
                R_KI, R_K, R_V, R_WI = Res("KI"), Res("K"), Res("V"), Res("WI")
                Isc = sb("Isc", [128, S], F32)
                Msk = sb("Msk", [128, S], BF16)
                MskT = sb("MskT", [128, NB, 128], BF16)
                R_I, R_M, R_MT = Res("I"), Res("M"), Res("MT")
                qi_r = Ring([(sb("qi%d" % i, [96, 3, 128], BF16), Res("qi%d" % i)) for i in range(2)])
                qs_r = Ring([(sb("qs%d" % i, [128, 2, 128], BF16), Res("qs%d" % i)) for i in range(2)])
                rl_r = Ring([(sb("rl%d" % i, [128, 512], F32), Res("rl%d" % i)) for i in range(2)])
                pt_r = Ring([(sb("pt%d" % i, [128, 512], BF16), Res("pt%d" % i)) for i in range(3)])
                st_r = Ring([(sb("st%d" % i, [128, 8], F32), Res("st%d" % i)) for i in range(2)])
                wk_r = Ring([(sb("wk%d" % i, [128, NBIS + 1], F32), Res("wk%d" % i)) for i in range(2)])
                os_r = Ring([(sb("os%d" % i, [128, 128], F32), Res("os%d" % i)) for i in range(2)])
                rr_r = Ring([(sb("rr%d" % i, [64, 128], F32), Res("rr%d" % i)) for i in range(2)])
                ob_r = Ring([(sb("ob%d" % i, [64, 128], BF16), Res("ob%d" % i)) for i in range(2)])
                i_r = Ring(PS[0:3])
                t_b = PS[3]
                s_r = Ring(PS[4:6])
                od_b = PS[6]
                pd_b = PS[7]
                mset("pool", Vx[:, :, :, 64:66], 1.0, (R_V,))
                dma(KI[:], KIT[:, :], (R_proj,), (R_KI,), q="sp")
                for i in range(2):
                    dma(KT[:, i, :], KTs[i, :, :], (R_proj,), (R_K,), q="sp")
                for h in range(4):
                    dma(Vx[:, :, h, 0:64], Vs[:, 64 * h:64 * h + 64].rearrange("(j p) e -> p j e", p=128),
                        (R_proj,), (R_V,), q="sp")
                dma(WIt[:], WI.rearrange("(j p) h -> p j h", p=128), (R_proj,), (R_WI,), q="sp")
                qs_of = {}

                def idx_part(qb):
                    nk = 128 * (qb + 1)
                    nch = (nk + 511) // 512
                    qi, R_qi = qi_r.next()
                    qs, R_qs = qs_r.next()
                    dma(qi[:, 0:2, :], QIT[0:2, :, 128 * qb:128 * qb + 128].rearrange("i p t -> p i t"), (R_proj,), (R_qi,))
                    dma(qi[0:64, 2, :], QIT[2, 0:64, 128 * qb:128 * qb + 128], (R_proj,), (R_qi,))
                    dma(qs[:], QTs[:, :, 128 * qb:128 * qb + 128].rearrange("i p t -> p i t"), (R_proj,), (R_qs,))
                    qs_of[qb] = (qs, R_qs)
                    for c in range(nch):
                        ncol = min(512, nk - 512 * c)
                        cs = slice(512 * c, 512 * c + ncol)
                        for hh in range(8):
                            po = 32 * (hh % 3)
                            bi = hh // 3
                            ps, R_ps = i_r.next()
                            mm(ps[:, 0:ncol], qi[po:po + 32, bi, :], KI[po:po + 32, cs], True, True,
                               (R_qi, R_KI), (R_ps,))
                            rl, R_rl = rl_r.next()
                            act(rl[:, 0:ncol], ps[:, 0:ncol], AF.Relu, (R_ps,), (R_rl,))
                            if hh == 0:
                                ts("dve", Isc[:, cs], rl[:, 0:ncol], WIt[:, qb, 0:1], None, ALU.mult, None,
                                   (R_rl, R_WI), (R_I,))
                            else:
                                stt(Isc[:, cs], rl[:, 0:ncol], WIt[:, qb, hh:hh + 1], Isc[:, cs], ALU.mult, ALU.add,
                                    (R_rl, R_WI, R_I), (R_I,))
                    dg = slice(128 * qb, 128 * qb + 128)
                    tt("dve", Isc[:, dg], Isc[:, dg], negtri[:], ALU.add, (R_I, R_const), (R_I,))

                def bisect_part(qb):
                    nk = 128 * (qb + 1)
                    if nk > TOPK:
                        st, R_st = st_r.next()
                        RS = (R_st,)
                        SC.op("dve", lambda e, o=st[:, 0:1], i_=Isc[:, 0:nk - 128]: e.tensor_reduce(out=o, in_=i_, axis=AX.X, op=ALU.min),
                              (R_I,), RS)
                        SC.op("dve", lambda e, o=st[:, 1:2], i_=Isc[:, 0:nk]: e.tensor_reduce(out=o, in_=i_, axis=AX.X, op=ALU.max),
                              (R_I,), RS)
                        tt("dve", st[:, 1:2], st[:, 1:2], st[:, 0:1], ALU.subtract, RS, RS)
                        wk, R_wk = wk_r.next()
                        ts("dve", wk[:], pw2[:], st[:, 1:2], None, ALU.mult, None, (R_st, R_const), (R_wk,))
                        stt(st[:, 2:3], st[:, 1:2], 0.5, st[:, 0:1], ALU.mult, ALU.add, RS, RS)
                        for it in range(NBIS):
                            ts("dve", Msk[:, 0:nk], Isc[:, 0:nk], st[:, 2:3], 0.0, ALU.is_ge, ALU.add,
                               (R_I, R_st), (R_M, R_st), accum_out=st[:, 3:4])
                            ts("dve", st[:, 4:5], st[:, 3:4], TOPK - 0.5, 0.5, ALU.is_ge, ALU.subtract, RS, RS)
                            stt(st[:, 2:3], st[:, 4:5], wk[:, it:it + 1], st[:, 2:3], ALU.mult, ALU.add,
                                (R_st, R_wk), RS)
                        stt(st[:, 0:1], wk[:, NBIS:NBIS + 1], -1.0, st[:, 2:3], ALU.mult, ALU.add, (R_st, R_wk), RS)
                        ts("dve", Msk[:, 0:nk], Isc[:, 0:nk], st[:, 0:1], None, ALU.is_ge, None, (R_I, R_st), (R_M,))
                    else:
                        ts("dve", Msk[:, 0:nk], Isc[:, 0:nk], -1.0e29, None, ALU.is_ge, None, (R_I,), (R_M,))
                    tb_, R_tb_ = t_b
                    tbb = tb_[:, :].bitcast(BF16)
                    for j0 in range(0, qb + 1, 8):
                        n8 = min(8, qb + 1 - j0)
                        for jj in range(n8):
                            j = j0 + jj
                            tr(tbb[:, 128 * jj:128 * jj + 128], Msk[:, 128 * j:128 * j + 128], (R_M, R_const), (R_tb_,))
                        cp("act", MskT[:, j0:j0 + n8, :], tbb[:, 0:128 * n8].rearrange("p (j t) -> p j t", t=128),
                           (R_tb_,), (R_MT,))

                def attn_part(qb):
                    nk = 128 * (qb + 1)
                    nch = (nk + 511) // 512
                    qs, R_qs = qs_of.pop(qb)
                    for h in range(4):
                        rows = slice(64 * (h % 2), 64 * (h % 2) + 64)
                        bi = h // 2
                        od, R_od = od_b
                        for c in range(nch):
                            nbc = min(4, qb + 1 - 4 * c)
                            ps, R_ps = s_r.next()
                            for jj in range(nbc):
                                j = 4 * c + jj
                                mm(ps[:, 128 * jj:128 * jj + 128], KT[rows, bi, 128 * j:128 * j + 128], qs[rows, bi, :],
                                   True, True, (R_K, R_qs), (R_ps,))
                            pt, R_pt = pt_r.next()
                            act(pt[:, 0:128 * nbc], ps[:, 0:128 * nbc], AF.Exp, (R_ps,), (R_pt,), scale=scale)
                            tt("pool", pt[:, 0:128 * nbc], pt[:, 0:128 * nbc],
                               MskT[:, 4 * c:4 * c + nbc, :].rearrange("p j t -> p (j t)"), ALU.mult,
                               (R_pt, R_MT), (R_pt,))
                            for jj in range(nbc):
                                j = 4 * c + jj
                                mm(od[0:65, 0:128], Vx[:, j, h, 0:65], pt[:, 128 * jj:128 * jj + 128], j == 0, j == qb,
                                   (R_V, R_pt), (R_od,))
                        osb, R_os = os_r.next()
                        cp("act", osb[0:65, :], od[0:65, 0:128], (R_od,), (R_os,))
                        pd, R_pd = pd_b
                        mm(pd[0:64, 0:128], sel[0:65, :], osb[0:65, :], True, True, (R_const, R_os), (R_pd,))
                        rr, R_rr = rr_r.next()
                        SC.op("dve", lambda e, o=rr[:, :], i_=pd[0:64, 0:128]: e.reciprocal(out=o, in_=i_), (R_pd,), (R_rr,))
                        ob, R_ob = ob_r.next()
                        tt("dve", ob[:, :], osb[0:64, :], rr[:, :], ALU.mult, (R_os, R_rr), (R_ob,))
                        dma(mixT[768 + 64 * h:768 + 64 * h + 64, 128 * qb:128 * qb + 128], ob[:, :], (R_ob,), (R_mix,))

                for qb in range(NB):
                    idx_part(qb)
                    if qb > 0:
                        attn_part(qb - 1)
                    bisect_part(qb)
                attn_part(NB - 1)
                SC.barrier()

        def norm_T(es_rings, xt, R_xt, hT, R_hT, b, bank):
            junk, R_junk, hb_r, st_r = es_rings
            hb, R_hb = hb_r.next()
            stt_, R_st_ = st_r.next()
            act(junk[:], xt[:], AF.Square, (R_xt,), (R_junk, R_st_), accum_out=stt_[:, 0:1])
            act(stt_[:, 1:2], stt_[:, 0:1], AF.Sqrt, (R_st_,), (R_st_,), scale=1.0 / D, bias=EPS)
            SC.op("dve", lambda e, o=stt_[:, 2:3], i=stt_[:, 1:2]: e.reciprocal(out=o, in_=i), (R_st_,), (R_st_,))
            ts("dve", hb[:], xt[:], stt_[:, 2:3], None, ALU.mult, None, (R_xt, R_st_), (R_hb,))
            pt_, R_pt = bank.next()
            ptb = pt_[:, :].bitcast(BF16)
            for k in range(8):
                tr(ptb[:, 128 * k:128 * k + 128], hb[:, 128 * k:128 * k + 128], (R_hb, R_const), (R_pt,))
            cp("act", hT[:, :, 128 * b:128 * b + 128], ptb.rearrange("p (k t) -> p k t", t=128), (R_pt,), (R_hT,))

        def phase_E1(l, xsrc, R_xs):
            uid[0] += 1
            with ExitStack() as es:
                def sb(name, shape, dt):
                    return es.enter_context(nc.sbuf_tensor(name + "_E1_%d" % uid[0], shape, dt))
                Wo = sb("Wo", [128, 8, D], BF16)
                Wup = sb("Wup", [128, 8, 2 * DFF], BF16)
                R_W = Res("W_E1")
                stg = Ring([(sb("stg%d" % i, [128, 2048], F32), Res("stg%d" % i)) for i in range(2)])
                for k in range(8):
                    st, R_st = stg.next()
                    dma(st[:, 0:1024], w_o[l, 128 * k:128 * k + 128, :], (), (R_st,))
                    cp("act", Wo[:, k, :], st[:, 0:1024], (R_st,), (R_W,))
                    for (c0, cn) in ((0, 2048), (2048, 2048), (4096, 1536)):
                        st, R_st = stg.next()
                        dma(st[:, 0:cn], w_up[l, 128 * k:128 * k + 128, c0:c0 + cn], (), (R_st,))
                        ts("dve", Wup[:, k, c0:c0 + cn], st[:, 0:cn], gF[:, l * 8 + k:l * 8 + k + 1], None, ALU.mult, None,
                           (R_st, R_const), (R_W,))
                halo = sb("halo", [128, 44, 2], F32)
                R_halo = Res("halo")
                mset("pool", halo[:], 0.0, (R_halo,))
                mT_r = Ring([(sb("mT%d" % i, [128, 8, 512], BF16), Res("mT%d" % i)) for i in range(2)])
                hT_r = Ring([(sb("hT%d" % i, [128, 8, 512], BF16), Res("hT%d" % i)) for i in range(2)])
                xt_r = Ring([(sb("xt%d" % i, [128, D], F32), Res("xt%d" % i)) for i in range(2)])
                x1_r = Ring([(sb("x1%d" % i, [128, D], F32), Res("x1%d" % i)) for i in range(2)])
                junk = sb("junk", [128, D], BF16)
                R_junk = Res("junk")
                hb_r = Ring([(sb("hb%d" % i, [128, D], BF16), Res("hb%d" % i)) for i in range(2)])
                st_r = Ring([(sb("st%d" % i, [128, 4], F32), Res("st%d" % i)) for i in range(2)])
                ub_r = Ring([(sb("ub%d" % i, [128, 514], F32), Res("ub%d" % i)) for i in range(3)])
                y_r = Ring([(sb("y%d" % i, [128, 512], F32), Res("y%d" % i)) for i in range(4)])
                sg_r = Ring([(sb("sg%d" % i, [128, 512], F32), Res("sg%d" % i)) for i in range(2)])
                ab_r = Ring([(sb("ab%d" % i, [128, 512], BF16), Res("ab%d" % i)) for i in range(3)])
                bank = Ring(PS)
                rings = (junk, R_junk, hb_r, st_r)
                for ti in range(NT):
                    t0 = 512 * ti
                    mT, R_mT = mT_r.next()
                    hT, R_hT = hT_r.next()
                    dma(mT[:], mixT[:, t0:t0 + 512].rearrange("(k p) t -> p k t", p=128), (R_mix,), (R_mT,), q="sp")
                    for b in range(4):
                        r0 = t0 + 128 * b
                        xt, R_xt = xt_r.next()
                        x1, R_x1t = x1_r.next()
                        dma(xt[:], xsrc[r0:r0 + 128, :], (R_xs,), (R_xt,))
                        for hf in range(2):
                            ps, R_ps = bank.next()
                            for k in range(8):
                                mm(ps[:, :], mT[:, k, 128 * b:128 * b + 128], Wo[:, k, 512 * hf:512 * hf + 512],
                                   k == 0, k == 7, (R_mT, R_W), (R_ps,))
                            tt("dve", x1[:, 512 * hf:512 * hf + 512], ps[:, :], xt[:, 512 * hf:512 * hf + 512], ALU.add,
                               (R_ps, R_xt), (R_x1t,))
                        dma(xres1[r0:r0 + 128, :], x1[:], (R_x1t,), (R_x1,))
                        norm_T(rings, x1, R_x1t, hT, R_hT, b, bank)
                    for f in range(22):
                        ys = []
                        for fc in (f, 22 + f):
                            ps, R_ps = bank.next()
                            for k in range(8):
                                mm(ps[:, :], Wup[:, k, 128 * fc:128 * fc + 128], hT[:, k, :], k == 0, k == 7,
                                   (R_W, R_hT), (R_ps,))
                            ub, R_ub = ub_r.next()
                            cp("dve", ub[:, 0:2], halo[:, fc, :], (R_halo,), (R_ub,))
                            cp("act", ub[:, 2:514], ps[:, :], (R_ps,), (R_ub,))
                            cp("dve", halo[:, fc, :], ub[:, 512:514], (R_ub,), (R_halo,))
                            y, R_y_ = y_r.next()
                            ci = l * 132 + fc
                            ts("dve", y[:], ps[:, :], cw[:, ci + 88:ci + 89], cb[:, l * 44 + fc:l * 44 + fc + 1],
                               ALU.mult, ALU.add, (R_ps, R_const), (R_y_,))
                            stt(y[:], ub[:, 1:513], cw[:, ci + 44:ci + 45], y[:], ALU.mult, ALU.add, (R_ub, R_const, R_y_), (R_y_,))
                            stt(y[:], ub[:, 0:512], cw[:, ci:ci + 1], y[:], ALU.mult, ALU.add, (R_ub, R_const, R_y_), (R_y_,))
                            ys.append((y, R_y_))
                        sg, R_sg = sg_r.next()
                        act(sg[:], ys[0][0][:], AF.Silu, (ys[0][1],), (R_sg,))
                        ab, R_ab = ab_r.next()
                        tt("dve", ab[:], sg[:], ys[1][0][:], ALU.mult, (R_sg, ys[1][1]), (R_ab,))
                        dma(actT[128 * f:128 * f + 128, t0:t0 + 512], ab[:], (R_ab,), (R_act,))
                SC.barrier()

        def phase_E2(l, last):
            uid[0] += 1
            with ExitStack() as es:
                def sb(name, shape, dt):
                    return es.enter_context(nc.sbuf_tensor(name + "_E2_%d" % uid[0], shape, dt))
                Wdn = sb("Wdn", [128, 22, D], BF16)
                R_W = Res("W_E2")
                stg = Ring([(sb("stg%d" % i, [128, D], F32), Res("stg%d" % i)) for i in range(2)])
                for f in range(22):
                    st, R_st = stg.next()
                    dma(st[:], w_down[l, 128 * f:128 * f + 128, :], (), (R_st,))
                    cp("act" if f % 2 else "dve", Wdn[:, f, :], st[:], (R_st,), (R_W,))
                gfin = sb("gfin", [128, D], F32)
                R_gf = Res("gfin")
                if last:
                    dma(gfin[:], gfin_d[:, :], (), (R_gf,))
                aT_r = Ring([(sb("aT%d" % i, [128, 22, 512], BF16), Res("aT%d" % i)) for i in range(2)])
                xt_r = Ring([(sb("xt%d" % i, [128, D], F32), Res("xt%d" % i)) for i in range(2)])
                x2_r = Ring([(sb("x2%d" % i, [128, D], F32), Res("x2%d" % i)) for i in range(2)])
                yo_r = Ring([(sb("yo%d" % i, [128, D], F32), Res("yo%d" % i)) for i in range(2)])
                junk = sb("junk", [128, D], BF16)
                R_junk = Res("junk")
                st_r = Ring([(sb("st%d" % i, [128, 4], F32), Res("st%d" % i)) for i in range(2)])
                bank = Ring(PS)
                for ti in range(NT):
                    t0 = 512 * ti
                    aT, R_aT = aT_r.next()
                    dma(aT[:], actT[:, t0:t0 + 512].rearrange("(f p) t -> p f t", p=128), (R_act,), (R_aT,), q="sp")
                    for b in range(4):
                        r0 = t0 + 128 * b
                        xt, R_xt = xt_r.next()
                        x2, R_x2 = x2_r.next()
                        dma(xt[:], xres1[r0:r0 + 128, :], (R_x1,), (R_xt,))
                        for hf in range(2):
                            ps, R_ps = bank.next()
                            for f in range(22):
                                mm(ps[:, :], aT[:, f, 128 * b:128 * b + 128], Wdn[:, f, 512 * hf:512 * hf + 512],
                                   f == 0, f == 21, (R_aT, R_W), (R_ps,))
                            tt("dve", x2[:, 512 * hf:512 * hf + 512], ps[:, :], xt[:, 512 * hf:512 * hf + 512], ALU.add,
                               (R_ps, R_xt), (R_x2,))
                        if not last:
                            dma(xres0[r0:r0 + 128, :], x2[:], (R_x2,), (R_x,))
                        else:
                            stt_, R_st_ = st_r.next()
                            yo, R_yo = yo_r.next()
                            act(junk[:], x2[:], AF.Square, (R_x2,), (R_junk, R_st_), accum_out=stt_[:, 0:1])
                            act(stt_[:, 1:2], stt_[:, 0:1], AF.Sqrt, (R_st_,), (R_st_,), scale=1.0 / D, bias=EPS)
                            SC.op("dve", lambda e, o=stt_[:, 2:3], i=stt_[:, 1:2]: e.reciprocal(out=o, in_=i), (R_st_,), (R_st_,))
                            stt(yo[:], x2[:], stt_[:, 2:3], gfin[:], ALU.mult, ALU.mult, (R_x2, R_st_, R_gf), (R_yo,))
                            dma(y_out[r0:r0 + 128, :], yo[:], (R_yo,), (R_y,))
                SC.barrier()

        for l in range(L):
            xsrc = x_in if l == 0 else xres0
            phase_A(l, xsrc)
            if stop_after == "A":
                break
            if "B" not in skip:
                phase_B()
            if stop_after == "B":
                break
            if "C" not in skip:
                phase_C()
            if stop_after == "C":
                break
            if "D" not in skip:
                phase_D()
            if stop_after == "D":
                break
            phase_E1(l, xsrc, R_x)
            if stop_after == "E1":
                break
            phase_E2(l, l == L - 1)
        SC.barrier()
        print("instructions:", SC.ninst)
    return nc


def make_consts(S):
    bf = ml_dtypes.bfloat16
    c = {}
    c["c_ident"] = np.eye(128, dtype=np.float32).astype(bf)
    c["c_ones"] = np.ones((128, 128), np.float32).astype(bf)
    sel = np.zeros((128, 64), np.float32)
    sel[64, :] = 1.0
    c["c_sel"] = sel
    k = np.arange(128)[:, None]
    q = np.arange(128)[None, :]
    c["c_band"] = np.concatenate([(k <= q), (k >= q)], axis=1).astype(np.float32).astype(bf)
    c["c_negtri"] = np.where(q.T >= k.T, 0.0, NEG).astype(np.float32) if False else \
        np.where(np.arange(128)[None, :] <= np.arange(128)[:, None], 0.0, NEG).astype(np.float32)
    c["c_pw2"] = np.ascontiguousarray(np.broadcast_to(
        (2.0 ** -(np.arange(NBIS + 1, dtype=np.float32) + 1.0)).astype(np.float32)[None, :], (128, NBIS + 1)))
    t = np.arange(S, dtype=np.float32)

    def tables(dim):
        inv = np.power(np.float32(500000.0), -np.arange(0, dim, 2, dtype=np.float32) / np.float32(dim)).astype(np.float32)
        ang = (t[:, None] * inv[None, :]).astype(np.float32)
        return np.cos(ang).astype(np.float32).T, np.sin(ang).astype(np.float32).T
    ch, sh = tables(16)
    ci, si = tables(8)
    ca, sa = tables(32)
    C = np.ones((128, S), np.float32)
    Sn = np.zeros((128, S), np.float32)
    for hh in range(2):
        C[64 * hh:64 * hh + 8] = ch
        C[64 * hh + 8:64 * hh + 16] = ch
        Sn[64 * hh:64 * hh + 8] = sh
        Sn[64 * hh + 8:64 * hh + 16] = sh
    c["tab64"] = np.stack([C, Sn])
    C = np.ones((128, S), np.float32)
    Sn = np.zeros((128, S), np.float32)
    for hh in range(4):
        C[32 * hh:32 * hh + 4] = ci
        C[32 * hh + 4:32 * hh + 8] = ci
        Sn[32 * hh:32 * hh + 4] = si
        Sn[32 * hh + 4:32 * hh + 8] = si
    c["tabidx"] = np.stack([C, Sn])
    C = np.ones((96, S), np.float32)
    Sn = np.zeros((96, S), np.float32)
    C[64:80] = ca
    C[80:96] = ca
    Sn[64:80] = sa
    Sn[80:96] = sa
    c["tabmla"] = np.stack([C, Sn])
    return c


def layout_params(inp, L):
    f = np.float32
    o = {}
    o["gA"] = np.ascontiguousarray(np.asarray(inp["g_attn"], f).reshape(L, 8, 128).transpose(2, 0, 1).reshape(128, L * 8))
    o["gF"] = np.ascontiguousarray(np.asarray(inp["g_ffn"], f).reshape(L, 8, 128).transpose(2, 0, 1).reshape(128, L * 8))
    o["gq"] = np.ascontiguousarray(np.asarray(inp["g_q_lat"], f).reshape(L, 2, 128).transpose(2, 0, 1).reshape(128, L * 2))
    o["gkv"] = np.ascontiguousarray(np.asarray(inp["g_kv_lat"], f).reshape(L, 128).T)
    o["cw"] = np.ascontiguousarray(np.asarray(inp["conv_w"], f).reshape(L, 3, 44, 128).transpose(3, 0, 1, 2).reshape(128, L * 3 * 44))
    o["cb"] = np.ascontiguousarray(np.asarray(inp["conv_b"], f).reshape(L, 44, 128).transpose(2, 0, 1).reshape(128, L * 44))
    o["gfin"] = np.ascontiguousarray(np.broadcast_to(np.asarray(inp["g_final"], f).reshape(1, D), (128, D)))
    for k in ("w_in", "w_uq", "w_ukv", "w_o", "w_up", "w_down"):
        o[k] = np.ascontiguousarray(np.asarray(inp[k], f))
    return o


_CACHE = {}


def kernel(**inputs):
    x = np.asarray(inputs["x"], np.float32)
    B, S, _ = x.shape
    L = inputs["w_in"].shape[0]
    key = (S, L)
    if key not in _CACHE:
        _CACHE[key] = (build(S, L), make_consts(S))
    nc, consts = _CACHE[key]
    shared = layout_params(inputs, L)
    shared.update(consts)
    n = 8
    in_maps = []
    for c in range(n):
        m = dict(shared)
        m["x"] = np.ascontiguousarray(x[c % B])
        in_maps.append(m)
    res = run_bass_kernel_spmd(nc, in_maps, core_ids=list(range(n)))
    return np.stack([res.results[b]["y"] for b in range(B)], axis=0).astype(np.float32)
```

```python
import numpy as np
import ml_dtypes
from contextlib import ExitStack
import concourse.bass as bass
import concourse.mybir as mybir
from concourse.bass_utils import run_bass_kernel_spmd

F32 = mybir.dt.float32
BF16 = mybir.dt.bfloat16
AF = mybir.ActivationFunctionType
ALU = mybir.AluOpType
AX = mybir.AxisListType

EPS = 1e-6
NEG = -1.0e30
D = 1024
NIN = 3016
DFF = 2816
TOPK = 256
NBIS = 17


class Res:
    __slots__ = ("name", "w", "r", "ordered")

    def __init__(self, name, ordered=True):
        self.name = name
        self.w = {}
        self.r = {}
        self.ordered = ordered


class Eng:
    def __init__(self, name, eng, sem):
        self.name = name
        self.eng = eng
        self.sem = sem
        self.n = 0
        self.waited = {}
        self.dsem = []
        self.dval = []
        self.di = 0


class Sched:
    K = 12

    def __init__(self, nc, es):
        self.nc = nc
        self.E = {}
        for name, e in (("pe", nc.tensor), ("act", nc.scalar), ("dve", nc.vector),
                        ("pool", nc.gpsimd), ("sp", nc.sync)):
            self.E[name] = Eng(name, e, es.enter_context(nc.semaphore("p_" + name)))
        for q in ("sp", "pool", "act"):
            E = self.E[q]
            for i in range(self.K):
                E.dsem.append(es.enter_context(nc.semaphore("d_%s%d" % (q, i))))
                E.dval.append(0)
        self.ninst = 0

    def _need(self, reads, writes):
        need = {}

        def add(d):
            for k, sv in d.items():
                if k not in need or need[k][1] < sv[1]:
                    need[k] = sv
        for r in reads:
            add(r.w)
        for w in writes:
            add(w.r)
            if w.ordered:
                add(w.w)
        return need

    def _emit_waits(self, E, need, is_dma):
        for k, (s, v) in need.items():
            if s is E.sem and not is_dma and E.name == "pe":
                continue
            if E.waited.get(k, 0) >= v:
                continue
            E.eng.wait_ge(s, v)
            E.waited[k] = v

    def _mark(self, tok, reads, writes):
        k = id(tok[0])
        for r in reads:
            if r.r.get(k, (None, 0))[1] < tok[1]:
                r.r[k] = tok
        for w in writes:
            if w.ordered:
                w.w = {k: tok}
                w.r = {}
            else:
                w.w[k] = tok

    def op(self, eng, fn, reads=(), writes=()):
        E = self.E[eng]
        self._emit_waits(E, self._need(reads, writes), False)
        inst = fn(E.eng)
        E.n += 1
        inst.then_inc(E.sem, 1)
        self._mark((E.sem, E.n), reads, writes)
        self.ninst += 1

    def dma(self, q, out, in_, reads=(), writes=()):
        E = self.E[q]
        self._emit_waits(E, self._need(reads, writes), True)
        i = E.di % self.K
        E.di += 1
        s = E.dsem[i]
        pv = E.dval[i]
        if pv > 0 and E.waited.get(id(s), 0) < pv:
            E.eng.wait_ge(s, pv)
            E.waited[id(s)] = pv
        E.eng.dma_start(out=out, in_=in_).then_inc(s, 16)
        E.dval[i] = pv + 16
        self._mark((s, pv + 16), reads, writes)
        self.ninst += 1

    def barrier(self):
        for E in self.E.values():
            for O in self.E.values():
                if O is not E and O.n > 0 and E.waited.get(id(O.sem), 0) < O.n:
                    E.eng.wait_ge(O.sem, O.n)
                    E.waited[id(O.sem)] = O.n
                for s, v in zip(O.dsem, O.dval):
                    if v > 0 and E.waited.get(id(s), 0) < v:
                        E.eng.wait_ge(s, v)
                        E.waited[id(s)] = v


class Ring:
    def __init__(self, items):
        self.items = items
        self.i = 0

    def next(self):
        it = self.items[self.i % len(self.items)]
        self.i += 1
        return it


def build(S=8192, L=4, dbg=False, stop_after=None, skip=()):
    NT = S // 512
    NB = S // 128
    nc = bass.Bass("TRN2", target_bir_lowering=False)

    def din(name, shape, dt=F32):
        return nc.dram_tensor(name, list(shape), dt, kind="ExternalInput").ap()

    def dscr(name, shape, dt):
        return nc.dram_tensor(name, list(shape), dt, kind=("ExternalOutput" if dbg else "Internal")).ap()

    x_in = din("x", [S, D])
    w_in = din("w_in", [L, D, NIN])
    w_uq = din("w_uq", [L, 256, 384])
    w_ukv = din("w_ukv", [L, 128, 512])
    w_o = din("w_o", [L, D, D])
    w_up = din("w_up", [L, D, 2 * DFF])
    w_down = din("w_down", [L, DFF, D])
    gA_d = din("gA", [128, L * 8])
    gF_d = din("gF", [128, L * 8])
    gq_d = din("gq", [128, L * 2])
    gkv_d = din("gkv", [128, L])
    cw_d = din("cw", [128, L * 3 * 44])
    cb_d = din("cb", [128, L * 44])
    gfin_d = din("gfin", [128, D])
    c_ident = din("c_ident", [128, 128], BF16)
    c_ones = din("c_ones", [128, 128], BF16)
    c_sel = din("c_sel", [128, 64])
    c_band = din("c_band", [128, 256], BF16)
    c_negtri = din("c_negtri", [128, 128])
    c_pw2 = din("c_pw2", [128, NBIS + 1])
    tab64 = din("tab64", [2, 128, S])
    tabidx = din("tabidx", [2, 128, S])
    tabmla = din("tabmla", [2, 96, S])
    y_out = nc.dram_tensor("y", [S, D], F32, kind="ExternalOutput").ap()

    xres0 = dscr("xres0", [S, D], F32)
    xres1 = dscr("xres1", [S, D], F32)
    QTm = dscr("QTm", [4, 96, S], BF16)
    KTm = dscr("KTm", [4, 96, S], BF16)
    Vm = dscr("Vm", [S, 256], BF16)
    QTd = dscr("QTd", [4, 128, S], BF16)
    KTd = dscr("KTd", [4, 128, S], BF16)
    Vd = dscr("Vd", [S, 512], BF16)
    QTs = dscr("QTs", [2, 128, S], BF16)
    KTs = dscr("KTs", [2, 128, S], BF16)
    Vs = dscr("Vs", [S, 256], BF16)
    QIT = dscr("QIT", [3, 96, S], BF16)
    KIT = dscr("KIT", [128, S], BF16)
    WI = dscr("WI", [S, 8], F32)
    mixT = dscr("mixT", [D, S], BF16)
    actT = dscr("actT", [DFF, S], BF16)

    ges = ExitStack()
    with ges:
        SC = Sched(nc, ges)
        R_x = Res("xres0", ordered=False)
        R_x1 = Res("xres1", ordered=False)
        R_proj = Res("proj", ordered=False)
        R_mix = Res("mixT", ordered=False)
        R_act = Res("actT", ordered=False)
        R_y = Res("y", ordered=False)
        R_const = Res("const")

        def gsb(name, shape, dt):
            return ges.enter_context(nc.sbuf_tensor("g_" + name, shape, dt))

        PS = []
        for i in range(8):
            t = ges.enter_context(nc.psum_tensor("ps%d" % i, [128, 512], F32))
            PS.append((t, Res("ps%d" % i)))

        gA = gsb("gA", [128, L * 8], F32)
        gF = gsb("gF", [128, L * 8], F32)
        gq = gsb("gq", [128, L * 2], F32)
        gkv = gsb("gkv", [128, L], F32)
        cw = gsb("cw", [128, L * 3 * 44], F32)
        cb = gsb("cb", [128, L * 44], F32)
        ident = gsb("ident", [128, 128], BF16)
        ones = gsb("ones", [128, 128], BF16)
        sel = gsb("sel", [128, 64], F32)
        band = gsb("band", [128, 256], BF16)
        negtri = gsb("negtri", [128, 128], F32)
        pw2 = gsb("pw2", [128, NBIS + 1], F32)
        for dst, src in ((gA, gA_d), (gF, gF_d), (gq, gq_d), (gkv, gkv_d), (cw, cw_d), (cb, cb_d),
                         (ident, c_ident), (ones, c_ones), (sel, c_sel), (band, c_band), (negtri, c_negtri), (pw2, c_pw2)):
            SC.dma("sp", dst[:], src[:, :], reads=(), writes=(R_const,))
        R_const.ordered = False

        def mm(out, lhsT, rhs, start, stop, reads, writes):
            SC.op("pe", lambda e: e.matmul(out, lhsT=lhsT, rhs=rhs, start=start, stop=stop), reads, writes)

        def tr(out, in_, reads, writes):
            SC.op("pe", lambda e: e.transpose(out, in_, ident[:]), reads, writes)

        def act(out, in_, func, reads, writes, scale=1.0, bias=None, accum_out=None):
            kw = {}
            if bias is not None:
                kw["bias"] = bias
            if accum_out is not None:
                kw["accum_out"] = accum_out
            SC.op("act", lambda e: e.activation(out=out, in_=in_, func=func, scale=scale, **kw), reads, writes)

        def ts(eng, out, in0, s1, s2, op0, op1, reads, writes, accum_out=None):
            kw = {}
            if accum_out is not None:
                kw["accum_out"] = accum_out
            if op1 is None:
                SC.op(eng, lambda e: e.tensor_scalar(out=out, in0=in0, scalar1=s1, scalar2=None, op0=op0, **kw),
                      reads, writes)
            else:
                SC.op(eng, lambda e: e.tensor_scalar(out=out, in0=in0, scalar1=s1, scalar2=s2, op0=op0, op1=op1, **kw),
                      reads, writes)

        def tt(eng, out, in0, in1, op, reads, writes):
            SC.op(eng, lambda e: e.tensor_tensor(out=out, in0=in0, in1=in1, op=op), reads, writes)

        def stt(out, in0, scalar, in1, op0, op1, reads, writes):
            SC.op("dve", lambda e: e.scalar_tensor_tensor(out=out, in0=in0, scalar=scalar, in1=in1, op0=op0, op1=op1),
                  reads, writes)

        def cp(eng, out, in_, reads, writes):
            if eng == "act":
                SC.op("act", lambda e: e.activation(out=out, in_=in_, func=AF.Copy), reads, writes)
            else:
                SC.op(eng, lambda e: e.tensor_copy(out=out, in_=in_), reads, writes)

        def mset(eng, ap, val, writes):
            SC.op(eng, lambda e: e.memset(ap, val), (), writes)

        uid = [0]
        dq = ["sp", "pool"]
        dqi = [0]

        def dma(out, in_, reads, writes, q=None):
            if q is None:
                q = dq[dqi[0] % 2]
                dqi[0] += 1
            SC.dma(q, out, in_, reads, writes)

        def rms_rows(es, xt, R_xt, hb, R_hb, name):
            pass

        def phase_A(l, xsrc):
            uid[0] += 1
            with ExitStack() as es:
                def sb(name, shape, dt):
                    return es.enter_context(nc.sbuf_tensor(name + "_A_%d" % uid[0], shape, dt))
                Win = sb("Win", [128, 8, NIN], BF16)
                WinR = sb("WinR", [128, 8, 1920], BF16)
                Wki = sb("Wki", [128, 8, 128], BF16)
                Wkp = sb("Wkp", [128, 8, 96], BF16)
                WkpR = sb("WkpR", [128, 8, 96], BF16)
                Wuq = sb("Wuq", [128, 2, 384], BF16)
                WuqR = sb("WuqR", [128, 2, 384], BF16)
                Wukv = sb("Wukv", [128, 512], BF16)
                Wkk = sb("Wkk", [128, 4, 96], BF16)
                Wv = sb("Wv", [128, 256], BF16)
                R_W = Res("W_A")
                stg = Ring([(sb("stg%d" % i, [128, 1508], F32), Res("stg%d" % i)) for i in range(2)])
                for k in range(8):
                    for hf in range(2):
                        st, R_st = stg.next()
                        dma(st[:], w_in[l, 128 * k:128 * k + 128, 1508 * hf:1508 * hf + 1508], (), (R_st,))
                        ts("dve", Win[:, k, 1508 * hf:1508 * hf + 1508], st[:], gA[:, l * 8 + k:l * 8 + k + 1], None,
                           ALU.mult, None, (R_st, R_const), (R_W,))
                for c in range(2):
                    st, R_st = stg.next()
                    dma(st[:, 0:384], w_uq[l, 128 * c:128 * c + 128, :], (), (R_st,))
                    ts("dve", Wuq[:, c, :], st[:, 0:384], gq[:, l * 2 + c:l * 2 + c + 1], None, ALU.mult, None,
                       (R_st, R_const), (R_W,))
                st, R_st = stg.next()
                dma(st[:, 0:512], w_ukv[l, :, :], (), (R_st,))
                ts("dve", Wukv[:], st[:, 0:512], gkv[:, l:l + 1], None, ALU.mult, None, (R_st, R_const), (R_W,))
                mset("pool", WinR[:], 0.0, (R_W,))
                mset("pool", Wkp[:], 0.0, (R_W,))
                mset("pool", WkpR[:], 0.0, (R_W,))
                mset("pool", WuqR[:], 0.0, (R_W,))
                mset("pool", Wkk[:], 0.0, (R_W,))
                RW = (R_W,)
                for k in range(8):
                    for base, nh, rb in ((416, 8, 0), (928, 8, 512), (1952, 4, 1024), (2208, 4, 1280)):
                        src = Win[:, k, base:base + nh * 64].rearrange("p (h e) -> p h e", e=64)
                        dst = WinR[:, k, rb:rb + nh * 64].rearrange("p (h e) -> p h e", e=64)
                        ts("dve", dst[:, :, 0:8], src[:, :, 8:16], -1.0, None, ALU.mult, None, RW, RW)
                        cp("dve", dst[:, :, 8:16], src[:, :, 0:8], RW, RW)
                    src = Win[:, k, 2720:2976].rearrange("p (h e) -> p h e", e=32)
                    dst = WinR[:, k, 1536:1792].rearrange("p (h e) -> p h e", e=32)
                    ts("dve", dst[:, :, 0:4], src[:, :, 4:8], -1.0, None, ALU.mult, None, RW, RW)
                    cp("dve", dst[:, :, 4:8], src[:, :, 0:4], RW, RW)
                    for r in range(4):
                        cp("dve", Wki[:, k, 32 * r:32 * r + 32], Win[:, k, 2976:3008], RW, RW)
                        ts("dve", WinR[:, k, 1792 + 32 * r:1792 + 32 * r + 4], Win[:, k, 2980:2984], -1.0, None,
                           ALU.mult, None, RW, RW)
                        cp("dve", WinR[:, k, 1792 + 32 * r + 4:1792 + 32 * r + 8], Win[:, k, 2976:2980], RW, RW)
                    cp("dve", Wkp[:, k, 64:96], Win[:, k, 384:416], RW, RW)
                    ts("dve", WkpR[:, k, 64:80], Win[:, k, 400:416], -1.0, None, ALU.mult, None, RW, RW)
                    cp("dve", WkpR[:, k, 80:96], Win[:, k, 384:400], RW, RW)
                for c in range(2):
                    src = Wuq[:, c, :].rearrange("p (h e) -> p h e", e=96)
                    dst = WuqR[:, c, :].rearrange("p (h e) -> p h e", e=96)
                    ts("dve", dst[:, :, 64:80], src[:, :, 80:96], -1.0, None, ALU.mult, None, RW, RW)
                    cp("dve", dst[:, :, 80:96], src[:, :, 64:80], RW, RW)
                ukv = Wukv[:].rearrange("p (h t e) -> p h t e", t=2, e=64)
                cp("dve", Wkk[:, :, 0:64], ukv[:, :, 0, :], RW, RW)
                cp("dve", Wv[:].rearrange("p (h e) -> p h e", e=64), ukv[:, :, 1, :], RW, RW)

                xt_r = Ring([(sb("xt%d" % i, [128, D], F32), Res("xt%d" % i)) for i in range(2)])
                junk = sb("junk", [128, D], BF16)
                R_junk = Res("junk")
                hb_r = Ring([(sb("hb%d" % i, [128, D], BF16), Res("hb%d" % i)) for i in range(2)])
                st_r = Ring([(sb("st%d" % i, [128, 4], F32), Res("st%d" % i)) for i in range(2)])
                hT_r = Ring([(sb("hT%d" % i, [128, 8, 512], BF16), Res("hT%d" % i)) for i in range(2)])
                tb_r = Ring([(sb("tb%d" % i, [128, 6, 512], F32), Res("tb%d" % i)) for i in range(2)])
                t1_r = Ring([(sb("t1_%d" % i, [128, 512], F32), Res("t1_%d" % i)) for i in range(2)])
                t2_r = Ring([(sb("t2_%d" % i, [128, 512], F32), Res("t2_%d" % i)) for i in range(2)])
                ob_r = Ring([(sb("ob%d" % i, [128, 512], BF16), Res("ob%d" % i)) for i in range(4)])
                sq_r = Ring([(sb("sq%d" % i, [128, 512], BF16), Res("sq%d" % i)) for i in range(2)])
                rs_r = Ring([(sb("rs%d" % i, [128, 512], F32), Res("rs%d" % i)) for i in range(2)])
                cqn = sb("cqn", [128, 2, 512], BF16)
                R_cqn = Res("cqn")
                ckvn = sb("ckvn", [128, 512], BF16)
                R_ckvn = Res("ckvn")
                vo_r = Ring([(sb("vo%d" % i, [128, 1024], BF16), Res("vo%d" % i)) for i in range(2)])
                wo_r = Ring([(sb("wo%d" % i, [128, 8], F32), Res("wo%d" % i)) for i in range(2)])
                bank = Ring(PS)

                def rope_out(rows, pz, R_pz, pr, R_pr, tC, tS, R_tb, dst_ap):
                    t1, R_t1 = t1_r.next()
                    t2, R_t2 = t2_r.next()
                    ob, R_ob = ob_r.next()
                    tt("dve", t1[0:rows, :], pz[0:rows, :], tC, ALU.mult, (R_pz, R_tb), (R_t1,))
                    tt("dve", t2[0:rows, :], pr[0:rows, :], tS, ALU.mult, (R_pr, R_tb), (R_t2,))
                    tt("pool", ob[0:rows, :], t1[0:rows, :], t2[0:rows, :], ALU.add, (R_t1, R_t2), (R_ob,))
                    dma(dst_ap, ob[0:rows, :], (R_ob,), (R_proj,))

                for ti in range(NT):
                    t0 = 512 * ti
                    hT, R_hT = hT_r.next()
                    for b in range(4):
                        xt, R_xt = xt_r.next()
                        hb, R_hb = hb_r.next()
                        stt_, R_st_ = st_r.next()
                        dma(xt[:], xsrc[t0 + 128 * b:t0 + 128 * b + 128, :], (R_x,), (R_xt,))
                        act(junk[:], xt[:], AF.Square, (R_xt,), (R_junk, R_st_), accum_out=stt_[:, 0:1])
                        act(stt_[:, 1:2], stt_[:, 0:1], AF.Sqrt, (R_st_,), (R_st_,), scale=1.0 / D, bias=EPS)
                        SC.op("dve", lambda e, o=stt_[:, 2:3], i=stt_[:, 1:2]: e.reciprocal(out=o, in_=i), (R_st_,), (R_st_,))
                        ts("dve", hb[:], xt[:], stt_[:, 2:3], None, ALU.mult, None, (R_xt, R_st_), (R_hb,))
                        pt_, R_pt = bank.next()
                        ptb = pt_[:, :].bitcast(BF16)
                        for k in range(8):
                            tr(ptb[:, 128 * k:128 * k + 128], hb[:, 128 * k:128 * k + 128], (R_hb, R_const), (R_pt,))
                        cp("act", hT[:, :, 128 * b:128 * b + 128], ptb.rearrange("p (k t) -> p k t", t=128),
                           (R_pt,), (R_hT,))
                    tb, R_tb = tb_r.next()
                    dma(tb[:, 0, :], tab64[0, :, t0:t0 + 512], (), (R_tb,))
                    dma(tb[:, 1, :], tab64[1, :, t0:t0 + 512], (), (R_tb,))
                    dma(tb[:, 2, :], tabidx[0, :, t0:t0 + 512], (), (R_tb,))
                    dma(tb[:, 3, :], tabidx[1, :, t0:t0 + 512], (), (R_tb,))
                    dma(tb[0:96, 4, :], tabmla[0, :, t0:t0 + 512], (), (R_tb,))
                    dma(tb[0:96, 5, :], tabmla[1, :, t0:t0 + 512], (), (R_tb,))
                    blocks = []
                    for i in range(4):
                        blocks.append((Win, 416 + 128 * i, WinR, 128 * i, 0, QTd[i, :, t0:t0 + 512], 128))
                    for i in range(4):
                        blocks.append((Win, 928 + 128 * i, WinR, 512 + 128 * i, 0, KTd[i, :, t0:t0 + 512], 128))
                    for i in range(2):
                        blocks.append((Win, 1952 + 128 * i, WinR, 1024 + 128 * i, 0, QTs[i, :, t0:t0 + 512], 128))
                    for i in range(2):
                        blocks.append((Win, 2208 + 128 * i, WinR, 1280 + 128 * i, 0, KTs[i, :, t0:t0 + 512], 128))
                    for i in range(3):
                        nr = 96 if i < 2 else 64
                        blocks.append((Win, 2720 + 96 * i, WinR, 1536 + 96 * i, 2, QIT[i, 0:nr, t0:t0 + 512], nr))
                    blocks.append((Wki, 0, WinR, 1792, 2, KIT[:, t0:t0 + 512], 128))
                    for (Wz, cz, Wr, cr, tbi, dst, nr) in blocks:
                        pz, R_pz = bank.next()
                        pr, R_pr = bank.next()
                        for k in range(8):
                            mm(pz[0:nr, :], Wz[:, k, cz:cz + nr], hT[:, k, :], k == 0, k == 7, (R_W, R_hT), (R_pz,))
                        for k in range(8):
                            mm(pr[0:nr, :], Wr[:, k, cr:cr + nr], hT[:, k, :], k == 0, k == 7, (R_W, R_hT), (R_pr,))
                        rope_out(nr, pz, R_pz, pr, R_pr, tb[0:nr, tbi, :], tb[0:nr, tbi + 1, :], R_tb, dst)
                    pq0, R_pq0 = bank.next()
                    pq1, R_pq1 = bank.next()
                    pkv, R_pkv = bank.next()
                    for (pp, R_pp, c0) in ((pq0, R_pq0, 0), (pq1, R_pq1, 128), (pkv, R_pkv, 256)):
                        for k in range(8):
                            mm(pp[:, :], Win[:, k, c0:c0 + 128], hT[:, k, :], k == 0, k == 7, (R_W, R_hT), (R_pp,))
                    pss, R_pss = bank.next()
                    sqs = []
                    for (pp, R_pp) in ((pq0, R_pq0), (pq1, R_pq1)):
                        sq, R_sq = sq_r.next()
                        act(sq[:], pp[:, :], AF.Square, (R_pp,), (R_sq,))
                        sqs.append((sq, R_sq))
                    for i, (sq, R_sq) in enumerate(sqs):
                        mm(pss[:, :], ones[:], sq[:], i == 0, i == 1, (R_const, R_sq), (R_pss,))
                    rs, R_rs = rs_r.next()
                    act(rs[:], pss[:, :], AF.Sqrt, (R_pss,), (R_rs,), scale=1.0 / 256, bias=EPS)
                    SC.op("dve", lambda e, o=rs[:], i=rs[:]: e.reciprocal(out=o, in_=i), (R_rs,), (R_rs,))
                    tt("dve", cqn[:, 0, :], pq0[:, :], rs[:], ALU.mult, (R_pq0, R_rs), (R_cqn,))
                    tt("dve", cqn[:, 1, :], pq1[:, :], rs[:], ALU.mult, (R_pq1, R_rs), (R_cqn,))
                    pss, R_pss = bank.next()
                    sq, R_sq = sq_r.next()
                    act(sq[:], pkv[:, :], AF.Square, (R_pkv,), (R_sq,))
                    mm(pss[:, :], ones[:], sq[:], True, True, (R_const, R_sq), (R_pss,))
                    rs, R_rs = rs_r.next()
                    act(rs[:], pss[:, :], AF.Sqrt, (R_pss,), (R_rs,), scale=1.0 / 128, bias=EPS)
                    SC.op("dve", lambda e, o=rs[:], i=rs[:]: e.reciprocal(out=o, in_=i), (R_rs,), (R_rs,))
                    tt("dve", ckvn[:], pkv[:, :], rs[:], ALU.mult, (R_pkv, R_rs), (R_ckvn,))
                    for h in range(4):
                        pz, R_pz = bank.next()
                        pr, R_pr = bank.next()
                        for c in range(2):
                            mm(pz[0:96, :], Wuq[:, c, 96 * h:96 * h + 96], cqn[:, c, :], c == 0, c == 1,
                               (R_W, R_cqn), (R_pz,))
                        for c in range(2):
                            mm(pr[0:96, :], WuqR[:, c, 96 * h:96 * h + 96], cqn[:, c, :], c == 0, c == 1,
                               (R_W, R_cqn), (R_pr,))
                        rope_out(96, pz, R_pz, pr, R_pr, tb[0:96, 4, :], tb[0:96, 5, :], R_tb, QTm[h, :, t0:t0 + 512])
                    for h in range(4):
                        pz, R_pz = bank.next()
                        pr, R_pr = bank.next()
                        mm(pz[0:96, :], Wkk[:, h, :], ckvn[:], True, False, (R_W, R_ckvn), (R_pz,))
                        for k in range(8):
                            mm(pz[0:96, :], Wkp[:, k, :], hT[:, k, :], False, k == 7, (R_W, R_hT), (R_pz,))
                        for k in range(8):
                            mm(pr[0:96, :], WkpR[:, k, :], hT[:, k, :], k == 0, k == 7, (R_W, R_hT), (R_pr,))
                        rope_out(96, pz, R_pz, pr, R_pr, tb[0:96, 4, :], tb[0:96, 5, :], R_tb, KTm[h, :, t0:t0 + 512])
                    for b in range(4):
                        tsl = slice(128 * b, 128 * b + 128)
                        r0 = t0 + 128 * b
                        vo, R_vo = vo_r.next()
                        wo, R_wo = wo_r.next()
                        p1, R_p1 = bank.next()
                        for k in range(8):
                            mm(p1[:, :], hT[:, k, tsl], Win[:, k, 1440:1952], k == 0, k == 7, (R_W, R_hT), (R_p1,))
                        cp("act", vo[:, 0:512], p1[:, :], (R_p1,), (R_vo,))
                        p2, R_p2 = bank.next()
                        for k in range(8):
                            mm(p2[:, 0:256], hT[:, k, tsl], Win[:, k, 2464:2720], k == 0, k == 7, (R_W, R_hT), (R_p2,))
                        cp("act", vo[:, 512:768], p2[:, 0:256], (R_p2,), (R_vo,))
                        p3, R_p3 = bank.next()
                        for k in range(8):
                            mm(p3[:, 0:8], hT[:, k, tsl], Win[:, k, 3008:3016], k == 0, k == 7, (R_W, R_hT), (R_p3,))
                        cp("act", wo[:], p3[:, 0:8], (R_p3,), (R_wo,))
                        p4, R_p4 = bank.next()
                        mm(p4[:, 0:256], ckvn[:, tsl], Wv[:], True, True, (R_W, R_ckvn), (R_p4,))
                        cp("act", vo[:, 768:1024], p4[:, 0:256], (R_p4,), (R_vo,))
                        dma(Vd[r0:r0 + 128, :], vo[:, 0:512], (R_vo,), (R_proj,))
                        dma(Vs[r0:r0 + 128, :], vo[:, 512:768], (R_vo,), (R_proj,))
                        dma(Vm[r0:r0 + 128, :], vo[:, 768:1024], (R_vo,), (R_proj,))
                        dma(WI[r0:r0 + 128, :], wo[:], (R_wo,), (R_proj,))
                SC.barrier()


        def phase_B():
            scale = 96 ** -0.5
            uid[0] += 1
            with ExitStack() as es:
                def sb(name, shape, dt):
                    return es.enter_context(nc.sbuf_tensor(name + "_B_%d" % uid[0], shape, dt))
                KT = sb("KT", [96, S], BF16)
                QT = sb("QT", [96, S], BF16)
                Vx = sb("Vx", [128, NB, 66], BF16)
                R_K, R_Q, R_V = Res("K"), Res("Q"), Res("V")
                pt_r = Ring([(sb("pt%d" % i, [128, 512], BF16), Res("pt%d" % i)) for i in range(3)])
                os_r = Ring([(sb("os%d" % i, [128, 512], F32), Res("os%d" % i)) for i in range(2)])
                rr_r = Ring([(sb("rr%d" % i, [64, 512], F32), Res("rr%d" % i)) for i in range(2)])
                ob_r = Ring([(sb("ob%d" % i, [64, 512], BF16), Res("ob%d" % i)) for i in range(2)])
                s_r = Ring(PS[0:4])
                od_r = Ring(PS[4:6])
                pd_r = Ring(PS[6:8])
                mset("pool", Vx[:, :, 64:66], 1.0, (R_V,))
                for h in range(4):
                    dma(KT[:], KTm[h, :, :], (R_proj,), (R_K,), q="sp")
                    dma(QT[:], QTm[h, :, :], (R_proj,), (R_Q,), q="sp")
                    dma(Vx[:, :, 0:64], Vm[:, 64 * h:64 * h + 64].rearrange("(j p) e -> p j e", p=128),
                        (R_proj,), (R_V,), q="sp")
                    for g in range(NT):
                        od, R_od = od_r.next()
                        nj = 4 * g + 4
                        for j in range(nj):
                            jj = j - 4 * g
                            c0 = 128 * jj if jj > 0 else 0
                            ps, R_ps = s_r.next()
                            mm(ps[:, c0:512], KT[:, 128 * j:128 * j + 128], QT[:, 512 * g + c0:512 * g + 512],
                               True, True, (R_K, R_Q), (R_ps,))
                            pt, R_pt = pt_r.next()
                            act(pt[:, c0:512], ps[:, c0:512], AF.Exp, (R_ps,), (R_pt,), scale=scale)
                            if jj >= 0:
                                tt("dve", pt[:, 128 * jj:128 * jj + 128], pt[:, 128 * jj:128 * jj + 128],
                                   band[:, 0:128], ALU.mult, (R_pt, R_const), (R_pt,))
                            mm(od[0:65, c0:512], Vx[:, j, 0:65], pt[:, c0:512], j == 0, j == nj - 1,
                               (R_V, R_pt), (R_od,))
                        osb, R_os = os_r.next()
                        cp("act", osb[0:65, :], od[0:65, :], (R_od,), (R_os,))
                        pd, R_pd = pd_r.next()
                        mm(pd[0:64, :], sel[0:65, :], osb[0:65, :], True, True, (R_const, R_os), (R_pd,))
                        rr, R_rr = rr_r.next()
                        SC.op("dve", lambda e, o=rr[:, :], i=pd[0:64, :]: e.reciprocal(out=o, in_=i), (R_pd,), (R_rr,))
                        ob, R_ob = ob_r.next()
                        tt("dve", ob[:, :], osb[0:64, :], rr[:, :], ALU.mult, (R_os, R_rr), (R_ob,))
                        dma(mixT[64 * h:64 * h + 64, 512 * g:512 * g + 512], ob[:, :], (R_ob,), (R_mix,))
                SC.barrier()

        def phase_C():
            scale = 64 ** -0.5
            uid[0] += 1
            with ExitStack() as es:
                def sb(name, shape, dt):
                    return es.enter_context(nc.sbuf_tensor(name + "_C_%d" % uid[0], shape, dt))
                KT = sb("KT", [128, S], BF16)
                QT = sb("QT", [128, S], BF16)
                R_K, R_Q = Res("K"), Res("Q")
                vx_r = Ring([(sb("Vx%d" % i, [128, NB, 66], BF16), Res("Vx%d" % i)) for i in range(2)])
                acc = sb("acc", [128, S], F32)
                R_acc = Res("acc")
                pt_r = Ring([(sb("pt%d" % i, [128, 256], BF16), Res("pt%d" % i)) for i in range(3)])
                rr_r = Ring([(sb("rr%d" % i, [64, 512], F32), Res("rr%d" % i)) for i in range(2)])
                ob_r = Ring([(sb("ob%d" % i, [64, 512], BF16), Res("ob%d" % i)) for i in range(2)])
                s_r = Ring(PS[0:4])
                od_b = PS[4:6]
                pd_r = Ring(PS[6:8])
                for (vx, R_vx) in vx_r.items:
                    mset("pool", vx[:, :, 64:66], 1.0, (R_vx,))
                for i in range(4):
                    dma(KT[:], KTd[i, :, :], (R_proj,), (R_K,), q="sp")
                    dma(QT[:], QTd[i, :, :], (R_proj,), (R_Q,), q="sp")
                    for hh in range(2):
                        h = 2 * i + hh
                        rows = slice(64 * hh, 64 * hh + 64)
                        for pi, d in enumerate((1, 4, 16)):
                            vx, R_vx = vx_r.next()
                            vsrc = Vd[:, 64 * h:64 * h + 64].rearrange("(n i d) e -> i n d e", i=128, d=d)
                            for r in range(d):
                                dma(vx[:, r:NB:d, 0:64], vsrc[:, :, r, :], (R_proj,), (R_vx,), q="sp")
                            nblk = S // (128 * d)
                            for r in range(d):
                                for m in range(nblk):
                                    blk = m * d + r
                                    ks = 128 * m * d + r
                                    nq = 256 if m + 1 < nblk else 128
                                    ps, R_ps = s_r.next()
                                    mm(ps[:, 0:nq], KT[rows, ks:ks + 127 * d + 1:d], QT[rows, ks:ks + (nq - 1) * d + 1:d],
                                       True, True, (R_K, R_Q), (R_ps,))
                                    pt, R_pt = pt_r.next()
                                    act(pt[:, 0:nq], ps[:, 0:nq], AF.Exp, (R_ps,), (R_pt,), scale=scale)
                                    tt("dve", pt[:, 0:nq], pt[:, 0:nq], band[:, 0:nq], ALU.mult, (R_pt, R_const), (R_pt,))
                                    od, R_od = od_b[m % 2]
                                    mm(od[0:65, 0:128], vx[:, blk, 0:65], pt[:, 0:128], m == 0, True,
                                       (R_vx, R_pt), (R_od,))
                                    dst = acc[0:65, ks:ks + 127 * d + 1:d]
                                    if pi == 0:
                                        cp("dve", dst, od[0:65, 0:128], (R_od,), (R_acc,))
                                    else:
                                        tt("dve", dst, dst, od[0:65, 0:128], ALU.add, (R_od, R_acc), (R_acc,))
                                    if m + 1 < nblk:
                                        od2, R_od2 = od_b[(m + 1) % 2]
                                        mm(od2[0:65, 0:128], vx[:, blk, 0:65], pt[:, 128:256], True, False,
                                           (R_vx, R_pt), (R_od2,))
                        for g in range(NT):
                            pd, R_pd = pd_r.next()
                            mm(pd[0:64, :], sel[0:65, :], acc[0:65, 512 * g:512 * g + 512], True, True,
                               (R_const, R_acc), (R_pd,))
                            rr, R_rr = rr_r.next()
                            SC.op("dve", lambda e, o=rr[:, :], i_=pd[0:64, :]: e.reciprocal(out=o, in_=i_), (R_pd,), (R_rr,))
                            ob, R_ob = ob_r.next()
                            tt("dve", ob[:, :], acc[0:64, 512 * g:512 * g + 512], rr[:, :], ALU.mult,
                               (R_acc, R_rr), (R_ob,))
                            dma(mixT[256 + 64 * h:256 + 64 * h + 64, 512 * g:512 * g + 512], ob[:, :], (R_ob,), (R_mix,))
                SC.barrier()

        def phase_D():
            scale = 64 ** -0.5
            uid[0] += 1
            with ExitStack() as es:
                def sb(name, shape, dt):
                    return es.enter_context(nc.sbuf_tensor(name + "_D_%d" % uid[0], shape, dt))
                KI = sb("KI", [128, S], BF16)
                KT = sb("KT", [128, 2, S], BF16)
                Vx = sb("Vx", [128, NB, 4, 66], BF16)
                WIt = sb("WIt", [128, NB, 8], F32)
                R_KI, R_K, R_V, R_WI = Res("KI"), Res("K"), Res("V"), Res("WI")
                Isc = sb("Isc", [128, S], F32)
                Msk = sb("Msk", [128, S], BF16)
                MskT = sb("MskT", [128, NB, 128], BF16)
                R_I, R_M, R_MT = Res("I"), Res("M"), Res("MT")
                qi_r = Ring([(sb("qi%d" % i, [96, 3, 128], BF16), Res("qi%d" % i)) for i in range(2)])
                qs_r = Ring([(sb("qs%d" % i, [128, 2, 128], BF16), Res("qs%d" % i)) for i in range(2)])
                rl_r = Ring([(sb("rl%d" % i, [128, 512], F32), Res("rl%d" % i)) for i in range(2)])
                pt_r = Ring([(sb("pt%d" % i, [128, 512], BF16), Res("pt%d" % i)) for i in range(3)])
                st_r = Ring([(sb("st%d" % i, [128, 8], F32), Res("st%d" % i)) for i in range(2)])
                wk_r = Ring([(sb("wk%d" % i, [128, NBIS + 1], F32), Res("wk%d" % i)) for i in range(2)])
                os_r = Ring([(sb("os%d" % i, [128, 128], F32), Res("os%d" % i)) for i in range(2)])
                rr_r = Ring([(sb("rr%d" % i, [64, 128], F32), Res("rr%d" % i)) for i in range(2)])
                ob_r = Ring([(sb("ob%d" % i, [64, 128], BF16), Res("ob%d" % i)) for i in range(2)])
                i_r = Ring(PS[0:3])
                t_b = PS[3]
                s_r = Ring(PS[4:6])
                od_b = PS[6]
                pd_b = PS[7]
                mset("pool", Vx[:, :, :, 64:66], 1.0, (R_V,))
                dma(KI[:], KIT[:, :], (R_proj,), (R_KI,), q="sp")
                for i in range(2):
                    dma(KT[:, i, :], KTs[i, :, :], (R_proj,), (R_K,), q="sp")
                for h in range(4):
                    dma(Vx[:, :, h, 0:64], Vs[:, 64 * h:64 * h + 64].rearrange("(j p) e -> p j e", p=128),
                        (R_proj,), (R_V,), q="sp")
                dma(WIt[:], WI.rearrange("(j p) h -> p j h", p=128), (R_proj,), (R_WI,), q="sp")
                qs_of = {}

                def idx_part(qb):
                    nk = 128 * (qb + 1)
                    nch = (nk + 511) // 512
                    qi, R_qi = qi_r.next()
                    qs, R_qs = qs_r.next()
                    dma(qi[:, 0:2, :], QIT[0:2, :, 128 * qb:128 * qb + 128].rearrange("i p t -> p i t"), (R_proj,), (R_qi,))
                    dma(qi[0:64, 2, :], QIT[2, 0:64, 128 * qb:128 * qb + 128], (R_proj,), (R_qi,))
                    dma(qs[:], QTs[:, :, 128 * qb:128 * qb + 128].rearrange("i p t -> p i t"), (R_proj,), (R_qs,))
                    qs_of[qb] = (qs, R_qs)
                    for c in range(nch):
                        ncol = min(512, nk - 512 * c)
                        cs = slice(512 * c, 512 * c + ncol)
                        for hh in range(8):
                            po = 32 * (hh % 3)
                            bi = hh // 3
                            ps, R_ps = i_r.next()
                            mm(ps[:, 0:ncol], qi[po:po + 32, bi, :], KI[po:po + 32, cs], True, True,
                               (R_qi, R_KI), (R_ps,))
                            rl, R_rl = rl_r.next()
                            act(rl[:, 0:ncol], ps[:, 0:ncol], AF.Relu, (R_ps,), (R_rl,))
                            if hh == 0:
                                ts("dve", Isc[:, cs], rl[:, 0:ncol], WIt[:, qb, 0:1], None, ALU.mult, None,
                                   (R_rl, R_WI), (R_I,))
                            else:
                                stt(Isc[:, cs], rl[:, 0:ncol], WIt[:, qb, hh:hh + 1], Isc[:, cs], ALU.mult, ALU.add,
                                    (R_rl, R_WI, R_I), (R_I,))
                    dg = slice(128 * qb, 128 * qb + 128)
                    tt("dve", Isc[:, dg], Isc[:, dg], negtri[:], ALU.add, (R_I, R_const), (R_I,))

                def bisect_part(qb):
                    nk = 128 * (qb + 1)
                    if nk > TOPK:
                        st, R_st = st_r.next()
                        RS = (R_st,)
                        SC.op("dve", lambda e, o=st[:, 0:1], i_=Isc[:, 0:nk - 128]: e.tensor_reduce(out=o, in_=i_, axis=AX.X, op=ALU.min),
                              (R_I,), RS)
                        SC.op("dve", lambda e, o=st[:, 1:2], i_=Isc[:, 0:nk]: e.tensor_reduce(out=o, in_=i_, axis=AX.X, op=ALU.max),
                              (R_I,), RS)
                        tt("dve", st[:, 1:2], st[:, 1:2], st[:, 0:1], ALU.subtract, RS, RS)
                        wk, R_wk = wk_r.next()
                        ts("dve", wk[:], pw2[:], st[:, 1:2], None, ALU.mult, None, (R_st, R_const), (R_wk,))
                        stt(st[:, 2:3], st[:, 1:2], 0.5, st[:, 0:1], ALU.mult, ALU.add, RS, RS)
                        for it in range(NBIS):
                            ts("dve", Msk[:, 0:nk], Isc[:, 0:nk], st[:, 2:3], 0.0, ALU.is_ge, ALU.add,
                               (R_I, R_st), (R_M, R_st), accum_out=st[:, 3:4])
                            ts("dve", st[:, 4:5], st[:, 3:4], TOPK - 0.5, 0.5, ALU.is_ge, ALU.subtract, RS, RS)
                            stt(st[:, 2:3], st[:, 4:5], wk[:, it:it + 1], st[:, 2:3], ALU.mult, ALU.add,
                                (R_st, R_wk), RS)
                        stt(st[:, 0:1], wk[:, NBIS:NBIS + 1], -1.0, st[:, 2:3], ALU.mult, ALU.add, (R_st, R_wk), RS)
                        ts("dve", Msk[:, 0:nk], Isc[:, 0:nk], st[:, 0:1], None, ALU.is_ge, None, (R_I, R_st), (R_M,))
                    else:
                        ts("dve", Msk[:, 0:nk], Isc[:, 0:nk], -1.0e29, None, ALU.is_ge, None, (R_I,), (R_M,))
                    tb_, R_tb_ = t_b
                    tbb = tb_[:, :].bitcast(BF16)
                    for j0 in range(0, qb + 1, 8):
                        n8 = min(8, qb + 1 - j0)
                        for jj in range(n8):
                            j = j0 + jj
                            tr(tbb[:, 128 * jj:128 * jj + 128], Msk[:, 128 * j:128 * j + 128], (R_M, R_const), (R_tb_,))
                        cp("act", MskT[:, j0:j0 + n8, :], tbb[:, 0:128 * n8].rearrange("p (j t) -> p j t", t=128),
                           (R_tb_,), (R_MT,))

                def attn_part(qb):
                    nk = 128 * (qb + 1)
                    nch = (nk + 511) // 512
                    qs, R_qs = qs_of.pop(qb)
                    for h in range(4):
                        rows = slice(64 * (h % 2), 64 * (h % 2) + 64)
                        bi = h // 2
                        od, R_od = od_b
                        for c in range(nch):
                            nbc = min(4, qb + 1 - 4 * c)
                            ps, R_ps = s_r.next()
                            for jj in range(nbc):
                                j = 4 * c + jj
                                mm(ps[:, 128 * jj:128 * jj + 128], KT[rows, bi, 128 * j:128 * j + 128], qs[rows, bi, :],
                                   True, True, (R_K, R_qs), (R_ps,))
                            pt, R_pt = pt_r.next()
                            act(pt[:, 0:128 * nbc], ps[:, 0:128 * nbc], AF.Exp, (R_ps,), (R_pt,), scale=scale)
                            tt("dve", pt[:, 0:128 * nbc], pt[:, 0:128 * nbc],
                               MskT[:, 4 * c:4 * c + nbc, :].rearrange("p j t -> p (j t)"), ALU.mult,
                               (R_pt, R_MT), (R_pt,))
                            for jj in range(nbc):
                                j = 4 * c + jj
                                mm(od[0:65, 0:128], Vx[:, j, h, 0:65], pt[:, 128 * jj:128 * jj + 128], j == 0, j == qb,
                                   (R_V, R_pt), (R_od,))
                        osb, R_os = os_r.next()
                        cp("act", osb[0:65, :], od[0:65, 0:128], (R_od,), (R_os,))
                        pd, R_pd = pd_b
                        mm(pd[0:64, 0:128], sel[0:65, :], osb[0:65, :], True, True, (R_const, R_os), (R_pd,))
                        rr, R_rr = rr_r.next()
                        SC.op("dve", lambda e, o=rr[:, :], i_=pd[0:64, 0:128]: e.reciprocal(out=o, in_=i_), (R_pd,), (R_rr,))
                        ob, R_ob = ob_r.next()
                        tt("dve", ob[:, :], osb[0:64, :], rr[:, :], ALU.mult, (R_os, R_rr), (R_ob,))
                        dma(mixT[768 + 64 * h:768 + 64 * h + 64, 128 * qb:128 * qb + 128], ob[:, :], (R_ob,), (R_mix,))

                for qb in range(NB):
                    idx_part(qb)
                    if qb > 0:
                        attn_part(qb - 1)
                    bisect_part(qb)
                attn_part(NB - 1)
                SC.barrier()

        def norm_T(es_rings, xt, R_xt, hT, R_hT, b, bank):
            junk, R_junk, hb_r, st_r = es_rings
            hb, R_hb = hb_r.next()
            stt_, R_st_ = st_r.next()
            act(junk[:], xt[:], AF.Square, (R_xt,), (R_junk, R_st_), accum_out=stt_[:, 0:1])
            act(stt_[:, 1:2], stt_[:, 0:1], AF.Sqrt, (R_st_,), (R_st_,), scale=1.0 / D, bias=EPS)
            SC.op("dve", lambda e, o=stt_[:, 2:3], i=stt_[:, 1:2]: e.reciprocal(out=o, in_=i), (R_st_,), (R_st_,))
            ts("dve", hb[:], xt[:], stt_[:, 2:3], None, ALU.mult, None, (R_xt, R_st_), (R_hb,))
            pt_, R_pt = bank.next()
            ptb = pt_[:, :].bitcast(BF16)
            for k in range(8):
                tr(ptb[:, 128 * k:128 * k + 128], hb[:, 128 * k:128 * k + 128], (R_hb, R_const), (R_pt,))
            cp("act", hT[:, :, 128 * b:128 * b + 128], ptb.rearrange("p (k t) -> p k t", t=128), (R_pt,), (R_hT,))

        def phase_E1(l, xsrc, R_xs):
            uid[0] += 1
            with ExitStack() as es:
                def sb(name, shape, dt):
                    return es.enter_context(nc.sbuf_tensor(name + "_E1_%d" % uid[0], shape, dt))
                Wo = sb("Wo", [128, 8, D], BF16)
                Wup = sb("Wup", [128, 8, 2 * DFF], BF16)
                R_W = Res("W_E1")
                stg = Ring([(sb("stg%d" % i, [128, 2048], F32), Res("stg%d" % i)) for i in range(2)])
                for k in range(8):
                    st, R_st = stg.next()
                    dma(st[:, 0:1024], w_o[l, 128 * k:128 * k + 128, :], (), (R_st,))
                    cp("act", Wo[:, k, :], st[:, 0:1024], (R_st,), (R_W,))
                    for (c0, cn) in ((0, 2048), (2048, 2048), (4096, 1536)):
                        st, R_st = stg.next()
                        dma(st[:, 0:cn], w_up[l, 128 * k:128 * k + 128, c0:c0 + cn], (), (R_st,))
                        ts("dve", Wup[:, k, c0:c0 + cn], st[:, 0:cn], gF[:, l * 8 + k:l * 8 + k + 1], None, ALU.mult, None,
                           (R_st, R_const), (R_W,))
                halo = sb("halo", [128, 44, 2], F32)
                R_halo = Res("halo")
                mset("pool", halo[:], 0.0, (R_halo,))
                mT_r = Ring([(sb("mT%d" % i, [128, 8, 512], BF16), Res("mT%d" % i)) for i in range(2)])
                hT_r = Ring([(sb("hT%d" % i, [128, 8, 512], BF16), Res("hT%d" % i)) for i in range(2)])
                xt_r = Ring([(sb("xt%d" % i, [128, D], F32), Res("xt%d" % i)) for i in range(2)])
                x1_r = Ring([(sb("x1%d" % i, [128, D], F32), Res("x1%d" % i)) for i in range(2)])
                junk = sb("junk", [128, D], BF16)
                R_junk = Res("junk")
                hb_r = Ring([(sb("hb%d" % i, [128, D], BF16), Res("hb%d" % i)) for i in range(2)])
                st_r = Ring([(sb("st%d" % i, [128, 4], F32), Res("st%d" % i)) for i in range(2)])
                ub_r = Ring([(sb("ub%d" % i, [128, 514], F32), Res("ub%d" % i)) for i in range(3)])
                y_r = Ring([(sb("y%d" % i, [128, 512], F32), Res("y%d" % i)) for i in range(4)])
                sg_r = Ring([(sb("sg%d" % i, [128, 512], F32), Res("sg%d" % i)) for i in range(2)])
                ab_r = Ring([(sb("ab%d" % i, [128, 512], BF16), Res("ab%d" % i)) for i in range(3)])
                bank = Ring(PS)
                rings = (junk, R_junk, hb_r, st_r)
                for ti in range(NT):
                    t0 = 512 * ti
                    mT, R_mT = mT_r.next()
                    hT, R_hT = hT_r.next()
                    dma(mT[:], mixT[:, t0:t0 + 512].rearrange("(k p) t -> p k t", p=128), (R_mix,), (R_mT,), q="sp")
                    for b in range(4):
                        r0 = t0 + 128 * b
                        xt, R_xt = xt_r.next()
                        x1, R_x1t = x1_r.next()
                        dma(xt[:], xsrc[r0:r0 + 128, :], (R_xs,), (R_xt,))
                        for hf in range(2):
                            ps, R_ps = bank.next()
                            for k in range(8):
                                mm(ps[:, :], mT[:, k, 128 * b:128 * b + 128], Wo[:, k, 512 * hf:512 * hf + 512],
                                   k == 0, k == 7, (R_mT, R_W), (R_ps,))
                            tt("dve", x1[:, 512 * hf:512 * hf + 512], ps[:, :], xt[:, 512 * hf:512 * hf + 512], ALU.add,
                               (R_ps, R_xt), (R_x1t,))
                        dma(xres1[r0:r0 + 128, :], x1[:], (R_x1t,), (R_x1,))
                        norm_T(rings, x1, R_x1t, hT, R_hT, b, bank)
                    for f in range(22):
                        ys = []
                        for fc in (f, 22 + f):
                            ps, R_ps = bank.next()
                            for k in range(8):
                                mm(ps[:, :], Wup[:, k, 128 * fc:128 * fc + 128], hT[:, k, :], k == 0, k == 7,
                                   (R_W, R_hT), (R_ps,))
                            ub, R_ub = ub_r.next()
                            cp("dve", ub[:, 0:2], halo[:, fc, :], (R_halo,), (R_ub,))
                            cp("act", ub[:, 2:514], ps[:, :], (R_ps,), (R_ub,))
                            cp("dve", halo[:, fc, :], ub[:, 512:514], (R_ub,), (R_halo,))
                            y, R_y_ = y_r.next()
                            ci = l * 132 + fc
                            ts("dve", y[:], ps[:, :], cw[:, ci + 88:ci + 89], cb[:, l * 44 + fc:l * 44 + fc + 1],
                               ALU.mult, ALU.add, (R_ps, R_const), (R_y_,))
                            stt(y[:], ub[:, 1:513], cw[:, ci + 44:ci + 45], y[:], ALU.mult, ALU.add, (R_ub, R_const, R_y_), (R_y_,))
                            stt(y[:], ub[:, 0:512], cw[:, ci:ci + 1], y[:], ALU.mult, ALU.add, (R_ub, R_const, R_y_), (R_y_,))
                            ys.append((y, R_y_))
                        sg, R_sg = sg_r.next()
                        act(sg[:], ys[0][0][:], AF.Silu, (ys[0][1],), (R_sg,))
                        ab, R_ab = ab_r.next()
                        tt("dve", ab[:], sg[:], ys[1][0][:], ALU.mult, (R_sg, ys[1][1]), (R_ab,))
                        dma(actT[128 * f:128 * f + 128, t0:t0 + 512], ab[:], (R_ab,), (R_act,))
                SC.barrier()

        def phase_E2(l, last):
            uid[0] += 1
            with ExitStack() as es:
                def sb(name, shape, dt):
                    return es.enter_context(nc.sbuf_tensor(name + "_E2_%d" % uid[0], shape, dt))
                Wdn = sb("Wdn", [128, 22, D], BF16)
                R_W = Res("W_E2")
                stg = Ring([(sb("stg%d" % i, [128, D], F32), Res("stg%d" % i)) for i in range(2)])
                for f in range(22):
                    st, R_st = stg.next()
                    dma(st[:], w_down[l, 128 * f:128 * f + 128, :], (), (R_st,))
                    cp("act" if f % 2 else "dve", Wdn[:, f, :], st[:], (R_st,), (R_W,))
                gfin = sb("gfin", [128, D], F32)
                R_gf = Res("gfin")
                if last:
                    dma(gfin[:], gfin_d[:, :], (), (R_gf,))
                aT_r = Ring([(sb("aT%d" % i, [128, 22, 512], BF16), Res("aT%d" % i)) for i in range(2)])
                xt_r = Ring([(sb("xt%d" % i, [128, D], F32), Res("xt%d" % i)) for i in range(2)])
                x2_r = Ring([(sb("x2%d" % i, [128, D], F32), Res("x2%d" % i)) for i in range(2)])
                yo_r = Ring([(sb("yo%d" % i, [128, D], F32), Res("yo%d" % i)) for i in range(2)])
                junk = sb("junk", [128, D], BF16)
                R_junk = Res("junk")
                st_r = Ring([(sb("st%d" % i, [128, 4], F32), Res("st%d" % i)) for i in range(2)])
                bank = Ring(PS)
                for ti in range(NT):
                    t0 = 512 * ti
                    aT, R_aT = aT_r.next()
                    dma(aT[:], actT[:, t0:t0 + 512].rearrange("(f p) t -> p f t", p=128), (R_act,), (R_aT,), q="sp")
                    for b in range(4):
                        r0 = t0 + 128 * b
                        xt, R_xt = xt_r.next()
                        x2, R_x2 = x2_r.next()
                        dma(xt[:], xres1[r0:r0 + 128, :], (R_x1,), (R_xt,))
                        for hf in range(2):
                            ps, R_ps = bank.next()
                            for f in range(22):
                                mm(ps[:, :], aT[:, f, 128 * b:128 * b + 128], Wdn[:, f, 512 * hf:512 * hf + 512],
                                   f == 0, f == 21, (R_aT, R_W), (R_ps,))
                            tt("dve", x2[:, 512 * hf:512 * hf + 512], ps[:, :], xt[:, 512 * hf:512 * hf + 512], ALU.add,
                               (R_ps, R_xt), (R_x2,))
                        if not last:
                            dma(xres0[r0:r0 + 128, :], x2[:], (R_x2,), (R_x,))
                        else:
                            stt_, R_st_ = st_r.next()
                            yo, R_yo = yo_r.next()
                            act(junk[:], x2[:], AF.Square, (R_x2,), (R_junk, R_st_), accum_out=stt_[:, 0:1])
                            act(stt_[:, 1:2], stt_[:, 0:1], AF.Sqrt, (R_st_,), (R_st_,), scale=1.0 / D, bias=EPS)
                            SC.op("dve", lambda e, o=stt_[:, 2:3], i=stt_[:, 1:2]: e.reciprocal(out=o, in_=i), (R_st_,), (R_st_,))
                            stt(yo[:], x2[:], stt_[:, 2:3], gfin[:], ALU.mult, ALU.mult, (R_x2, R_st_, R_gf), (R_yo,))
                            dma(y_out[r0:r0 + 128, :], yo[:], (R_yo,), (R_y,))
                SC.barrier()

        for l in range(L):
            xsrc = x_in if l == 0 else xres0
            phase_A(l, xsrc)
            if stop_after == "A":
                break
            if "B" not in skip:
                phase_B()
            if stop_after == "B":
                break
            if "C" not in skip:
                phase_C()
            if stop_after == "C":
                break
            if "D" not in skip:
                phase_D()
            if stop_after == "D":
                break
            phase_E1(l, xsrc, R_x)
            if stop_after == "E1":
                break
            phase_E2(l, l == L - 1)
        SC.barrier()
        print("instructions:", SC.ninst)
    return nc


def make_consts(S):
    bf = ml_dtypes.bfloat16
    c = {}
    c["c_ident"] = np.eye(128, dtype=np.float32).astype(bf)
    c["c_ones"] = np.ones((128, 128), np.float32).astype(bf)
    sel = np.zeros((128, 64), np.float32)
    sel[64, :] = 1.0
    c["c_sel"] = sel
    k = np.arange(128)[:, None]
    q = np.arange(128)[None, :]
    c["c_band"] = np.concatenate([(k <= q), (k >= q)], axis=1).astype(np.float32).astype(bf)
    c["c_negtri"] = np.where(q.T >= k.T, 0.0, NEG).astype(np.float32) if False else \
        np.where(np.arange(128)[None, :] <= np.arange(128)[:, None], 0.0, NEG).astype(np.float32)
    c["c_pw2"] = np.ascontiguousarray(np.broadcast_to(
        (2.0 ** -(np.arange(NBIS + 1, dtype=np.float32) + 1.0)).astype(np.float32)[None, :], (128, NBIS + 1)))
    t = np.arange(S, dtype=np.float32)

    def tables(dim):
        inv = np.power(np.float32(500000.0), -np.arange(0, dim, 2, dtype=np.float32) / np.float32(dim)).astype(np.float32)
        ang = (t[:, None] * inv[None, :]).astype(np.float32)
        return np.cos(ang).astype(np.float32).T, np.sin(ang).astype(np.float32).T
    ch, sh = tables(16)
    ci, si = tables(8)
    ca, sa = tables(32)
    C = np.ones((128, S), np.float32)
    Sn = np.zeros((128, S), np.float32)
    for hh in range(2):
        C[64 * hh:64 * hh + 8] = ch
        C[64 * hh + 8:64 * hh + 16] = ch
        Sn[64 * hh:64 * hh + 8] = sh
        Sn[64 * hh + 8:64 * hh + 16] = sh
    c["tab64"] = np.stack([C, Sn])
    C = np.ones((128, S), np.float32)
    Sn = np.zeros((128, S), np.float32)
    for hh in range(4):
        C[32 * hh:32 * hh + 4] = ci
        C[32 * hh + 4:32 * hh + 8] = ci
        Sn[32 * hh:32 * hh + 4] = si
        Sn[32 * hh + 4:32 * hh + 8] = si
    c["tabidx"] = np.stack([C, Sn])
    C = np.ones((96, S), np.float32)
    Sn = np.zeros((96, S), np.float32)
    C[64:80] = ca
    C[80:96] = ca
    Sn[64:80] = sa
    Sn[80:96] = sa
    c["tabmla"] = np.stack([C, Sn])
    return c


def layout_params(inp, L):
    f = np.float32
    o = {}
    o["gA"] = np.ascontiguousarray(np.asarray(inp["g_attn"], f).reshape(L, 8, 128).transpose(2, 0, 1).reshape(128, L * 8))
    o["gF"] = np.ascontiguousarray(np.asarray(inp["g_ffn"], f).reshape(L, 8, 128).transpose(2, 0, 1).reshape(128, L * 8))
    o["gq"] = np.ascontiguousarray(np.asarray(inp["g_q_lat"], f).reshape(L, 2, 128).transpose(2, 0, 1).reshape(128, L * 2))
    o["gkv"] = np.ascontiguousarray(np.asarray(inp["g_kv_lat"], f).reshape(L, 128).T)
    o["cw"] = np.ascontiguousarray(np.asarray(inp["conv_w"], f).reshape(L, 3, 44, 128).transpose(3, 0, 1, 2).reshape(128, L * 3 * 44))
    o["cb"] = np.ascontiguousarray(np.asarray(inp["conv_b"], f).reshape(L, 44, 128).transpose(2, 0, 1).reshape(128, L * 44))
    o["gfin"] = np.ascontiguousarray(np.broadcast_to(np.asarray(inp["g_final"], f).reshape(1, D), (128, D)))
    for k in ("w_in", "w_uq", "w_ukv", "w_o", "w_up", "w_down"):
        o[k] = np.ascontiguousarray(np.asarray(inp[k], f))
    return o


_CACHE = {}


def kernel(**inputs):
    x = np.asarray(inputs["x"], np.float32)
    B, S, _ = x.shape
    L = inputs["w_in"].shape[0]
    key = (S, L)
    if key not in _CACHE:
        _CACHE[key] = (build(S, L), make_consts(S))
    nc, consts = _CACHE[key]
    shared = layout_params(inputs, L)
    shared.update(consts)
    n = 8
    in_maps = []
    for c in range(n):
        m = dict(shared)
        m["x"] = np.ascontiguousarray(x[c % B])
        in_maps.append(m)
    res = run_bass_kernel_spmd(nc, in_maps, core_ids=list(range(n)))
    return np.stack([res.results[b]["y"] for b in range(B)], axis=0).astype(np.float32)
```

```python
import numpy as np
import ml_dtypes
from contextlib import ExitStack
import concourse.bass as bass
import concourse.mybir as mybir
from concourse.bass_utils import run_bass_kernel_spmd

F32 = mybir.dt.float32
BF16 = mybir.dt.bfloat16
AF = mybir.ActivationFunctionType
ALU = mybir.AluOpType
AX = mybir.AxisListType

EPS = 1e-6
NEG = -1.0e30
D = 1024
NIN = 3016
DFF = 2816
TOPK = 256
NBIS = 17


class Res:
    __slots__ = ("name", "w", "r", "ordered")

    def __init__(self, name, ordered=True):
        self.name = name
        self.w = {}
        self.r = {}
        self.ordered = ordered


class Eng:
    def __init__(self, name, eng, sem):
        self.name = name
        self.eng = eng
        self.sem = sem
        self.n = 0
        self.waited = {}
        self.dsem = []
        self.dval = []
        self.di = 0


class Sched:
    K = 12

    def __init__(self, nc, es):
        self.nc = nc
        self.E = {}
        for name, e in (("pe", nc.tensor), ("act", nc.scalar), ("dve", nc.vector),
                        ("pool", nc.gpsimd), ("sp", nc.sync)):
            self.E[name] = Eng(name, e, es.enter_context(nc.semaphore("p_" + name)))
        for q in ("sp", "pool", "act"):
            E = self.E[q]
            for i in range(self.K):
                E.dsem.append(es.enter_context(nc.semaphore("d_%s%d" % (q, i))))
                E.dval.append(0)
        self.ninst = 0

    def _need(self, reads, writes, own=None):
        need = {}

        def add(d, skip_own):
            for k, sv in d.items():
                if skip_own and sv[0] is own:
                    continue
                if k not in need or need[k][1] < sv[1]:
                    need[k] = sv
        for r in reads:
            add(r.w, False)
        for w in writes:
            add(w.r, True)
            if w.ordered:
                add(w.w, True)
        return need

    def _emit_waits(self, E, need, is_dma):
        for k, (s, v) in need.items():
            if s is E.sem and not is_dma and E.name == "pe":
                continue
            if E.waited.get(k, 0) >= v:
                continue
            E.eng.wait_ge(s, v)
            E.waited[k] = v

    def _mark(self, tok, reads, writes):
        k = id(tok[0])
        for r in reads:
            if r.r.get(k, (None, 0))[1] < tok[1]:
                r.r[k] = tok
        for w in writes:
            if w.ordered:
                w.w = {k: tok}
                w.r = {}
            else:
                w.w[k] = tok

    def op(self, eng, fn, reads=(), writes=()):
        E = self.E[eng]
        self._emit_waits(E, self._need(reads, writes, own=E.sem), False)
        inst = fn(E.eng)
        E.n += 1
        inst.then_inc(E.sem, 1)
        self._mark((E.sem, E.n), reads, writes)
        self.ninst += 1

    def dma(self, q, out, in_, reads=(), writes=()):
        E = self.E[q]
        self._emit_waits(E, self._need(reads, writes), True)
        i = E.di % self.K
        E.di += 1
        s = E.dsem[i]
        pv = E.dval[i]
        if pv > 0 and E.waited.get(id(s), 0) < pv:
            E.eng.wait_ge(s, pv)
            E.waited[id(s)] = pv
        E.eng.dma_start(out=out, in_=in_).then_inc(s, 16)
        E.dval[i] = pv + 16
        self._mark((s, pv + 16), reads, writes)
        self.ninst += 1

    def barrier(self):
        for E in self.E.values():
            for O in self.E.values():
                if O is not E and O.n > 0 and E.waited.get(id(O.sem), 0) < O.n:
                    E.eng.wait_ge(O.sem, O.n)
                    E.waited[id(O.sem)] = O.n
                for s, v in zip(O.dsem, O.dval):
                    if v > 0 and E.waited.get(id(s), 0) < v:
                        E.eng.wait_ge(s, v)
                        E.waited[id(s)] = v


class Ring:
    def __init__(self, items):
        self.items = items
        self.i = 0

    def next(self):
        it = self.items[self.i % len(self.items)]
        self.i += 1
        return it


def build(S=8192, L=4, dbg=False, stop_after=None, skip=()):
    NT = S // 512
    NB = S // 128
    nc = bass.Bass("TRN2", target_bir_lowering=False)

    def din(name, shape, dt=F32):
        return nc.dram_tensor(name, list(shape), dt, kind="ExternalInput").ap()

    def dscr(name, shape, dt):
        return nc.dram_tensor(name, list(shape), dt, kind=("ExternalOutput" if dbg else "Internal")).ap()

    x_in = din("x", [S, D])
    w_in = din("w_in", [L, D, NIN])
    w_uq = din("w_uq", [L, 256, 384])
    w_ukv = din("w_ukv", [L, 128, 512])
    w_o = din("w_o", [L, D, D])
    w_up = din("w_up", [L, D, 2 * DFF])
    w_down = din("w_down", [L, DFF, D])
    gA_d = din("gA", [128, L * 8])
    gF_d = din("gF", [128, L * 8])
    gq_d = din("gq", [128, L * 2])
    gkv_d = din("gkv", [128, L])
    cw_d = din("cw", [128, L * 3 * 44])
    cb_d = din("cb", [128, L * 44])
    gfin_d = din("gfin", [128, D])
    c_ident = din("c_ident", [128, 128], BF16)
    c_ones = din("c_ones", [128, 128], BF16)
    c_sel = din("c_sel", [128, 64])
    c_band = din("c_band", [128, 256], BF16)
    c_negtri = din("c_negtri", [128, 128])
    c_pw2 = din("c_pw2", [128, NBIS + 1])
    tab64 = din("tab64", [2, 128, S])
    tabidx = din("tabidx", [2, 128, S])
    tabmla = din("tabmla", [2, 96, S])
    y_out = nc.dram_tensor("y", [S, D], F32, kind="ExternalOutput").ap()

    xres0 = dscr("xres0", [S, D], F32)
    xres1 = dscr("xres1", [S, D], F32)
    QTm = dscr("QTm", [4, 96, S], BF16)
    KTm = dscr("KTm", [4, 96, S], BF16)
    Vm = dscr("Vm", [S, 256], BF16)
    QTd = dscr("QTd", [4, 128, S], BF16)
    KTd = dscr("KTd", [4, 128, S], BF16)
    Vd = dscr("Vd", [S, 512], BF16)
    QTs = dscr("QTs", [2, 128, S], BF16)
    KTs = dscr("KTs", [2, 128, S], BF16)
    Vs = dscr("Vs", [S, 256], BF16)
    QIT = dscr("QIT", [3, 96, S], BF16)
    KIT = dscr("KIT", [128, S], BF16)
    WI = dscr("WI", [S, 8], F32)
    mixT = dscr("mixT", [D, S], BF16)
    actT = dscr("actT", [DFF, S], BF16)

    ges = ExitStack()
    with ges:
        SC = Sched(nc, ges)
        R_x = Res("xres0", ordered=False)
        R_x1 = Res("xres1", ordered=False)
        R_proj = Res("proj", ordered=False)
        R_mix = Res("mixT", ordered=False)
        R_act = Res("actT", ordered=False)
        R_y = Res("y", ordered=False)
        R_const = Res("const")
        DRAM_RES = (R_x, R_x1, R_proj, R_mix, R_act, R_y)

        def gsb(name, shape, dt):
            return ges.enter_context(nc.sbuf_tensor("g_" + name, shape, dt))

        PS = []
        for i in range(8):
            t = ges.enter_context(nc.psum_tensor("ps%d" % i, [128, 512], F32))
            PS.append((t, Res("ps%d" % i)))

        gA = gsb("gA", [128, L * 8], F32)
        gF = gsb("gF", [128, L * 8], F32)
        gq = gsb("gq", [128, L * 2], F32)
        gkv = gsb("gkv", [128, L], F32)
        cw = gsb("cw", [128, L * 3 * 44], F32)
        cb = gsb("cb", [128, L * 44], F32)
        ident = gsb("ident", [128, 128], BF16)
        ones = gsb("ones", [128, 128], BF16)
        sel = gsb("sel", [128, 64], F32)
        band = gsb("band", [128, 256], BF16)
        negtri = gsb("negtri", [128, 128], F32)
        pw2 = gsb("pw2", [128, NBIS + 1], F32)
        for dst, src in ((gA, gA_d), (gF, gF_d), (gq, gq_d), (gkv, gkv_d), (cw, cw_d), (cb, cb_d),
                         (ident, c_ident), (ones, c_ones), (sel, c_sel), (band, c_band), (negtri, c_negtri), (pw2, c_pw2)):
            SC.dma("sp", dst[:], src[:, :], reads=(), writes=(R_const,))
        R_const.ordered = False

        def mm(out, lhsT, rhs, start, stop, reads, writes):
            SC.op("pe", lambda e: e.matmul(out, lhsT=lhsT, rhs=rhs, start=start, stop=stop), reads, writes)

        def tr(out, in_, reads, writes):
            SC.op("pe", lambda e: e.transpose(out, in_, ident[:]), reads, writes)

        def act(out, in_, func, reads, writes, scale=1.0, bias=None, accum_out=None):
            kw = {}
            if bias is not None:
                kw["bias"] = bias
            if accum_out is not None:
                kw["accum_out"] = accum_out
            SC.op("act", lambda e: e.activation(out=out, in_=in_, func=func, scale=scale, **kw), reads, writes)

        def ts(eng, out, in0, s1, s2, op0, op1, reads, writes, accum_out=None):
            kw = {}
            if accum_out is not None:
                kw["accum_out"] = accum_out
            if op1 is None:
                SC.op(eng, lambda e: e.tensor_scalar(out=out, in0=in0, scalar1=s1, scalar2=None, op0=op0, **kw),
                      reads, writes)
            else:
                SC.op(eng, lambda e: e.tensor_scalar(out=out, in0=in0, scalar1=s1, scalar2=s2, op0=op0, op1=op1, **kw),
                      reads, writes)

        def tt(eng, out, in0, in1, op, reads, writes):
            SC.op(eng, lambda e: e.tensor_tensor(out=out, in0=in0, in1=in1, op=op), reads, writes)

        def stt(out, in0, scalar, in1, op0, op1, reads, writes):
            SC.op("dve", lambda e: e.scalar_tensor_tensor(out=out, in0=in0, scalar=scalar, in1=in1, op0=op0, op1=op1),
                  reads, writes)

        def cp(eng, out, in_, reads, writes):
            if eng == "act":
                SC.op("act", lambda e: e.activation(out=out, in_=in_, func=AF.Copy), reads, writes)
            else:
                SC.op(eng, lambda e: e.tensor_copy(out=out, in_=in_), reads, writes)

        def mset(eng, ap, val, writes):
            SC.op(eng, lambda e: e.memset(ap, val), (), writes)

        uid = [0]
        dq = ["sp", "pool"]
        dqi = [0]

        def dma(out, in_, reads, writes, q=None):
            if q is None:
                q = "pool" if any(w in DRAM_RES for w in writes) else "sp"
            SC.dma(q, out, in_, reads, writes)

        def rms_rows(es, xt, R_xt, hb, R_hb, name):
            pass

        def phase_A(l, xsrc):
            uid[0] += 1
            with ExitStack() as es:
                def sb(name, shape, dt):
                    return es.enter_context(nc.sbuf_tensor(name + "_A_%d" % uid[0], shape, dt))
                Win = sb("Win", [128, 8, NIN], BF16)
                WinR = sb("WinR", [128, 8, 1920], BF16)
                Wki = sb("Wki", [128, 8, 128], BF16)
                Wkp = sb("Wkp", [128, 8, 96], BF16)
                WkpR = sb("WkpR", [128, 8, 96], BF16)
                Wuq = sb("Wuq", [128, 2, 384], BF16)
                WuqR = sb("WuqR", [128, 2, 384], BF16)
                Wukv = sb("Wukv", [128, 512], BF16)
                Wkk = sb("Wkk", [128, 4, 96], BF16)
                Wv = sb("Wv", [128, 256], BF16)
                R_W = Res("W_A")
                stg = Ring([(sb("stg%d" % i, [128, 1508], F32), Res("stg%d" % i)) for i in range(2)])
                for k in range(8):
                    for hf in range(2):
                        st, R_st = stg.next()
                        dma(st[:], w_in[l, 128 * k:128 * k + 128, 1508 * hf:1508 * hf + 1508], (), (R_st,))
                        ts("dve", Win[:, k, 1508 * hf:1508 * hf + 1508], st[:], gA[:, l * 8 + k:l * 8 + k + 1], None,
                           ALU.mult, None, (R_st, R_const), (R_W,))
                for c in range(2):
                    st, R_st = stg.next()
                    dma(st[:, 0:384], w_uq[l, 128 * c:128 * c + 128, :], (), (R_st,))
                    ts("dve", Wuq[:, c, :], st[:, 0:384], gq[:, l * 2 + c:l * 2 + c + 1], None, ALU.mult, None,
                       (R_st, R_const), (R_W,))
                st, R_st = stg.next()
                dma(st[:, 0:512], w_ukv[l, :, :], (), (R_st,))
                ts("dve", Wukv[:], st[:, 0:512], gkv[:, l:l + 1], None, ALU.mult, None, (R_st, R_const), (R_W,))
                mset("pool", WinR[:], 0.0, (R_W,))
                mset("pool", Wkp[:], 0.0, (R_W,))
                mset("pool", WkpR[:], 0.0, (R_W,))
                mset("pool", WuqR[:], 0.0, (R_W,))
                mset("pool", Wkk[:], 0.0, (R_W,))
                RW = (R_W,)
                for k in range(8):
                    for base, nh, rb in ((416, 8, 0), (928, 8, 512), (1952, 4, 1024), (2208, 4, 1280)):
                        src = Win[:, k, base:base + nh * 64].rearrange("p (h e) -> p h e", e=64)
                        dst = WinR[:, k, rb:rb + nh * 64].rearrange("p (h e) -> p h e", e=64)
                        ts("dve", dst[:, :, 0:8], src[:, :, 8:16], -1.0, None, ALU.mult, None, RW, RW)
                        cp("dve", dst[:, :, 8:16], src[:, :, 0:8], RW, RW)
                    src = Win[:, k, 2720:2976].rearrange("p (h e) -> p h e", e=32)
                    dst = WinR[:, k, 1536:1792].rearrange("p (h e) -> p h e", e=32)
                    ts("dve", dst[:, :, 0:4], src[:, :, 4:8], -1.0, None, ALU.mult, None, RW, RW)
                    cp("dve", dst[:, :, 4:8], src[:, :, 0:4], RW, RW)
                    for r in range(4):
                        cp("dve", Wki[:, k, 32 * r:32 * r + 32], Win[:, k, 2976:3008], RW, RW)
                        ts("dve", WinR[:, k, 1792 + 32 * r:1792 + 32 * r + 4], Win[:, k, 2980:2984], -1.0, None,
                           ALU.mult, None, RW, RW)
                        cp("dve", WinR[:, k, 1792 + 32 * r + 4:1792 + 32 * r + 8], Win[:, k, 2976:2980], RW, RW)
                    cp("dve", Wkp[:, k, 64:96], Win[:, k, 384:416], RW, RW)
                    ts("dve", WkpR[:, k, 64:80], Win[:, k, 400:416], -1.0, None, ALU.mult, None, RW, RW)
                    cp("dve", WkpR[:, k, 80:96], Win[:, k, 384:400], RW, RW)
                for c in range(2):
                    src = Wuq[:, c, :].rearrange("p (h e) -> p h e", e=96)
                    dst = WuqR[:, c, :].rearrange("p (h e) -> p h e", e=96)
                    ts("dve", dst[:, :, 64:80], src[:, :, 80:96], -1.0, None, ALU.mult, None, RW, RW)
                    cp("dve", dst[:, :, 80:96], src[:, :, 64:80], RW, RW)
                ukv = Wukv[:].rearrange("p (h t e) -> p h t e", t=2, e=64)
                cp("dve", Wkk[:, :, 0:64], ukv[:, :, 0, :], RW, RW)
                cp("dve", Wv[:].rearrange("p (h e) -> p h e", e=64), ukv[:, :, 1, :], RW, RW)

                xt_r = Ring([(sb("xt%d" % i, [128, D], F32), Res("xt%d" % i)) for i in range(2)])
                junk = sb("junk", [128, D], BF16)
                R_junk = Res("junk")
                hb_r = Ring([(sb("hb%d" % i, [128, D], BF16), Res("hb%d" % i)) for i in range(2)])
                st_r = Ring([(sb("st%d" % i, [128, 4], F32), Res("st%d" % i)) for i in range(2)])
                hT_r = Ring([(sb("hT%d" % i, [128, 8, 512], BF16), Res("hT%d" % i)) for i in range(2)])
                tb_r = Ring([(sb("tb%d" % i, [128, 6, 512], F32), Res("tb%d" % i)) for i in range(2)])
                t1_r = Ring([(sb("t1_%d" % i, [128, 512], F32), Res("t1_%d" % i)) for i in range(2)])
                t2_r = Ring([(sb("t2_%d" % i, [128, 512], F32), Res("t2_%d" % i)) for i in range(2)])
                ob_r = Ring([(sb("ob%d" % i, [128, 512], BF16), Res("ob%d" % i)) for i in range(4)])
                sq_r = Ring([(sb("sq%d" % i, [128, 512], BF16), Res("sq%d" % i)) for i in range(2)])
                rs_r = Ring([(sb("rs%d" % i, [128, 512], F32), Res("rs%d" % i)) for i in range(2)])
                cqn = sb("cqn", [128, 2, 512], BF16)
                R_cqn = Res("cqn")
                ckvn = sb("ckvn", [128, 512], BF16)
                R_ckvn = Res("ckvn")
                vo_r = Ring([(sb("vo%d" % i, [128, 1024], BF16), Res("vo%d" % i)) for i in range(2)])
                wo_r = Ring([(sb("wo%d" % i, [128, 8], F32), Res("wo%d" % i)) for i in range(2)])
                bank = Ring(PS)

                def rope_out(rows, pz, R_pz, pr, R_pr, tC, tS, R_tb, dst_ap):
                    t1, R_t1 = t1_r.next()
                    t2, R_t2 = t2_r.next()
                    ob, R_ob = ob_r.next()
                    tt("dve", t1[0:rows, :], pz[0:rows, :], tC, ALU.mult, (R_pz, R_tb), (R_t1,))
                    tt("dve", t2[0:rows, :], pr[0:rows, :], tS, ALU.mult, (R_pr, R_tb), (R_t2,))
                    tt("pool", ob[0:rows, :], t1[0:rows, :], t2[0:rows, :], ALU.add, (R_t1, R_t2), (R_ob,))
                    dma(dst_ap, ob[0:rows, :], (R_ob,), (R_proj,))

                for ti in range(NT):
                    t0 = 512 * ti
                    hT, R_hT = hT_r.next()
                    for b in range(4):
                        xt, R_xt = xt_r.next()
                        hb, R_hb = hb_r.next()
                        stt_, R_st_ = st_r.next()
                        dma(xt[:], xsrc[t0 + 128 * b:t0 + 128 * b + 128, :], (R_x,), (R_xt,))
                        act(junk[:], xt[:], AF.Square, (R_xt,), (R_junk, R_st_), accum_out=stt_[:, 0:1])
                        act(stt_[:, 1:2], stt_[:, 0:1], AF.Sqrt, (R_st_,), (R_st_,), scale=1.0 / D, bias=EPS)
                        SC.op("dve", lambda e, o=stt_[:, 2:3], i=stt_[:, 1:2]: e.reciprocal(out=o, in_=i), (R_st_,), (R_st_,))
                        ts("dve", hb[:], xt[:], stt_[:, 2:3], None, ALU.mult, None, (R_xt, R_st_), (R_hb,))
                        pt_, R_pt = bank.next()
                        ptb = pt_[:, :].bitcast(BF16)
                        for k in range(8):
                            tr(ptb[:, 128 * k:128 * k + 128], hb[:, 128 * k:128 * k + 128], (R_hb, R_const), (R_pt,))
                        cp("act", hT[:, :, 128 * b:128 * b + 128], ptb.rearrange("p (k t) -> p k t", t=128),
                           (R_pt,), (R_hT,))
                    tb, R_tb = tb_r.next()
                    dma(tb[:, 0, :], tab64[0, :, t0:t0 + 512], (), (R_tb,))
                    dma(tb[:, 1, :], tab64[1, :, t0:t0 + 512], (), (R_tb,))
                    dma(tb[:, 2, :], tabidx[0, :, t0:t0 + 512], (), (R_tb,))
                    dma(tb[:, 3, :], tabidx[1, :, t0:t0 + 512], (), (R_tb,))
                    dma(tb[0:96, 4, :], tabmla[0, :, t0:t0 + 512], (), (R_tb,))
                    dma(tb[0:96, 5, :], tabmla[1, :, t0:t0 + 512], (), (R_tb,))
                    blocks = []
                    for i in range(4):
                        blocks.append((Win, 416 + 128 * i, WinR, 128 * i, 0, QTd[i, :, t0:t0 + 512], 128))
                    for i in range(4):
                        blocks.append((Win, 928 + 128 * i, WinR, 512 + 128 * i, 0, KTd[i, :, t0:t0 + 512], 128))
                    for i in range(2):
                        blocks.append((Win, 1952 + 128 * i, WinR, 1024 + 128 * i, 0, QTs[i, :, t0:t0 + 512], 128))
                    for i in range(2):
                        blocks.append((Win, 2208 + 128 * i, WinR, 1280 + 128 * i, 0, KTs[i, :, t0:t0 + 512], 128))
                    for i in range(3):
                        nr = 96 if i < 2 else 64
                        blocks.append((Win, 2720 + 96 * i, WinR, 1536 + 96 * i, 2, QIT[i, 0:nr, t0:t0 + 512], nr))
                    blocks.append((Wki, 0, WinR, 1792, 2, KIT[:, t0:t0 + 512], 128))
                    for (Wz, cz, Wr, cr, tbi, dst, nr) in blocks:
                        pz, R_pz = bank.next()
                        pr, R_pr = bank.next()
                        for k in range(8):
                            mm(pz[0:nr, :], Wz[:, k, cz:cz + nr], hT[:, k, :], k == 0, k == 7, (R_W, R_hT), (R_pz,))
                        for k in range(8):
                            mm(pr[0:nr, :], Wr[:, k, cr:cr + nr], hT[:, k, :], k == 0, k == 7, (R_W, R_hT), (R_pr,))
                        rope_out(nr, pz, R_pz, pr, R_pr, tb[0:nr, tbi, :], tb[0:nr, tbi + 1, :], R_tb, dst)
                    pq0, R_pq0 = bank.next()
                    pq1, R_pq1 = bank.next()
                    pkv, R_pkv = bank.next()
                    for (pp, R_pp, c0) in ((pq0, R_pq0, 0), (pq1, R_pq1, 128), (pkv, R_pkv, 256)):
                        for k in range(8):
                            mm(pp[:, :], Win[:, k, c0:c0 + 128], hT[:, k, :], k == 0, k == 7, (R_W, R_hT), (R_pp,))
                    pss, R_pss = bank.next()
                    sqs = []
                    for (pp, R_pp) in ((pq0, R_pq0), (pq1, R_pq1)):
                        sq, R_sq = sq_r.next()
                        act(sq[:], pp[:, :], AF.Square, (R_pp,), (R_sq,))
                        sqs.append((sq, R_sq))
                    for i, (sq, R_sq) in enumerate(sqs):
                        mm(pss[:, :], ones[:], sq[:], i == 0, i == 1, (R_const, R_sq), (R_pss,))
                    rs, R_rs = rs_r.next()
                    act(rs[:], pss[:, :], AF.Sqrt, (R_pss,), (R_rs,), scale=1.0 / 256, bias=EPS)
                    SC.op("dve", lambda e, o=rs[:], i=rs[:]: e.reciprocal(out=o, in_=i), (R_rs,), (R_rs,))
                    tt("dve", cqn[:, 0, :], pq0[:, :], rs[:], ALU.mult, (R_pq0, R_rs), (R_cqn,))
                    tt("dve", cqn[:, 1, :], pq1[:, :], rs[:], ALU.mult, (R_pq1, R_rs), (R_cqn,))
                    pss, R_pss = bank.next()
                    sq, R_sq = sq_r.next()
                    act(sq[:], pkv[:, :], AF.Square, (R_pkv,), (R_sq,))
                    mm(pss[:, :], ones[:], sq[:], True, True, (R_const, R_sq), (R_pss,))
                    rs, R_rs = rs_r.next()
                    act(rs[:], pss[:, :], AF.Sqrt, (R_pss,), (R_rs,), scale=1.0 / 128, bias=EPS)
                    SC.op("dve", lambda e, o=rs[:], i=rs[:]: e.reciprocal(out=o, in_=i), (R_rs,), (R_rs,))
                    tt("dve", ckvn[:], pkv[:, :], rs[:], ALU.mult, (R_pkv, R_rs), (R_ckvn,))
                    for h in range(4):
                        pz, R_pz = bank.next()
                        pr, R_pr = bank.next()
                        for c in range(2):
                            mm(pz[0:96, :], Wuq[:, c, 96 * h:96 * h + 96], cqn[:, c, :], c == 0, c == 1,
                               (R_W, R_cqn), (R_pz,))
                        for c in range(2):
                            mm(pr[0:96, :], WuqR[:, c, 96 * h:96 * h + 96], cqn[:, c, :], c == 0, c == 1,
                               (R_W, R_cqn), (R_pr,))
                        rope_out(96, pz, R_pz, pr, R_pr, tb[0:96, 4, :], tb[0:96, 5, :], R_tb, QTm[h, :, t0:t0 + 512])
                    for h in range(4):
                        pz, R_pz = bank.next()
                        pr, R_pr = bank.next()
                        mm(pz[0:96, :], Wkk[:, h, :], ckvn[:], True, False, (R_W, R_ckvn), (R_pz,))
                        for k in range(8):
                            mm(pz[0:96, :], Wkp[:, k, :], hT[:, k, :], False, k == 7, (R_W, R_hT), (R_pz,))
                        for k in range(8):
                            mm(pr[0:96, :], WkpR[:, k, :], hT[:, k, :], k == 0, k == 7, (R_W, R_hT), (R_pr,))
                        rope_out(96, pz, R_pz, pr, R_pr, tb[0:96, 4, :], tb[0:96, 5, :], R_tb, KTm[h, :, t0:t0 + 512])
                    for b in range(4):
                        tsl = slice(128 * b, 128 * b + 128)
                        r0 = t0 + 128 * b
                        vo, R_vo = vo_r.next()
                        wo, R_wo = wo_r.next()
                        p1, R_p1 = bank.next()
                        for k in range(8):
                            mm(p1[:, :], hT[:, k, tsl], Win[:, k, 1440:1952], k == 0, k == 7, (R_W, R_hT), (R_p1,))
                        cp("act", vo[:, 0:512], p1[:, :], (R_p1,), (R_vo,))
                        p2, R_p2 = bank.next()
                        for k in range(8):
                            mm(p2[:, 0:256], hT[:, k, tsl], Win[:, k, 2464:2720], k == 0, k == 7, (R_W, R_hT), (R_p2,))
                        cp("act", vo[:, 512:768], p2[:, 0:256], (R_p2,), (R_vo,))
                        p3, R_p3 = bank.next()
                        for k in range(8):
                            mm(p3[:, 0:8], hT[:, k, tsl], Win[:, k, 3008:3016], k == 0, k == 7, (R_W, R_hT), (R_p3,))
                        cp("act", wo[:], p3[:, 0:8], (R_p3,), (R_wo,))
                        p4, R_p4 = bank.next()
                        mm(p4[:, 0:256], ckvn[:, tsl], Wv[:], True, True, (R_W, R_ckvn), (R_p4,))
                        cp("act", vo[:, 768:1024], p4[:, 0:256], (R_p4,), (R_vo,))
                        dma(Vd[r0:r0 + 128, :], vo[:, 0:512], (R_vo,), (R_proj,))
                        dma(Vs[r0:r0 + 128, :], vo[:, 512:768], (R_vo,), (R_proj,))
                        dma(Vm[r0:r0 + 128, :], vo[:, 768:1024], (R_vo,), (R_proj,))
                        dma(WI[r0:r0 + 128, :], wo[:], (R_wo,), (R_proj,))
                SC.barrier()


        def phase_B():
            scale = 96 ** -0.5
            uid[0] += 1
            with ExitStack() as es:
                def sb(name, shape, dt):
                    return es.enter_context(nc.sbuf_tensor(name + "_B_%d" % uid[0], shape, dt))
                KT = sb("KT", [96, S], BF16)
                QT = sb("QT", [96, S], BF16)
                Vx = sb("Vx", [128, NB, 66], BF16)
                R_K, R_Q, R_V = Res("K"), Res("Q"), Res("V")
                pt_r = Ring([(sb("pt%d" % i, [128, 512], BF16), Res("pt%d" % i)) for i in range(3)])
                os_r = Ring([(sb("os%d" % i, [128, 512], F32), Res("os%d" % i)) for i in range(2)])
                rr_r = Ring([(sb("rr%d" % i, [64, 512], F32), Res("rr%d" % i)) for i in range(2)])
                ob_r = Ring([(sb("ob%d" % i, [64, 512], BF16), Res("ob%d" % i)) for i in range(2)])
                s_r = Ring(PS[0:4])
                od_r = Ring(PS[4:6])
                pd_r = Ring(PS[6:8])
                mset("pool", Vx[:, :, 64:66], 1.0, (R_V,))
                for h in range(4):
                    dma(KT[:], KTm[h, :, :], (R_proj,), (R_K,), q="sp")
                    dma(QT[:], QTm[h, :, :], (R_proj,), (R_Q,), q="sp")
                    dma(Vx[:, :, 0:64], Vm[:, 64 * h:64 * h + 64].rearrange("(j p) e -> p j e", p=128),
                        (R_proj,), (R_V,), q="sp")
                    for g in range(NT):
                        od, R_od = od_r.next()
                        nj = 4 * g + 4
                        for j in range(nj):
                            jj = j - 4 * g
                            c0 = 128 * jj if jj > 0 else 0
                            ps, R_ps = s_r.next()
                            mm(ps[:, c0:512], KT[:, 128 * j:128 * j + 128], QT[:, 512 * g + c0:512 * g + 512],
                               True, True, (R_K, R_Q), (R_ps,))
                            pt, R_pt = pt_r.next()
                            act(pt[:, c0:512], ps[:, c0:512], AF.Exp, (R_ps,), (R_pt,), scale=scale)
                            if jj >= 0:
                                tt("dve", pt[:, 128 * jj:128 * jj + 128], pt[:, 128 * jj:128 * jj + 128],
                                   band[:, 0:128], ALU.mult, (R_pt, R_const), (R_pt,))
                            mm(od[0:65, c0:512], Vx[:, j, 0:65], pt[:, c0:512], j == 0, j == nj - 1,
                               (R_V, R_pt), (R_od,))
                        osb, R_os = os_r.next()
                        cp("act", osb[0:65, :], od[0:65, :], (R_od,), (R_os,))
                        pd, R_pd = pd_r.next()
                        mm(pd[0:64, :], sel[0:65, :], osb[0:65, :], True, True, (R_const, R_os), (R_pd,))
                        rr, R_rr = rr_r.next()
                        SC.op("dve", lambda e, o=rr[:, :], i=pd[0:64, :]: e.reciprocal(out=o, in_=i), (R_pd,), (R_rr,))
                        ob, R_ob = ob_r.next()
                        tt("dve", ob[:, :], osb[0:64, :], rr[:, :], ALU.mult, (R_os, R_rr), (R_ob,))
                        dma(mixT[64 * h:64 * h + 64, 512 * g:512 * g + 512], ob[:, :], (R_ob,), (R_mix,))
                SC.barrier()

        def phase_C():
            scale = 64 ** -0.5
            uid[0] += 1
            with ExitStack() as es:
                def sb(name, shape, dt):
                    return es.enter_context(nc.sbuf_tensor(name + "_C_%d" % uid[0], shape, dt))
                KT = sb("KT", [128, S], BF16)
                QT = sb("QT", [128, S], BF16)
                R_K, R_Q = Res("K"), Res("Q")
                vx_r = Ring([(sb("Vx%d" % i, [128, NB, 66], BF16), Res("Vx%d" % i)) for i in range(2)])
                acc = sb("acc", [128, S], F32)
                R_acc = Res("acc")
                pt_r = Ring([(sb("pt%d" % i, [128, 256], BF16), Res("pt%d" % i)) for i in range(3)])
                rr_r = Ring([(sb("rr%d" % i, [64, 512], F32), Res("rr%d" % i)) for i in range(2)])
                ob_r = Ring([(sb("ob%d" % i, [64, 512], BF16), Res("ob%d" % i)) for i in range(2)])
                s_r = Ring(PS[0:4])
                od_b = PS[4:6]
                pd_r = Ring(PS[6:8])
                for (vx, R_vx) in vx_r.items:
                    mset("pool", vx[:, :, 64:66], 1.0, (R_vx,))
                for i in range(4):
                    dma(KT[:], KTd[i, :, :], (R_proj,), (R_K,), q="sp")
                    dma(QT[:], QTd[i, :, :], (R_proj,), (R_Q,), q="sp")
                    for hh in range(2):
                        h = 2 * i + hh
                        rows = slice(64 * hh, 64 * hh + 64)
                        for pi, d in enumerate((1, 4, 16)):
                            vx, R_vx = vx_r.next()
                            vsrc = Vd[:, 64 * h:64 * h + 64].rearrange("(n i d) e -> i n d e", i=128, d=d)
                            for r in range(d):
                                dma(vx[:, r:NB:d, 0:64], vsrc[:, :, r, :], (R_proj,), (R_vx,), q="sp")
                            nblk = S // (128 * d)
                            for r in range(d):
                                for m in range(nblk):
                                    blk = m * d + r
                                    ks = 128 * m * d + r
                                    nq = 256 if m + 1 < nblk else 128
                                    ps, R_ps = s_r.next()
                                    mm(ps[:, 0:nq], KT[rows, ks:ks + 127 * d + 1:d], QT[rows, ks:ks + (nq - 1) * d + 1:d],
                                       True, True, (R_K, R_Q), (R_ps,))
                                    pt, R_pt = pt_r.next()
                                    act(pt[:, 0:nq], ps[:, 0:nq], AF.Exp, (R_ps,), (R_pt,), scale=scale)
                                    tt("dve", pt[:, 0:nq], pt[:, 0:nq], band[:, 0:nq], ALU.mult, (R_pt, R_const), (R_pt,))
                                    od, R_od = od_b[m % 2]
                                    mm(od[0:65, 0:128], vx[:, blk, 0:65], pt[:, 0:128], m == 0, True,
                                       (R_vx, R_pt), (R_od,))
                                    dst = acc[0:65, ks:ks + 127 * d + 1:d]
                                    if pi == 0:
                                        cp("dve", dst, od[0:65, 0:128], (R_od,), (R_acc,))
                                    else:
                                        tt("dve", dst, dst, od[0:65, 0:128], ALU.add, (R_od, R_acc), (R_acc,))
                                    if m + 1 < nblk:
                                        od2, R_od2 = od_b[(m + 1) % 2]
                                        mm(od2[0:65, 0:128], vx[:, blk, 0:65], pt[:, 128:256], True, False,
                                           (R_vx, R_pt), (R_od2,))
                        for g in range(NT):
                            pd, R_pd = pd_r.next()
                            mm(pd[0:64, :], sel[0:65, :], acc[0:65, 512 * g:512 * g + 512], True, True,
                               (R_const, R_acc), (R_pd,))
                            rr, R_rr = rr_r.next()
                            SC.op("dve", lambda e, o=rr[:, :], i_=pd[0:64, :]: e.reciprocal(out=o, in_=i_), (R_pd,), (R_rr,))
                            ob, R_ob = ob_r.next()
                            tt("dve", ob[:, :], acc[0:64, 512 * g:512 * g + 512], rr[:, :], ALU.mult,
                               (R_acc, R_rr), (R_ob,))
                            dma(mixT[256 + 64 * h:256 + 64 * h + 64, 512 * g:512 * g + 512], ob[:, :], (R_ob,), (R_mix,))
                SC.barrier()

        def phase_D():
            scale = 64 ** -0.5
            uid[0] += 1
            with ExitStack() as es:
                def sb(name, shape, dt):
                    return es.enter_context(nc.sbuf_tensor(name + "_D_%d" % uid[0], shape, dt))
                KI = sb("KI", [128, S], BF16)
                KT = sb("KT", [128, 2, S], BF16)
                Vx = sb("Vx", [128, NB, 4, 66], BF16)
                WIt = sb("WIt", [128, NB, 8], F32)
                R_KI, R_K, R_V, R_WI = Res("KI"), Res("K"), Res("V"), Res("WI")
                Isc = sb("Isc", [128, S], F32)
                Msk = sb("Msk", [128, S], BF16)
                MskT = sb("MskT", [128, NB, 128], BF16)
                R_I, R_M, R_MT = Res("I"), Res("M"), Res("MT")
                qi_r = Ring([(sb("qi%d" % i, [96, 3, 128], BF16), Res("qi%d" % i)) for i in range(2)])
                qs_r = Ring([(sb("qs%d" % i, [128, 2, 128], BF16), Res("qs%d" % i)) for i in range(2)])
                rl_r = Ring([(sb("rl%d" % i, [128, 512], F32), Res("rl%d" % i)) for i in range(2)])
                pt_r = Ring([(sb("pt%d" % i, [128, 512], BF16), Res("pt%d" % i)) for i in range(3)])
                st_r = Ring([(sb("st%d" % i, [128, 8], F32), Res("st%d" % i)) for i in range(2)])
                wk_r = Ring([(sb("wk%d" % i, [128, NBIS + 1], F32), Res("wk%d" % i)) for i in range(2)])
                os_r = Ring([(sb("os%d" % i, [128, 128], F32), Res("os%d" % i)) for i in range(2)])
                rr_r = Ring([(sb("rr%d" % i, [64, 128], F32), Res("rr%d" % i)) for i in range(2)])
                ob_r = Ring([(sb("ob%d" % i, [64, 128], BF16), Res("ob%d" % i)) for i in range(2)])
                i_r = Ring(PS[0:3])
                t_b = PS[3]
                s_r = Ring(PS[4:6])
                od_b = PS[6]
                pd_b = PS[7]
                mset("pool", Vx[:, :, :, 64:66], 1.0, (R_V,))
                dma(KI[:], KIT[:, :], (R_proj,), (R_KI,), q="sp")
                for i in range(2):
                    dma(KT[:, i, :], KTs[i, :, :], (R_proj,), (R_K,), q="sp")
                for h in range(4):
                    dma(Vx[:, :, h, 0:64], Vs[:, 64 * h:64 * h + 64].rearrange("(j p) e -> p j e", p=128),
                        (R_proj,), (R_V,), q="sp")
                dma(WIt[:], WI.rearrange("(j p) h -> p j h", p=128), (R_proj,), (R_WI,), q="sp")
                qs_of = {}

                def idx_part(qb):
                    nk = 128 * (qb + 1)
                    nch = (nk + 511) // 512
                    qi, R_qi = qi_r.next()
                    qs, R_qs = qs_r.next()
                    dma(qi[:, 0:2, :], QIT[0:2, :, 128 * qb:128 * qb + 128].rearrange("i p t -> p i t"), (R_proj,), (R_qi,))
                    dma(qi[0:64, 2, :], QIT[2, 0:64, 128 * qb:128 * qb + 128], (R_proj,), (R_qi,))
                    dma(qs[:], QTs[:, :, 128 * qb:128 * qb + 128].rearrange("i p t -> p i t"), (R_proj,), (R_qs,))
                    qs_of[qb] = (qs, R_qs)
                    for c in range(nch):
                        ncol = min(512, nk - 512 * c)
                        cs = slice(512 * c, 512 * c + ncol)
                        for hh in range(8):
                            po = 32 * (hh % 3)
                            bi = hh // 3
                            ps, R_ps = i_r.next()
                            mm(ps[:, 0:ncol], qi[po:po + 32, bi, :], KI[po:po + 32, cs], True, True,
                               (R_qi, R_KI), (R_ps,))
                            rl, R_rl = rl_r.next()
                            act(rl[:, 0:ncol], ps[:, 0:ncol], AF.Relu, (R_ps,), (R_rl,))
                            if hh == 0:
                                ts("dve", Isc[:, cs], rl[:, 0:ncol], WIt[:, qb, 0:1], None, ALU.mult, None,
                                   (R_rl, R_WI), (R_I,))
                            else:
                                stt(Isc[:, cs], rl[:, 0:ncol], WIt[:, qb, hh:hh + 1], Isc[:, cs], ALU.mult, ALU.add,
                                    (R_rl, R_WI, R_I), (R_I,))
                    dg = slice(128 * qb, 128 * qb + 128)
                    tt("dve", Isc[:, dg], Isc[:, dg], negtri[:], ALU.add, (R_I, R_const), (R_I,))

                def bisect_part(qb):
                    nk = 128 * (qb + 1)
                    if nk > TOPK:
                        st, R_st = st_r.next()
                        RS = (R_st,)
                        SC.op("dve", lambda e, o=st[:, 0:1], i_=Isc[:, 0:nk - 128]: e.tensor_reduce(out=o, in_=i_, axis=AX.X, op=ALU.min),
                              (R_I,), RS)
                        SC.op("dve", lambda e, o=st[:, 1:2], i_=Isc[:, 0:nk]: e.tensor_reduce(out=o, in_=i_, axis=AX.X, op=ALU.max),
                              (R_I,), RS)
                        tt("dve", st[:, 1:2], st[:, 1:2], st[:, 0:1], ALU.subtract, RS, RS)
                        wk, R_wk = wk_r.next()
                        ts("dve", wk[:], pw2[:], st[:, 1:2], None, ALU.mult, None, (R_st, R_const), (R_wk,))
                        stt(st[:, 2:3], st[:, 1:2], 0.5, st[:, 0:1], ALU.mult, ALU.add, RS, RS)
                        for it in range(NBIS):
                            ts("dve", Msk[:, 0:nk], Isc[:, 0:nk], st[:, 2:3], 0.0, ALU.is_ge, ALU.add,
                               (R_I, R_st), (R_M, R_st), accum_out=st[:, 3:4])
                            ts("dve", st[:, 4:5], st[:, 3:4], TOPK - 0.5, 0.5, ALU.is_ge, ALU.subtract, RS, RS)
                            stt(st[:, 2:3], st[:, 4:5], wk[:, it:it + 1], st[:, 2:3], ALU.mult, ALU.add,
                                (R_st, R_wk), RS)
                        stt(st[:, 0:1], wk[:, NBIS:NBIS + 1], -1.0, st[:, 2:3], ALU.mult, ALU.add, (R_st, R_wk), RS)
                        ts("dve", Msk[:, 0:nk], Isc[:, 0:nk], st[:, 0:1], None, ALU.is_ge, None, (R_I, R_st), (R_M,))
                    else:
                        ts("dve", Msk[:, 0:nk], Isc[:, 0:nk], -1.0e29, None, ALU.is_ge, None, (R_I,), (R_M,))
                    tb_, R_tb_ = t_b
                    tbb = tb_[:, :].bitcast(BF16)
                    for j0 in range(0, qb + 1, 8):
                        n8 = min(8, qb + 1 - j0)
                        for jj in range(n8):
                            j = j0 + jj
                            tr(tbb[:, 128 * jj:128 * jj + 128], Msk[:, 128 * j:128 * j + 128], (R_M, R_const), (R_tb_,))
                        cp("act", MskT[:, j0:j0 + n8, :], tbb[:, 0:128 * n8].rearrange("p (j t) -> p j t", t=128),
                           (R_tb_,), (R_MT,))

                def attn_part(qb):
                    nk = 128 * (qb + 1)
                    nch = (nk + 511) // 512
                    qs, R_qs = qs_of.pop(qb)
                    for h in range(4):
                        rows = slice(64 * (h % 2), 64 * (h % 2) + 64)
                        bi = h // 2
                        od, R_od = od_b
                        for c in range(nch):
                            nbc = min(4, qb + 1 - 4 * c)
                            ps, R_ps = s_r.next()
                            for jj in range(nbc):
                                j = 4 * c + jj
                                mm(ps[:, 128 * jj:128 * jj + 128], KT[rows, bi, 128 * j:128 * j + 128], qs[rows, bi, :],
                                   True, True, (R_K, R_qs), (R_ps,))
                            pt, R_pt = pt_r.next()
                            act(pt[:, 0:128 * nbc], ps[:, 0:128 * nbc], AF.Exp, (R_ps,), (R_pt,), scale=scale)
                            tt("dve", pt[:, 0:128 * nbc], pt[:, 0:128 * nbc],
                               MskT[:, 4 * c:4 * c + nbc, :].rearrange("p j t -> p (j t)"), ALU.mult,
                               (R_pt, R_MT), (R_pt,))
                            for jj in range(nbc):
                                j = 4 * c + jj
                                mm(od[0:65, 0:128], Vx[:, j, h, 0:65], pt[:, 128 * jj:128 * jj + 128], j == 0, j == qb,
                                   (R_V, R_pt), (R_od,))
                        osb, R_os = os_r.next()
                        cp("act", osb[0:65, :], od[0:65, 0:128], (R_od,), (R_os,))
                        pd, R_pd = pd_b
                        mm(pd[0:64, 0:128], sel[0:65, :], osb[0:65, :], True, True, (R_const, R_os), (R_pd,))
                        rr, R_rr = rr_r.next()
                        SC.op("dve", lambda e, o=rr[:, :], i_=pd[0:64, 0:128]: e.reciprocal(out=o, in_=i_), (R_pd,), (R_rr,))
                        ob, R_ob = ob_r.next()
                        tt("dve", ob[:, :], osb[0:64, :], rr[:, :], ALU.mult, (R_os, R_rr), (R_ob,))
                        dma(mixT[768 + 64 * h:768 + 64 * h + 64, 128 * qb:128 * qb + 128], ob[:, :], (R_ob,), (R_mix,))

                for qb in range(NB):
                    idx_part(qb)
                    if qb > 0:
                        attn_part(qb - 1)
                    bisect_part(qb)
                attn_part(NB - 1)
                SC.barrier()

        def norm_T(es_rings, xt, R_xt, hT, R_hT, b, bank):
            junk, R_junk, hb_r, st_r = es_rings
            hb, R_hb = hb_r.next()
            stt_, R_st_ = st_r.next()
            act(junk[:], xt[:], AF.Square, (R_xt,), (R_junk, R_st_), accum_out=stt_[:, 0:1])
            act(stt_[:, 1:2], stt_[:, 0:1], AF.Sqrt, (R_st_,), (R_st_,), scale=1.0 / D, bias=EPS)
            SC.op("dve", lambda e, o=stt_[:, 2:3], i=stt_[:, 1:2]: e.reciprocal(out=o, in_=i), (R_st_,), (R_st_,))
            ts("dve", hb[:], xt[:], stt_[:, 2:3], None, ALU.mult, None, (R_xt, R_st_), (R_hb,))
            pt_, R_pt = bank.next()
            ptb = pt_[:, :].bitcast(BF16)
            for k in range(8):
                tr(ptb[:, 128 * k:128 * k + 128], hb[:, 128 * k:128 * k + 128], (R_hb, R_const), (R_pt,))
            cp("act", hT[:, :, 128 * b:128 * b + 128], ptb.rearrange("p (k t) -> p k t", t=128), (R_pt,), (R_hT,))

        def phase_E1(l, xsrc, R_xs):
            uid[0] += 1
            with ExitStack() as es:
                def sb(name, shape, dt):
                    return es.enter_context(nc.sbuf_tensor(name + "_E1_%d" % uid[0], shape, dt))
                Wo = sb("Wo", [128, 8, D], BF16)
                Wup = sb("Wup", [128, 8, 2 * DFF], BF16)
                R_W = Res("W_E1")
                stg = Ring([(sb("stg%d" % i, [128, 2048], F32), Res("stg%d" % i)) for i in range(2)])
                for k in range(8):
                    st, R_st = stg.next()
                    dma(st[:, 0:1024], w_o[l, 128 * k:128 * k + 128, :], (), (R_st,))
                    cp("act", Wo[:, k, :], st[:, 0:1024], (R_st,), (R_W,))
                    for (c0, cn) in ((0, 2048), (2048, 2048), (4096, 1536)):
                        st, R_st = stg.next()
                        dma(st[:, 0:cn], w_up[l, 128 * k:128 * k + 128, c0:c0 + cn], (), (R_st,))
                        ts("dve", Wup[:, k, c0:c0 + cn], st[:, 0:cn], gF[:, l * 8 + k:l * 8 + k + 1], None, ALU.mult, None,
                           (R_st, R_const), (R_W,))
                halo = sb("halo", [128, 44, 2], F32)
                R_halo = Res("halo")
                mset("pool", halo[:], 0.0, (R_halo,))
                mT_r = Ring([(sb("mT%d" % i, [128, 8, 512], BF16), Res("mT%d" % i)) for i in range(2)])
                hT_r = Ring([(sb("hT%d" % i, [128, 8, 512], BF16), Res("hT%d" % i)) for i in range(2)])
                xt_r = Ring([(sb("xt%d" % i, [128, D], F32), Res("xt%d" % i)) for i in range(2)])
                x1_r = Ring([(sb("x1%d" % i, [128, D], F32), Res("x1%d" % i)) for i in range(2)])
                junk = sb("junk", [128, D], BF16)
                R_junk = Res("junk")
                hb_r = Ring([(sb("hb%d" % i, [128, D], BF16), Res("hb%d" % i)) for i in range(2)])
                st_r = Ring([(sb("st%d" % i, [128, 4], F32), Res("st%d" % i)) for i in range(2)])
                ub_r = Ring([(sb("ub%d" % i, [128, 514], F32), Res("ub%d" % i)) for i in range(3)])
                y_r = Ring([(sb("y%d" % i, [128, 512], F32), Res("y%d" % i)) for i in range(4)])
                sg_r = Ring([(sb("sg%d" % i, [128, 512], F32), Res("sg%d" % i)) for i in range(2)])
                ab_r = Ring([(sb("ab%d" % i, [128, 512], BF16), Res("ab%d" % i)) for i in range(3)])
                bank = Ring(PS)
                rings = (junk, R_junk, hb_r, st_r)
                for ti in range(NT):
                    t0 = 512 * ti
                    mT, R_mT = mT_r.next()
                    hT, R_hT = hT_r.next()
                    dma(mT[:], mixT[:, t0:t0 + 512].rearrange("(k p) t -> p k t", p=128), (R_mix,), (R_mT,), q="sp")
                    for b in range(4):
                        r0 = t0 + 128 * b
                        xt, R_xt = xt_r.next()
                        x1, R_x1t = x1_r.next()
                        dma(xt[:], xsrc[r0:r0 + 128, :], (R_xs,), (R_xt,))
                        for hf in range(2):
                            ps, R_ps = bank.next()
                            for k in range(8):
                                mm(ps[:, :], mT[:, k, 128 * b:128 * b + 128], Wo[:, k, 512 * hf:512 * hf + 512],
                                   k == 0, k == 7, (R_mT, R_W), (R_ps,))
                            tt("dve", x1[:, 512 * hf:512 * hf + 512], ps[:, :], xt[:, 512 * hf:512 * hf + 512], ALU.add,
                               (R_ps, R_xt), (R_x1t,))
                        dma(xres1[r0:r0 + 128, :], x1[:], (R_x1t,), (R_x1,))
                        norm_T(rings, x1, R_x1t, hT, R_hT, b, bank)
                    for f in range(22):
                        ys = []
                        for fc in (f, 22 + f):
                            ps, R_ps = bank.next()
                            for k in range(8):
                                mm(ps[:, :], Wup[:, k, 128 * fc:128 * fc + 128], hT[:, k, :], k == 0, k == 7,
                                   (R_W, R_hT), (R_ps,))
                            ub, R_ub = ub_r.next()
                            cp("dve", ub[:, 0:2], halo[:, fc, :], (R_halo,), (R_ub,))
                            cp("act", ub[:, 2:514], ps[:, :], (R_ps,), (R_ub,))
                            cp("dve", halo[:, fc, :], ub[:, 512:514], (R_ub,), (R_halo,))
                            y, R_y_ = y_r.next()
                            ci = l * 132 + fc
                            ts("dve", y[:], ps[:, :], cw[:, ci + 88:ci + 89], cb[:, l * 44 + fc:l * 44 + fc + 1],
                               ALU.mult, ALU.add, (R_ps, R_const), (R_y_,))
                            stt(y[:], ub[:, 1:513], cw[:, ci + 44:ci + 45], y[:], ALU.mult, ALU.add, (R_ub, R_const, R_y_), (R_y_,))
                            stt(y[:], ub[:, 0:512], cw[:, ci:ci + 1], y[:], ALU.mult, ALU.add, (R_ub, R_const, R_y_), (R_y_,))
                            ys.append((y, R_y_))
                        sg, R_sg = sg_r.next()
                        act(sg[:], ys[0][0][:], AF.Silu, (ys[0][1],), (R_sg,))
                        ab, R_ab = ab_r.next()
                        tt("dve", ab[:], sg[:], ys[1][0][:], ALU.mult, (R_sg, ys[1][1]), (R_ab,))
                        dma(actT[128 * f:128 * f + 128, t0:t0 + 512], ab[:], (R_ab,), (R_act,))
                SC.barrier()

        def phase_E2(l, last):
            uid[0] += 1
            with ExitStack() as es:
                def sb(name, shape, dt):
                    return es.enter_context(nc.sbuf_tensor(name + "_E2_%d" % uid[0], shape, dt))
                Wdn = sb("Wdn", [128, 22, D], BF16)
                R_W = Res("W_E2")
                stg = Ring([(sb("stg%d" % i, [128, D], F32), Res("stg%d" % i)) for i in range(2)])
                for f in range(22):
                    st, R_st = stg.next()
                    dma(st[:], w_down[l, 128 * f:128 * f + 128, :], (), (R_st,))
                    cp("act" if f % 2 else "dve", Wdn[:, f, :], st[:], (R_st,), (R_W,))
                gfin = sb("gfin", [128, D], F32)
                R_gf = Res("gfin")
                if last:
                    dma(gfin[:], gfin_d[:, :], (), (R_gf,))
                aT_r = Ring([(sb("aT%d" % i, [128, 22, 512], BF16), Res("aT%d" % i)) for i in range(2)])
                xt_r = Ring([(sb("xt%d" % i, [128, D], F32), Res("xt%d" % i)) for i in range(2)])
                x2_r = Ring([(sb("x2%d" % i, [128, D], F32), Res("x2%d" % i)) for i in range(2)])
                yo_r = Ring([(sb("yo%d" % i, [128, D], F32), Res("yo%d" % i)) for i in range(2)])
                junk = sb("junk", [128, D], BF16)
                R_junk = Res("junk")
                st_r = Ring([(sb("st%d" % i, [128, 4], F32), Res("st%d" % i)) for i in range(2)])
                bank = Ring(PS)
                for ti in range(NT):
                    t0 = 512 * ti
                    aT, R_aT = aT_r.next()
                    dma(aT[:], actT[:, t0:t0 + 512].rearrange("(f p) t -> p f t", p=128), (R_act,), (R_aT,), q="sp")
                    for b in range(4):
                        r0 = t0 + 128 * b
                        xt, R_xt = xt_r.next()
                        x2, R_x2 = x2_r.next()
                        dma(xt[:], xres1[r0:r0 + 128, :], (R_x1,), (R_xt,))
                        for hf in range(2):
                            ps, R_ps = bank.next()
                            for f in range(22):
                                mm(ps[:, :], aT[:, f, 128 * b:128 * b + 128], Wdn[:, f, 512 * hf:512 * hf + 512],
                                   f == 0, f == 21, (R_aT, R_W), (R_ps,))
                            tt("dve", x2[:, 512 * hf:512 * hf + 512], ps[:, :], xt[:, 512 * hf:512 * hf + 512], ALU.add,
                               (R_ps, R_xt), (R_x2,))
                        if not last:
                            dma(xres0[r0:r0 + 128, :], x2[:], (R_x2,), (R_x,))
                        else:
                            stt_, R_st_ = st_r.next()
                            yo, R_yo = yo_r.next()
                            act(junk[:], x2[:], AF.Square, (R_x2,), (R_junk, R_st_), accum_out=stt_[:, 0:1])
                            act(stt_[:, 1:2], stt_[:, 0:1], AF.Sqrt, (R_st_,), (R_st_,), scale=1.0 / D, bias=EPS)
                            SC.op("dve", lambda e, o=stt_[:, 2:3], i=stt_[:, 1:2]: e.reciprocal(out=o, in_=i), (R_st_,), (R_st_,))
                            stt(yo[:], x2[:], stt_[:, 2:3], gfin[:], ALU.mult, ALU.mult, (R_x2, R_st_, R_gf), (R_yo,))
                            dma(y_out[r0:r0 + 128, :], yo[:], (R_yo,), (R_y,))
                SC.barrier()

        for l in range(L):
            xsrc = x_in if l == 0 else xres0
            phase_A(l, xsrc)
            if stop_after == "A":
                break
            if "B" not in skip:
                phase_B()
            if stop_after == "B":
                break
            if "C" not in skip:
                phase_C()
            if stop_after == "C":
                break
            if "D" not in skip:
                phase_D()
            if stop_after == "D":
                break
            phase_E1(l, xsrc, R_x)
            if stop_after == "E1":
                break
            phase_E2(l, l == L - 1)
        SC.barrier()
        print("instructions:", SC.ninst)
    return nc


def make_consts(S):
    bf = ml_dtypes.bfloat16
    c = {}
    c["c_ident"] = np.eye(128, dtype=np.float32).astype(bf)
    c["c_ones"] = np.ones((128, 128), np.float32).astype(bf)
    sel = np.zeros((128, 64), np.float32)
    sel[64, :] = 1.0
    c["c_sel"] = sel
    k = np.arange(128)[:, None]
    q = np.arange(128)[None, :]
    c["c_band"] = np.concatenate([(k <= q), (k >= q)], axis=1).astype(np.float32).astype(bf)
    c["c_negtri"] = np.where(q.T >= k.T, 0.0, NEG).astype(np.float32) if False else \
        np.where(np.arange(128)[None, :] <= np.arange(128)[:, None], 0.0, NEG).astype(np.float32)
    c["c_pw2"] = np.ascontiguousarray(np.broadcast_to(
        (2.0 ** -(np.arange(NBIS + 1, dtype=np.float32) + 1.0)).astype(np.float32)[None, :], (128, NBIS + 1)))
    t = np.arange(S, dtype=np.float32)

    def tables(dim):
        inv = np.power(np.float32(500000.0), -np.arange(0, dim, 2, dtype=np.float32) / np.float32(dim)).astype(np.float32)
        ang = (t[:, None] * inv[None, :]).astype(np.float32)
        return np.cos(ang).astype(np.float32).T, np.sin(ang).astype(np.float32).T
    ch, sh = tables(16)
    ci, si = tables(8)
    ca, sa = tables(32)
    C = np.ones((128, S), np.float32)
    Sn = np.zeros((128, S), np.float32)
    for hh in range(2):
        C[64 * hh:64 * hh + 8] = ch
        C[64 * hh + 8:64 * hh + 16] = ch
        Sn[64 * hh:64 * hh + 8] = sh
        Sn[64 * hh + 8:64 * hh + 16] = sh
    c["tab64"] = np.stack([C, Sn])
    C = np.ones((128, S), np.float32)
    Sn = np.zeros((128, S), np.float32)
    for hh in range(4):
        C[32 * hh:32 * hh + 4] = ci
        C[32 * hh + 4:32 * hh + 8] = ci
        Sn[32 * hh:32 * hh + 4] = si
        Sn[32 * hh + 4:32 * hh + 8] = si
    c["tabidx"] = np.stack([C, Sn])
    C = np.ones((96, S), np.float32)
    Sn = np.zeros((96, S), np.float32)
    C[64:80] = ca
    C[80:96] = ca
    Sn[64:80] = sa
    Sn[80:96] = sa
    c["tabmla"] = np.stack([C, Sn])
    return c


def layout_params(inp, L):
    f = np.float32
    o = {}
    o["gA"] = np.ascontiguousarray(np.asarray(inp["g_attn"], f).reshape(L, 8, 128).transpose(2, 0, 1).reshape(128, L * 8))
    o["gF"] = np.ascontiguousarray(np.asarray(inp["g_ffn"], f).reshape(L, 8, 128).transpose(2, 0, 1).reshape(128, L * 8))
    o["gq"] = np.ascontiguousarray(np.asarray(inp["g_q_lat"], f).reshape(L, 2, 128).transpose(2, 0, 1).reshape(128, L * 2))
    o["gkv"] = np.ascontiguousarray(np.asarray(inp["g_kv_lat"], f).reshape(L, 128).T)
    o["cw"] = np.ascontiguousarray(np.asarray(inp["conv_w"], f).reshape(L, 3, 44, 128).transpose(3, 0, 1, 2).reshape(128, L * 3 * 44))
    o["cb"] = np.ascontiguousarray(np.asarray(inp["conv_b"], f).reshape(L, 44, 128).transpose(2, 0, 1).reshape(128, L * 44))
    o["gfin"] = np.ascontiguousarray(np.broadcast_to(np.asarray(inp["g_final"], f).reshape(1, D), (128, D)))
    for k in ("w_in", "w_uq", "w_ukv", "w_o", "w_up", "w_down"):
        o[k] = np.ascontiguousarray(np.asarray(inp[k], f))
    return o


_CACHE = {}


def kernel(**inputs):
    x = np.asarray(inputs["x"], np.float32)
    B, S, _ = x.shape
    L = inputs["w_in"].shape[0]
    key = (S, L)
    if key not in _CACHE:
        _CACHE[key] = (build(S, L), make_consts(S))
    nc, consts = _CACHE[key]
    shared = layout_params(inputs, L)
    shared.update(consts)
    n = 8
    in_maps = []
    for c in range(n):
        m = dict(shared)
        m["x"] = np.ascontiguousarray(x[c % B])
        in_maps.append(m)
    res = run_bass_kernel_spmd(nc, in_maps, core_ids=list(range(n)))
    return np.stack([res.results[b]["y"] for b in range(B)], axis=0).astype(np.float32)
```

```python
import numpy as np
import ml_dtypes
from contextlib import ExitStack
import concourse.bass as bass
import concourse.mybir as mybir
from concourse.bass_utils import run_bass_kernel_spmd

F32 = mybir.dt.float32
BF16 = mybir.dt.bfloat16
AF = mybir.ActivationFunctionType
ALU = mybir.AluOpType
AX = mybir.AxisListType

EPS = 1e-6
NEG = -1.0e30
D = 1024
NIN = 3016
DFF = 2816
TOPK = 256
NBIS = 17


class Res:
    __slots__ = ("name", "w", "r", "ordered")

    def __init__(self, name, ordered=True):
        self.name = name
        self.w = {}
        self.r = {}
        self.ordered = ordered


class Eng:
    def __init__(self, name, eng, sem):
        self.name = name
        self.eng = eng
        self.sem = sem
        self.n = 0
        self.waited = {}
        self.dsem = []
        self.dval = []
        self.di = 0


class Sched:
    K = 12

    def __init__(self, nc, es):
        self.nc = nc
        self.E = {}
        for name, e in (("pe", nc.tensor), ("act", nc.scalar), ("dve", nc.vector),
                        ("pool", nc.gpsimd), ("sp", nc.sync)):
            self.E[name] = Eng(name, e, es.enter_context(nc.semaphore("p_" + name)))
        for q in ("sp", "pool", "act"):
            E = self.E[q]
            for i in range(self.K):
                E.dsem.append(es.enter_context(nc.semaphore("d_%s%d" % (q, i))))
                E.dval.append(0)
        self.ninst = 0

    def _need(self, reads, writes, own=None):
        need = {}

        def add(d, skip_own):
            for k, sv in d.items():
                if skip_own and sv[0] is own:
                    continue
                if k not in need or need[k][1] < sv[1]:
                    need[k] = sv
        for r in reads:
            add(r.w, False)
        for w in writes:
            add(w.r, True)
            if w.ordered:
                add(w.w, True)
        return need

    def _emit_waits(self, E, need, is_dma):
        for k, (s, v) in need.items():
            if s is E.sem and not is_dma and E.name == "pe":
                continue
            if E.waited.get(k, 0) >= v:
                continue
            E.eng.wait_ge(s, v)
            E.waited[k] = v

    def _mark(self, tok, reads, writes):
        k = id(tok[0])
        for r in reads:
            if r.r.get(k, (None, 0))[1] < tok[1]:
                r.r[k] = tok
        for w in writes:
            if w.ordered:
                w.w = {k: tok}
                w.r = {}
            else:
                w.w[k] = tok

    def op(self, eng, fn, reads=(), writes=()):
        E = self.E[eng]
        self._emit_waits(E, self._need(reads, writes, own=E.sem), False)
        inst = fn(E.eng)
        E.n += 1
        inst.then_inc(E.sem, 1)
        self._mark((E.sem, E.n), reads, writes)
        self.ninst += 1

    def dma(self, q, out, in_, reads=(), writes=()):
        E = self.E[q]
        self._emit_waits(E, self._need(reads, writes), True)
        i = E.di % self.K
        E.di += 1
        s = E.dsem[i]
        pv = E.dval[i]
        if pv > 0 and E.waited.get(id(s), 0) < pv:
            E.eng.wait_ge(s, pv)
            E.waited[id(s)] = pv
        E.eng.dma_start(out=out, in_=in_).then_inc(s, 16)
        E.dval[i] = pv + 16
        self._mark((s, pv + 16), reads, writes)
        self.ninst += 1

    def barrier(self):
        for E in self.E.values():
            for O in self.E.values():
                if O is not E and O.n > 0 and E.waited.get(id(O.sem), 0) < O.n:
                    E.eng.wait_ge(O.sem, O.n)
                    E.waited[id(O.sem)] = O.n
                for s, v in zip(O.dsem, O.dval):
                    if v > 0 and E.waited.get(id(s), 0) < v:
                        E.eng.wait_ge(s, v)
                        E.waited[id(s)] = v


class Ring:
    def __init__(self, items):
        self.items = items
        self.i = 0

    def next(self):
        it = self.items[self.i % len(self.items)]
        self.i += 1
        return it


def build(S=8192, L=4, dbg=False, stop_after=None, skip=()):
    NT = S // 512
    NB = S // 128
    nc = bass.Bass("TRN2", target_bir_lowering=False)

    def din(name, shape, dt=F32):
        return nc.dram_tensor(name, list(shape), dt, kind="ExternalInput").ap()

    def dscr(name, shape, dt):
        return nc.dram_tensor(name, list(shape), dt, kind=("ExternalOutput" if dbg else "Internal")).ap()

    x_in = din("x", [S, D])
    w_in = din("w_in", [L, D, NIN])
    w_uq = din("w_uq", [L, 256, 384])
    w_ukv = din("w_ukv", [L, 128, 512])
    w_o = din("w_o", [L, D, D])
    w_up = din("w_up", [L, D, 2 * DFF])
    w_down = din("w_down", [L, DFF, D])
    gA_d = din("gA", [128, L * 8])
    gF_d = din("gF", [128, L * 8])
    gq_d = din("gq", [128, L * 2])
    gkv_d = din("gkv", [128, L])
    cw_d = din("cw", [128, L * 3 * 44])
    cb_d = din("cb", [128, L * 44])
    gfin_d = din("gfin", [128, D])
    c_ident = din("c_ident", [128, 128], BF16)
    c_ones = din("c_ones", [128, 128], BF16)
    c_sel = din("c_sel", [128, 64])
    c_band = din("c_band", [128, 256], BF16)
    c_negtri = din("c_negtri", [128, 128])
    c_pw2 = din("c_pw2", [128, NBIS + 1])
    tab64 = din("tab64", [2, 128, S])
    tabidx = din("tabidx", [2, 128, S])
    tabmla = din("tabmla", [2, 96, S])
    y_out = nc.dram_tensor("y", [S, D], F32, kind="ExternalOutput").ap()

    xres0 = dscr("xres0", [S, D], F32)
    xres1 = dscr("xres1", [S, D], F32)
    QTm = dscr("QTm", [4, 96, S], BF16)
    KTm = dscr("KTm", [4, 96, S], BF16)
    Vm = dscr("Vm", [S, 256], BF16)
    QTd = dscr("QTd", [4, 128, S], BF16)
    KTd = dscr("KTd", [4, 128, S], BF16)
    Vd = dscr("Vd", [S, 512], BF16)
    QTs = dscr("QTs", [2, 128, S], BF16)
    KTs = dscr("KTs", [2, 128, S], BF16)
    Vs = dscr("Vs", [S, 256], BF16)
    QIT = dscr("QIT", [3, 96, S], BF16)
    KIT = dscr("KIT", [128, S], BF16)
    WI = dscr("WI", [S, 8], F32)
    mixT = dscr("mixT", [D, S], BF16)
    actT = dscr("actT", [DFF, S], BF16)

    ges = ExitStack()
    with ges:
        SC = Sched(nc, ges)
        R_x = Res("xres0", ordered=False)
        R_x1 = Res("xres1", ordered=False)
        R_proj = Res("proj", ordered=False)
        R_mix = Res("mixT", ordered=False)
        R_act = Res("actT", ordered=False)
        R_y = Res("y", ordered=False)
        R_const = Res("const")
        DRAM_RES = (R_x, R_x1, R_proj, R_mix, R_act, R_y)

        def gsb(name, shape, dt):
            return ges.enter_context(nc.sbuf_tensor("g_" + name, shape, dt))

        PS = []
        for i in range(8):
            t = ges.enter_context(nc.psum_tensor("ps%d" % i, [128, 512], F32))
            PS.append((t, Res("ps%d" % i)))

        gA = gsb("gA", [128, L * 8], F32)
        gF = gsb("gF", [128, L * 8], F32)
        gq = gsb("gq", [128, L * 2], F32)
        gkv = gsb("gkv", [128, L], F32)
        cw = gsb("cw", [128, L * 3 * 44], F32)
        cb = gsb("cb", [128, L * 44], F32)
        ident = gsb("ident", [128, 128], BF16)
        ones = gsb("ones", [128, 128], BF16)
        sel = gsb("sel", [128, 64], F32)
        band = gsb("band", [128, 256], BF16)
        negtri = gsb("negtri", [128, 128], F32)
        pw2 = gsb("pw2", [128, NBIS + 1], F32)
        for dst, src in ((gA, gA_d), (gF, gF_d), (gq, gq_d), (gkv, gkv_d), (cw, cw_d), (cb, cb_d),
                         (ident, c_ident), (ones, c_ones), (sel, c_sel), (band, c_band), (negtri, c_negtri), (pw2, c_pw2)):
            SC.dma("sp", dst[:], src[:, :], reads=(), writes=(R_const,))
        R_const.ordered = False

        def mm(out, lhsT, rhs, start, stop, reads, writes):
            SC.op("pe", lambda e: e.matmul(out, lhsT=lhsT, rhs=rhs, start=start, stop=stop), reads, writes)

        def tr(out, in_, reads, writes):
            SC.op("pe", lambda e: e.transpose(out, in_, ident[:]), reads, writes)

        def act(out, in_, func, reads, writes, scale=1.0, bias=None, accum_out=None):
            kw = {}
            if bias is not None:
                kw["bias"] = bias
            if accum_out is not None:
                kw["accum_out"] = accum_out
            SC.op("act", lambda e: e.activation(out=out, in_=in_, func=func, scale=scale, **kw), reads, writes)

        def ts(eng, out, in0, s1, s2, op0, op1, reads, writes, accum_out=None):
            kw = {}
            if accum_out is not None:
                kw["accum_out"] = accum_out
            if op1 is None:
                SC.op(eng, lambda e: e.tensor_scalar(out=out, in0=in0, scalar1=s1, scalar2=None, op0=op0, **kw),
                      reads, writes)
            else:
                SC.op(eng, lambda e: e.tensor_scalar(out=out, in0=in0, scalar1=s1, scalar2=s2, op0=op0, op1=op1, **kw),
                      reads, writes)

        def tt(eng, out, in0, in1, op, reads, writes):
            SC.op(eng, lambda e: e.tensor_tensor(out=out, in0=in0, in1=in1, op=op), reads, writes)

        def stt(out, in0, scalar, in1, op0, op1, reads, writes):
            SC.op("dve", lambda e: e.scalar_tensor_tensor(out=out, in0=in0, scalar=scalar, in1=in1, op0=op0, op1=op1),
                  reads, writes)

        def cp(eng, out, in_, reads, writes):
            if eng == "act":
                SC.op("act", lambda e: e.activation(out=out, in_=in_, func=AF.Copy), reads, writes)
            else:
                SC.op(eng, lambda e: e.tensor_copy(out=out, in_=in_), reads, writes)

        def mset(eng, ap, val, writes):
            SC.op(eng, lambda e: e.memset(ap, val), (), writes)

        uid = [0]
        dq = ["sp", "pool"]
        dqi = [0]

        def dma(out, in_, reads, writes, q=None):
            if q is None:
                q = "pool" if any(w in DRAM_RES for w in writes) else "sp"
            SC.dma(q, out, in_, reads, writes)

        def rms_rows(es, xt, R_xt, hb, R_hb, name):
            pass

        def phase_A(l, xsrc):
            uid[0] += 1
            with ExitStack() as es:
                def sb(name, shape, dt):
                    return es.enter_context(nc.sbuf_tensor(name + "_A_%d" % uid[0], shape, dt))
                Win = sb("Win", [128, 8, NIN], BF16)
                WinR = sb("WinR", [128, 8, 1920], BF16)
                Wki = sb("Wki", [128, 8, 128], BF16)
                Wkp = sb("Wkp", [128, 8, 96], BF16)
                WkpR = sb("WkpR", [128, 8, 96], BF16)
                Wuq = sb("Wuq", [128, 2, 384], BF16)
                WuqR = sb("WuqR", [128, 2, 384], BF16)
                Wukv = sb("Wukv", [128, 512], BF16)
                Wkk = sb("Wkk", [128, 4, 96], BF16)
                Wv = sb("Wv", [128, 256], BF16)
                R_W = Res("W_A")
                stg = Ring([(sb("stg%d" % i, [128, 1508], F32), Res("stg%d" % i)) for i in range(2)])
                for k in range(8):
                    for hf in range(2):
                        st, R_st = stg.next()
                        dma(st[:], w_in[l, 128 * k:128 * k + 128, 1508 * hf:1508 * hf + 1508], (), (R_st,))
                        ts("dve", Win[:, k, 1508 * hf:1508 * hf + 1508], st[:], gA[:, l * 8 + k:l * 8 + k + 1], None,
                           ALU.mult, None, (R_st, R_const), (R_W,))
                for c in range(2):
                    st, R_st = stg.next()
                    dma(st[:, 0:384], w_uq[l, 128 * c:128 * c + 128, :], (), (R_st,))
                    ts("dve", Wuq[:, c, :], st[:, 0:384], gq[:, l * 2 + c:l * 2 + c + 1], None, ALU.mult, None,
                       (R_st, R_const), (R_W,))
                st, R_st = stg.next()
                dma(st[:, 0:512], w_ukv[l, :, :], (), (R_st,))
                ts("dve", Wukv[:], st[:, 0:512], gkv[:, l:l + 1], None, ALU.mult, None, (R_st, R_const), (R_W,))
                mset("pool", WinR[:], 0.0, (R_W,))
                mset("pool", Wkp[:], 0.0, (R_W,))
                mset("pool", WkpR[:], 0.0, (R_W,))
                mset("pool", WuqR[:], 0.0, (R_W,))
                mset("pool", Wkk[:], 0.0, (R_W,))
                RW = (R_W,)
                for k in range(8):
                    for base, nh, rb in ((416, 8, 0), (928, 8, 512), (1952, 4, 1024), (2208, 4, 1280)):
                        src = Win[:, k, base:base + nh * 64].rearrange("p (h e) -> p h e", e=64)
                        dst = WinR[:, k, rb:rb + nh * 64].rearrange("p (h e) -> p h e", e=64)
                        ts("dve", dst[:, :, 0:8], src[:, :, 8:16], -1.0, None, ALU.mult, None, RW, RW)
                        cp("dve", dst[:, :, 8:16], src[:, :, 0:8], RW, RW)
                    src = Win[:, k, 2720:2976].rearrange("p (h e) -> p h e", e=32)
                    dst = WinR[:, k, 1536:1792].rearrange("p (h e) -> p h e", e=32)
                    ts("dve", dst[:, :, 0:4], src[:, :, 4:8], -1.0, None, ALU.mult, None, RW, RW)
                    cp("dve", dst[:, :, 4:8], src[:, :, 0:4], RW, RW)
                    for r in range(4):
                        cp("dve", Wki[:, k, 32 * r:32 * r + 32], Win[:, k, 2976:3008], RW, RW)
                        ts("dve", WinR[:, k, 1792 + 32 * r:1792 + 32 * r + 4], Win[:, k, 2980:2984], -1.0, None,
                           ALU.mult, None, RW, RW)
                        cp("dve", WinR[:, k, 1792 + 32 * r + 4:1792 + 32 * r + 8], Win[:, k, 2976:2980], RW, RW)
                    cp("dve", Wkp[:, k, 64:96], Win[:, k, 384:416], RW, RW)
                    ts("dve", WkpR[:, k, 64:80], Win[:, k, 400:416], -1.0, None, ALU.mult, None, RW, RW)
                    cp("dve", WkpR[:, k, 80:96], Win[:, k, 384:400], RW, RW)
                for c in range(2):
                    src = Wuq[:, c, :].rearrange("p (h e) -> p h e", e=96)
                    dst = WuqR[:, c, :].rearrange("p (h e) -> p h e", e=96)
                    ts("dve", dst[:, :, 64:80], src[:, :, 80:96], -1.0, None, ALU.mult, None, RW, RW)
                    cp("dve", dst[:, :, 80:96], src[:, :, 64:80], RW, RW)
                ukv = Wukv[:].rearrange("p (h t e) -> p h t e", t=2, e=64)
                cp("dve", Wkk[:, :, 0:64], ukv[:, :, 0, :], RW, RW)
                cp("dve", Wv[:].rearrange("p (h e) -> p h e", e=64), ukv[:, :, 1, :], RW, RW)

                xt_r = Ring([(sb("xt%d" % i, [128, D], F32), Res("xt%d" % i)) for i in range(2)])
                junk = sb("junk", [128, D], BF16)
                R_junk = Res("junk")
                hb_r = Ring([(sb("hb%d" % i, [128, D], BF16), Res("hb%d" % i)) for i in range(2)])
                st_r = Ring([(sb("st%d" % i, [128, 4], F32), Res("st%d" % i)) for i in range(2)])
                hT_r = Ring([(sb("hT%d" % i, [128, 8, 512], BF16), Res("hT%d" % i)) for i in range(2)])
                tb_r = Ring([(sb("tb%d" % i, [128, 6, 512], F32), Res("tb%d" % i)) for i in range(2)])
                t1_r = Ring([(sb("t1_%d" % i, [128, 512], F32), Res("t1_%d" % i)) for i in range(2)])
                t2_r = Ring([(sb("t2_%d" % i, [128, 512], F32), Res("t2_%d" % i)) for i in range(2)])
                ob_r = Ring([(sb("ob%d" % i, [128, 512], BF16), Res("ob%d" % i)) for i in range(4)])
                sq_r = Ring([(sb("sq%d" % i, [128, 512], BF16), Res("sq%d" % i)) for i in range(2)])
                rs_r = Ring([(sb("rs%d" % i, [128, 512], F32), Res("rs%d" % i)) for i in range(2)])
                cqn = sb("cqn", [128, 2, 512], BF16)
                R_cqn = Res("cqn")
                ckvn = sb("ckvn", [128, 512], BF16)
                R_ckvn = Res("ckvn")
                vo_r = Ring([(sb("vo%d" % i, [128, 1024], BF16), Res("vo%d" % i)) for i in range(2)])
                wo_r = Ring([(sb("wo%d" % i, [128, 8], F32), Res("wo%d" % i)) for i in range(2)])
                bank = Ring(PS)

                def rope_out(rows, pz, R_pz, pr, R_pr, tC, tS, R_tb, dst_ap):
                    t1, R_t1 = t1_r.next()
                    t2, R_t2 = t2_r.next()
                    ob, R_ob = ob_r.next()
                    tt("dve", t1[0:rows, :], pz[0:rows, :], tC, ALU.mult, (R_pz, R_tb), (R_t1,))
                    tt("dve", t2[0:rows, :], pr[0:rows, :], tS, ALU.mult, (R_pr, R_tb), (R_t2,))
                    tt("pool", ob[0:rows, :], t1[0:rows, :], t2[0:rows, :], ALU.add, (R_t1, R_t2), (R_ob,))
                    dma(dst_ap, ob[0:rows, :], (R_ob,), (R_proj,))

                for ti in range(NT):
                    t0 = 512 * ti
                    hT, R_hT = hT_r.next()
                    for b in range(4):
                        xt, R_xt = xt_r.next()
                        hb, R_hb = hb_r.next()
                        stt_, R_st_ = st_r.next()
                        dma(xt[:], xsrc[t0 + 128 * b:t0 + 128 * b + 128, :], (R_x,), (R_xt,))
                        act(junk[:], xt[:], AF.Square, (R_xt,), (R_junk, R_st_), accum_out=stt_[:, 0:1])
                        act(stt_[:, 1:2], stt_[:, 0:1], AF.Sqrt, (R_st_,), (R_st_,), scale=1.0 / D, bias=EPS)
                        SC.op("dve", lambda e, o=stt_[:, 2:3], i=stt_[:, 1:2]: e.reciprocal(out=o, in_=i), (R_st_,), (R_st_,))
                        ts("dve", hb[:], xt[:], stt_[:, 2:3], None, ALU.mult, None, (R_xt, R_st_), (R_hb,))
                        pt_, R_pt = bank.next()
                        ptb = pt_[:, :].bitcast(BF16)
                        for k in range(8):
                            tr(ptb[:, 128 * k:128 * k + 128], hb[:, 128 * k:128 * k + 128], (R_hb, R_const), (R_pt,))
                        cp("act", hT[:, :, 128 * b:128 * b + 128], ptb.rearrange("p (k t) -> p k t", t=128),
                           (R_pt,), (R_hT,))
                    tb, R_tb = tb_r.next()
                    dma(tb[:, 0, :], tab64[0, :, t0:t0 + 512], (), (R_tb,))
                    dma(tb[:, 1, :], tab64[1, :, t0:t0 + 512], (), (R_tb,))
                    dma(tb[:, 2, :], tabidx[0, :, t0:t0 + 512], (), (R_tb,))
                    dma(tb[:, 3, :], tabidx[1, :, t0:t0 + 512], (), (R_tb,))
                    dma(tb[0:96, 4, :], tabmla[0, :, t0:t0 + 512], (), (R_tb,))
                    dma(tb[0:96, 5, :], tabmla[1, :, t0:t0 + 512], (), (R_tb,))
                    blocks = []
                    for i in range(4):
                        blocks.append((Win, 416 + 128 * i, WinR, 128 * i, 0, QTd[i, :, t0:t0 + 512], 128))
                    for i in range(4):
                        blocks.append((Win, 928 + 128 * i, WinR, 512 + 128 * i, 0, KTd[i, :, t0:t0 + 512], 128))
                    for i in range(2):
                        blocks.append((Win, 1952 + 128 * i, WinR, 1024 + 128 * i, 0, QTs[i, :, t0:t0 + 512], 128))
                    for i in range(2):
                        blocks.append((Win, 2208 + 128 * i, WinR, 1280 + 128 * i, 0, KTs[i, :, t0:t0 + 512], 128))
                    for i in range(3):
                        nr = 96 if i < 2 else 64
                        blocks.append((Win, 2720 + 96 * i, WinR, 1536 + 96 * i, 2, QIT[i, 0:nr, t0:t0 + 512], nr))
                    blocks.append((Wki, 0, WinR, 1792, 2, KIT[:, t0:t0 + 512], 128))
                    for (Wz, cz, Wr, cr, tbi, dst, nr) in blocks:
                        pz, R_pz = bank.next()
                        pr, R_pr = bank.next()
                        for k in range(8):
                            mm(pz[0:nr, :], Wz[:, k, cz:cz + nr], hT[:, k, :], k == 0, k == 7, (R_W, R_hT), (R_pz,))
                        for k in range(8):
                            mm(pr[0:nr, :], Wr[:, k, cr:cr + nr], hT[:, k, :], k == 0, k == 7, (R_W, R_hT), (R_pr,))
                        rope_out(nr, pz, R_pz, pr, R_pr, tb[0:nr, tbi, :], tb[0:nr, tbi + 1, :], R_tb, dst)
                    pq0, R_pq0 = bank.next()
                    pq1, R_pq1 = bank.next()
                    pkv, R_pkv = bank.next()
                    for (pp, R_pp, c0) in ((pq0, R_pq0, 0), (pq1, R_pq1, 128), (pkv, R_pkv, 256)):
                        for k in range(8):
                            mm(pp[:, :], Win[:, k, c0:c0 + 128], hT[:, k, :], k == 0, k == 7, (R_W, R_hT), (R_pp,))
                    pss, R_pss = bank.next()
                    sqs = []
                    for (pp, R_pp) in ((pq0, R_pq0), (pq1, R_pq1)):
                        sq, R_sq = sq_r.next()
                        act(sq[:], pp[:, :], AF.Square, (R_pp,), (R_sq,))
                        sqs.append((sq, R_sq))
                    for i, (sq, R_sq) in enumerate(sqs):
                        mm(pss[:, :], ones[:], sq[:], i == 0, i == 1, (R_const, R_sq), (R_pss,))
                    rs, R_rs = rs_r.next()
                    act(rs[:], pss[:, :], AF.Sqrt, (R_pss,), (R_rs,), scale=1.0 / 256, bias=EPS)
                    SC.op("dve", lambda e, o=rs[:], i=rs[:]: e.reciprocal(out=o, in_=i), (R_rs,), (R_rs,))
                    tt("dve", cqn[:, 0, :], pq0[:, :], rs[:], ALU.mult, (R_pq0, R_rs), (R_cqn,))
                    tt("dve", cqn[:, 1, :], pq1[:, :], rs[:], ALU.mult, (R_pq1, R_rs), (R_cqn,))
                    pss, R_pss = bank.next()
                    sq, R_sq = sq_r.next()
                    act(sq[:], pkv[:, :], AF.Square, (R_pkv,), (R_sq,))
                    mm(pss[:, :], ones[:], sq[:], True, True, (R_const, R_sq), (R_pss,))
                    rs, R_rs = rs_r.next()
                    act(rs[:], pss[:, :], AF.Sqrt, (R_pss,), (R_rs,), scale=1.0 / 128, bias=EPS)
                    SC.op("dve", lambda e, o=rs[:], i=rs[:]: e.reciprocal(out=o, in_=i), (R_rs,), (R_rs,))
                    tt("dve", ckvn[:], pkv[:, :], rs[:], ALU.mult, (R_pkv, R_rs), (R_ckvn,))
                    for h in range(4):
                        pz, R_pz = bank.next()
                        pr, R_pr = bank.next()
                        for c in range(2):
                            mm(pz[0:96, :], Wuq[:, c, 96 * h:96 * h + 96], cqn[:, c, :], c == 0, c == 1,
                               (R_W, R_cqn), (R_pz,))
                        for c in range(2):
                            mm(pr[0:96, :], WuqR[:, c, 96 * h:96 * h + 96], cqn[:, c, :], c == 0, c == 1,
                               (R_W, R_cqn), (R_pr,))
                        rope_out(96, pz, R_pz, pr, R_pr, tb[0:96, 4, :], tb[0:96, 5, :], R_tb, QTm[h, :, t0:t0 + 512])
                    for h in range(4):
                        pz, R_pz = bank.next()
                        pr, R_pr = bank.next()
                        mm(pz[0:96, :], Wkk[:, h, :], ckvn[:], True, False, (R_W, R_ckvn), (R_pz,))
                        for k in range(8):
                            mm(pz[0:96, :], Wkp[:, k, :], hT[:, k, :], False, k == 7, (R_W, R_hT), (R_pz,))
                        for k in range(8):
                            mm(pr[0:96, :], WkpR[:, k, :], hT[:, k, :], k == 0, k == 7, (R_W, R_hT), (R_pr,))
                        rope_out(96, pz, R_pz, pr, R_pr, tb[0:96, 4, :], tb[0:96, 5, :], R_tb, KTm[h, :, t0:t0 + 512])
                    for b in range(4):
                        tsl = slice(128 * b, 128 * b + 128)
                        r0 = t0 + 128 * b
                        vo, R_vo = vo_r.next()
                        wo, R_wo = wo_r.next()
                        p1, R_p1 = bank.next()
                        for k in range(8):
                            mm(p1[:, :], hT[:, k, tsl], Win[:, k, 1440:1952], k == 0, k == 7, (R_W, R_hT), (R_p1,))
                        cp("act", vo[:, 0:512], p1[:, :], (R_p1,), (R_vo,))
                        p2, R_p2 = bank.next()
                        for k in range(8):
                            mm(p2[:, 0:256], hT[:, k, tsl], Win[:, k, 2464:2720], k == 0, k == 7, (R_W, R_hT), (R_p2,))
                        cp("act", vo[:, 512:768], p2[:, 0:256], (R_p2,), (R_vo,))
                        p3, R_p3 = bank.next()
                        for k in range(8):
                            mm(p3[:, 0:8], hT[:, k, tsl], Win[:, k, 3008:3016], k == 0, k == 7, (R_W, R_hT), (R_p3,))
                        cp("act", wo[:], p3[:, 0:8], (R_p3,), (R_wo,))
                        p4, R_p4 = bank.next()
                        mm(p4[:, 0:256], ckvn[:, tsl], Wv[:], True, True, (R_W, R_ckvn), (R_p4,))
                        cp("act", vo[:, 768:1024], p4[:, 0:256], (R_p4,), (R_vo,))
                        dma(Vd[r0:r0 + 128, :], vo[:, 0:512], (R_vo,), (R_proj,))
                        dma(Vs[r0:r0 + 128, :], vo[:, 512:768], (R_vo,), (R_proj,))
                        dma(Vm[r0:r0 + 128, :], vo[:, 768:1024], (R_vo,), (R_proj,))
                        dma(WI[r0:r0 + 128, :], wo[:], (R_wo,), (R_proj,))
                SC.barrier()


        def phase_B():
            scale = 96 ** -0.5
            uid[0] += 1
            with ExitStack() as es:
                def sb(name, shape, dt):
                    return es.enter_context(nc.sbuf_tensor(name + "_B_%d" % uid[0], shape, dt))
                KT = sb("KT", [96, S], BF16)
                QT = sb("QT", [96, S], BF16)
                Vx = sb("Vx", [128, NB, 66], BF16)
                R_K, R_Q, R_V = Res("K"), Res("Q"), Res("V")
                pt_r = Ring([(sb("pt%d" % i, [128, 512], BF16), Res("pt%d" % i)) for i in range(3)])
                os_r = Ring([(sb("os%d" % i, [128, 512], F32), Res("os%d" % i)) for i in range(2)])
                rr_r = Ring([(sb("rr%d" % i, [64, 512], F32), Res("rr%d" % i)) for i in range(2)])
                ob_r = Ring([(sb("ob%d" % i, [64, 512], BF16), Res("ob%d" % i)) for i in range(2)])
                s_r = Ring(PS[0:4])
                od_r = Ring(PS[4:6])
                pd_r = Ring(PS[6:8])
                mset("pool", Vx[:, :, 64:66], 1.0, (R_V,))
                for h in range(4):
                    dma(KT[:], KTm[h, :, :], (R_proj,), (R_K,), q="sp")
                    dma(QT[:], QTm[h, :, :], (R_proj,), (R_Q,), q="sp")
                    dma(Vx[:, :, 0:64], Vm[:, 64 * h:64 * h + 64].rearrange("(j p) e -> p j e", p=128),
                        (R_proj,), (R_V,), q="sp")
                    for g in range(NT):
                        od, R_od = od_r.next()
                        nj = 4 * g + 4
                        for j in range(nj):
                            jj = j - 4 * g
                            c0 = 128 * jj if jj > 0 else 0
                            ps, R_ps = s_r.next()
                            mm(ps[:, c0:512], KT[:, 128 * j:128 * j + 128], QT[:, 512 * g + c0:512 * g + 512],
                               True, True, (R_K, R_Q), (R_ps,))
                            pt, R_pt = pt_r.next()
                            act(pt[:, c0:512], ps[:, c0:512], AF.Exp, (R_ps,), (R_pt,), scale=scale)
                            if jj >= 0:
                                tt("dve", pt[:, 128 * jj:128 * jj + 128], pt[:, 128 * jj:128 * jj + 128],
                                   band[:, 0:128], ALU.mult, (R_pt, R_const), (R_pt,))
                            mm(od[0:65, c0:512], Vx[:, j, 0:65], pt[:, c0:512], j == 0, j == nj - 1,
                               (R_V, R_pt), (R_od,))
                        osb, R_os = os_r.next()
                        cp("act", osb[0:65, :], od[0:65, :], (R_od,), (R_os,))
                        pd, R_pd = pd_r.next()
                        mm(pd[0:64, :], sel[0:65, :], osb[0:65, :], True, True, (R_const, R_os), (R_pd,))
                        rr, R_rr = rr_r.next()
                        SC.op("dve", lambda e, o=rr[:, :], i=pd[0:64, :]: e.reciprocal(out=o, in_=i), (R_pd,), (R_rr,))
                        ob, R_ob = ob_r.next()
                        tt("dve", ob[:, :], osb[0:64, :], rr[:, :], ALU.mult, (R_os, R_rr), (R_ob,))
                        dma(mixT[64 * h:64 * h + 64, 512 * g:512 * g + 512], ob[:, :], (R_ob,), (R_mix,))
                SC.barrier()

        def phase_C():
            scale = 64 ** -0.5
            uid[0] += 1
            with ExitStack() as es:
                def sb(name, shape, dt):
                    return es.enter_context(nc.sbuf_tensor(name + "_C_%d" % uid[0], shape, dt))
                KT = sb("KT", [128, S], BF16)
                QT = sb("QT", [128, S], BF16)
                R_K, R_Q = Res("K"), Res("Q")
                vx_r = Ring([(sb("Vx%d" % i, [128, NB, 66], BF16), Res("Vx%d" % i)) for i in range(2)])
                acc = sb("acc", [128, S], F32)
                R_acc = Res("acc")
                pt_r = Ring([(sb("pt%d" % i, [128, 256], BF16), Res("pt%d" % i)) for i in range(3)])
                rr_r = Ring([(sb("rr%d" % i, [64, 512], F32), Res("rr%d" % i)) for i in range(2)])
                ob_r = Ring([(sb("ob%d" % i, [64, 512], BF16), Res("ob%d" % i)) for i in range(2)])
                s_r = Ring(PS[0:4])
                od_b = PS[4:6]
                pd_r = Ring(PS[6:8])
                for (vx, R_vx) in vx_r.items:
                    mset("pool", vx[:, :, 64:66], 1.0, (R_vx,))
                for i in range(4):
                    dma(KT[:], KTd[i, :, :], (R_proj,), (R_K,), q="sp")
                    dma(QT[:], QTd[i, :, :], (R_proj,), (R_Q,), q="sp")
                    for hh in range(2):
                        h = 2 * i + hh
                        rows = slice(64 * hh, 64 * hh + 64)
                        for pi, d in enumerate((1, 4, 16)):
                            vx, R_vx = vx_r.next()
                            vsrc = Vd[:, 64 * h:64 * h + 64].rearrange("(n i d) e -> i n d e", i=128, d=d)
                            for r in range(d):
                                dma(vx[:, r:NB:d, 0:64], vsrc[:, :, r, :], (R_proj,), (R_vx,), q="sp")
                            nblk = S // (128 * d)
                            for r in range(d):
                                for m in range(nblk):
                                    blk = m * d + r
                                    ks = 128 * m * d + r
                                    nq = 256 if m + 1 < nblk else 128
                                    ps, R_ps = s_r.next()
                                    mm(ps[:, 0:nq], KT[rows, ks:ks + 127 * d + 1:d], QT[rows, ks:ks + (nq - 1) * d + 1:d],
                                       True, True, (R_K, R_Q), (R_ps,))
                                    pt, R_pt = pt_r.next()
                                    act(pt[:, 0:nq], ps[:, 0:nq], AF.Exp, (R_ps,), (R_pt,), scale=scale)
                                    tt("dve", pt[:, 0:nq], pt[:, 0:nq], band[:, 0:nq], ALU.mult, (R_pt, R_const), (R_pt,))
                                    od, R_od = od_b[m % 2]
                                    mm(od[0:65, 0:128], vx[:, blk, 0:65], pt[:, 0:128], m == 0, True,
                                       (R_vx, R_pt), (R_od,))
                                    dst = acc[0:65, ks:ks + 127 * d + 1:d]
                                    if pi == 0:
                                        cp("dve", dst, od[0:65, 0:128], (R_od,), (R_acc,))
                                    else:
                                        tt("dve", dst, dst, od[0:65, 0:128], ALU.add, (R_od, R_acc), (R_acc,))
                                    if m + 1 < nblk:
                                        od2, R_od2 = od_b[(m + 1) % 2]
                                        mm(od2[0:65, 0:128], vx[:, blk, 0:65], pt[:, 128:256], True, False,
                                           (R_vx, R_pt), (R_od2,))
                        for g in range(NT):
                            pd, R_pd = pd_r.next()
                            mm(pd[0:64, :], sel[0:65, :], acc[0:65, 512 * g:512 * g + 512], True, True,
                               (R_const, R_acc), (R_pd,))
                            rr, R_rr = rr_r.next()
                            SC.op("dve", lambda e, o=rr[:, :], i_=pd[0:64, :]: e.reciprocal(out=o, in_=i_), (R_pd,), (R_rr,))
                            ob, R_ob = ob_r.next()
                            tt("dve", ob[:, :], acc[0:64, 512 * g:512 * g + 512], rr[:, :], ALU.mult,
                               (R_acc, R_rr), (R_ob,))
                            dma(mixT[256 + 64 * h:256 + 64 * h + 64, 512 * g:512 * g + 512], ob[:, :], (R_ob,), (R_mix,))
                SC.barrier()

        def phase_D():
            scale = 64 ** -0.5
            uid[0] += 1
            with ExitStack() as es:
                def sb(name, shape, dt):
                    return es.enter_context(nc.sbuf_tensor(name + "_D_%d" % uid[0], shape, dt))
                KI = sb("KI", [128, S], BF16)
                KT = sb("KT", [128, 2, S], BF16)
                Vx = sb("Vx", [128, NB, 4, 66], BF16)
                WIt = sb("WIt", [128, NB, 8], F32)
                R_KI, R_K, R_V, R_WI = Res("KI"), Res("K"), Res("V"), Res("WI")
                Isc = sb("Isc", [128, S], F32)
                Msk = sb("Msk", [128, S], BF16)
                MskT = sb("MskT", [128, NB, 128], BF16)
                R_I, R_M, R_MT = Res("I"), Res("M"), Res("MT")
                qi_r = Ring([(sb("qi%d" % i, [96, 3, 128], BF16), Res("qi%d" % i)) for i in range(2)])
                qs_r = Ring([(sb("qs%d" % i, [128, 2, 128], BF16), Res("qs%d" % i)) for i in range(2)])
                rl_r = Ring([(sb("rl%d" % i, [128, 512], F32), Res("rl%d" % i)) for i in range(4)])
                pt_r = Ring([(sb("pt%d" % i, [128, 512], BF16), Res("pt%d" % i)) for i in range(4)])
                st_r = Ring([(sb("st%d" % i, [128, 8], F32), Res("st%d" % i)) for i in range(2)])
                wk_r = Ring([(sb("wk%d" % i, [128, NBIS + 1], F32), Res("wk%d" % i)) for i in range(2)])
                os_r = Ring([(sb("os%d" % i, [128, 128], F32), Res("os%d" % i)) for i in range(2)])
                rr_r = Ring([(sb("rr%d" % i, [64, 128], F32), Res("rr%d" % i)) for i in range(2)])
                ob_r = Ring([(sb("ob%d" % i, [64, 128], BF16), Res("ob%d" % i)) for i in range(2)])
                i_r = Ring(PS[0:3])
                t_b = PS[3]
                s_r = Ring(PS[4:6])
                od_b = PS[6]
                pd_b = PS[7]
                mset("pool", Vx[:, :, :, 64:66], 1.0, (R_V,))
                dma(KI[:], KIT[:, :], (R_proj,), (R_KI,), q="sp")
                for i in range(2):
                    dma(KT[:, i, :], KTs[i, :, :], (R_proj,), (R_K,), q="sp")
                for h in range(4):
                    dma(Vx[:, :, h, 0:64], Vs[:, 64 * h:64 * h + 64].rearrange("(j p) e -> p j e", p=128),
                        (R_proj,), (R_V,), q="sp")
                dma(WIt[:], WI.rearrange("(j p) h -> p j h", p=128), (R_proj,), (R_WI,), q="sp")
                qs_of = {}

                def idx_part(qb):
                    nk = 128 * (qb + 1)
                    nch = (nk + 511) // 512
                    qi, R_qi = qi_r.next()
                    qs, R_qs = qs_r.next()
                    dma(qi[:, 0:2, :], QIT[0:2, :, 128 * qb:128 * qb + 128].rearrange("i p t -> p i t"), (R_proj,), (R_qi,))
                    dma(qi[0:64, 2, :], QIT[2, 0:64, 128 * qb:128 * qb + 128], (R_proj,), (R_qi,))
                    dma(qs[:], QTs[:, :, 128 * qb:128 * qb + 128].rearrange("i p t -> p i t"), (R_proj,), (R_qs,))
                    qs_of[qb] = (qs, R_qs)
                    for c in range(nch):
                        ncol = min(512, nk - 512 * c)
                        cs = slice(512 * c, 512 * c + ncol)
                        for hh in range(8):
                            po = 32 * (hh % 3)
                            bi = hh // 3
                            ps, R_ps = i_r.next()
                            mm(ps[:, 0:ncol], qi[po:po + 32, bi, :], KI[po:po + 32, cs], True, True,
                               (R_qi, R_KI), (R_ps,))
                            rl, R_rl = rl_r.next()
                            act(rl[:, 0:ncol], ps[:, 0:ncol], AF.Relu, (R_ps,), (R_rl,))
                            if hh == 0:
                                ts("dve", Isc[:, cs], rl[:, 0:ncol], WIt[:, qb, 0:1], None, ALU.mult, None,
                                   (R_rl, R_WI), (R_I,))
                            else:
                                stt(Isc[:, cs], rl[:, 0:ncol], WIt[:, qb, hh:hh + 1], Isc[:, cs], ALU.mult, ALU.add,
                                    (R_rl, R_WI, R_I), (R_I,))
                    dg = slice(128 * qb, 128 * qb + 128)
                    tt("dve", Isc[:, dg], Isc[:, dg], negtri[:], ALU.add, (R_I, R_const), (R_I,))

                def bisect_part(qb):
                    nk = 128 * (qb + 1)
                    if nk > TOPK:
                        st, R_st = st_r.next()
                        RS = (R_st,)
                        SC.op("dve", lambda e, o=st[:, 0:1], i_=Isc[:, 0:nk - 128]: e.tensor_reduce(out=o, in_=i_, axis=AX.X, op=ALU.min),
                              (R_I,), RS)
                        SC.op("dve", lambda e, o=st[:, 1:2], i_=Isc[:, 0:nk]: e.tensor_reduce(out=o, in_=i_, axis=AX.X, op=ALU.max),
                              (R_I,), RS)
                        tt("dve", st[:, 1:2], st[:, 1:2], st[:, 0:1], ALU.subtract, RS, RS)
                        wk, R_wk = wk_r.next()
                        ts("dve", wk[:], pw2[:], st[:, 1:2], None, ALU.mult, None, (R_st, R_const), (R_wk,))
                        stt(st[:, 2:3], st[:, 1:2], 0.5, st[:, 0:1], ALU.mult, ALU.add, RS, RS)
                        for it in range(NBIS):
                            ts("dve", Msk[:, 0:nk], Isc[:, 0:nk], st[:, 2:3], 0.0, ALU.is_ge, ALU.add,
                               (R_I, R_st), (R_M, R_st), accum_out=st[:, 3:4])
                            ts("dve", st[:, 4:5], st[:, 3:4], TOPK - 0.5, 0.5, ALU.is_ge, ALU.subtract, RS, RS)
                            stt(st[:, 2:3], st[:, 4:5], wk[:, it:it + 1], st[:, 2:3], ALU.mult, ALU.add,
                                (R_st, R_wk), RS)
                        stt(st[:, 0:1], wk[:, NBIS:NBIS + 1], -1.0, st[:, 2:3], ALU.mult, ALU.add, (R_st, R_wk), RS)
                        ts("dve", Msk[:, 0:nk], Isc[:, 0:nk], st[:, 0:1], None, ALU.is_ge, None, (R_I, R_st), (R_M,))
                    else:
                        ts("dve", Msk[:, 0:nk], Isc[:, 0:nk], -1.0e29, None, ALU.is_ge, None, (R_I,), (R_M,))
                    tb_, R_tb_ = t_b
                    tbb = tb_[:, :].bitcast(BF16)
                    for j0 in range(0, qb + 1, 8):
                        n8 = min(8, qb + 1 - j0)
                        for jj in range(n8):
                            j = j0 + jj
                            tr(tbb[:, 128 * jj:128 * jj + 128], Msk[:, 128 * j:128 * j + 128], (R_M, R_const), (R_tb_,))
                        cp("act", MskT[:, j0:j0 + n8, :], tbb[:, 0:128 * n8].rearrange("p (j t) -> p j t", t=128),
                           (R_tb_,), (R_MT,))

                def attn_part(qb):
                    nk = 128 * (qb + 1)
                    nch = (nk + 511) // 512
                    qs, R_qs = qs_of.pop(qb)
                    for h in range(4):
                        rows = slice(64 * (h % 2), 64 * (h % 2) + 64)
                        bi = h // 2
                        od, R_od = od_b
                        for c in range(nch):
                            nbc = min(4, qb + 1 - 4 * c)
                            ps, R_ps = s_r.next()
                            for jj in range(nbc):
                                j = 4 * c + jj
                                mm(ps[:, 128 * jj:128 * jj + 128], KT[rows, bi, 128 * j:128 * j + 128], qs[rows, bi, :],
                                   True, True, (R_K, R_qs), (R_ps,))
                            pt, R_pt = pt_r.next()
                            act(pt[:, 0:128 * nbc], ps[:, 0:128 * nbc], AF.Exp, (R_ps,), (R_pt,), scale=scale)
                            tt("dve", pt[:, 0:128 * nbc], pt[:, 0:128 * nbc],
                               MskT[:, 4 * c:4 * c + nbc, :].rearrange("p j t -> p (j t)"), ALU.mult,
                               (R_pt, R_MT), (R_pt,))
                            for jj in range(nbc):
                                j = 4 * c + jj
                                mm(od[0:65, 0:128], Vx[:, j, h, 0:65], pt[:, 128 * jj:128 * jj + 128], j == 0, j == qb,
                                   (R_V, R_pt), (R_od,))
                        osb, R_os = os_r.next()
                        cp("act", osb[0:65, :], od[0:65, 0:128], (R_od,), (R_os,))
                        pd, R_pd = pd_b
                        mm(pd[0:64, 0:128], sel[0:65, :], osb[0:65, :], True, True, (R_const, R_os), (R_pd,))
                        rr, R_rr = rr_r.next()
                        SC.op("dve", lambda e, o=rr[:, :], i_=pd[0:64, 0:128]: e.reciprocal(out=o, in_=i_), (R_pd,), (R_rr,))
                        ob, R_ob = ob_r.next()
                        tt("dve", ob[:, :], osb[0:64, :], rr[:, :], ALU.mult, (R_os, R_rr), (R_ob,))
                        dma(mixT[768 + 64 * h:768 + 64 * h + 64, 128 * qb:128 * qb + 128], ob[:, :], (R_ob,), (R_mix,))

                for qb in range(NB):
                    idx_part(qb)
                    if qb > 0:
                        attn_part(qb - 1)
                    bisect_part(qb)
                attn_part(NB - 1)
                SC.barrier()

        def norm_T(es_rings, xt, R_xt, hT, R_hT, b, bank):
            junk, R_junk, hb_r, st_r = es_rings
            hb, R_hb = hb_r.next()
            stt_, R_st_ = st_r.next()
            act(junk[:], xt[:], AF.Square, (R_xt,), (R_junk, R_st_), accum_out=stt_[:, 0:1])
            act(stt_[:, 1:2], stt_[:, 0:1], AF.Sqrt, (R_st_,), (R_st_,), scale=1.0 / D, bias=EPS)
            SC.op("dve", lambda e, o=stt_[:, 2:3], i=stt_[:, 1:2]: e.reciprocal(out=o, in_=i), (R_st_,), (R_st_,))
            ts("dve", hb[:], xt[:], stt_[:, 2:3], None, ALU.mult, None, (R_xt, R_st_), (R_hb,))
            pt_, R_pt = bank.next()
            ptb = pt_[:, :].bitcast(BF16)
            for k in range(8):
                tr(ptb[:, 128 * k:128 * k + 128], hb[:, 128 * k:128 * k + 128], (R_hb, R_const), (R_pt,))
            cp("act", hT[:, :, 128 * b:128 * b + 128], ptb.rearrange("p (k t) -> p k t", t=128), (R_pt,), (R_hT,))

        def phase_E1(l, xsrc, R_xs):
            uid[0] += 1
            with ExitStack() as es:
                def sb(name, shape, dt):
                    return es.enter_context(nc.sbuf_tensor(name + "_E1_%d" % uid[0], shape, dt))
                Wo = sb("Wo", [128, 8, D], BF16)
                Wup = sb("Wup", [128, 8, 2 * DFF], BF16)
                R_W = Res("W_E1")
                stg = Ring([(sb("stg%d" % i, [128, 2048], F32), Res("stg%d" % i)) for i in range(2)])
                for k in range(8):
                    st, R_st = stg.next()
                    dma(st[:, 0:1024], w_o[l, 128 * k:128 * k + 128, :], (), (R_st,))
                    cp("act", Wo[:, k, :], st[:, 0:1024], (R_st,), (R_W,))
                    for (c0, cn) in ((0, 2048), (2048, 2048), (4096, 1536)):
                        st, R_st = stg.next()
                        dma(st[:, 0:cn], w_up[l, 128 * k:128 * k + 128, c0:c0 + cn], (), (R_st,))
                        ts("dve", Wup[:, k, c0:c0 + cn], st[:, 0:cn], gF[:, l * 8 + k:l * 8 + k + 1], None, ALU.mult, None,
                           (R_st, R_const), (R_W,))
                halo = sb("halo", [128, 44, 2], F32)
                R_halo = Res("halo")
                mset("pool", halo[:], 0.0, (R_halo,))
                mT_r = Ring([(sb("mT%d" % i, [128, 8, 512], BF16), Res("mT%d" % i)) for i in range(2)])
                hT_r = Ring([(sb("hT%d" % i, [128, 8, 512], BF16), Res("hT%d" % i)) for i in range(2)])
                xt_r = Ring([(sb("xt%d" % i, [128, D], F32), Res("xt%d" % i)) for i in range(2)])
                x1_r = Ring([(sb("x1%d" % i, [128, D], F32), Res("x1%d" % i)) for i in range(2)])
                junk = sb("junk", [128, D], BF16)
                R_junk = Res("junk")
                hb_r = Ring([(sb("hb%d" % i, [128, D], BF16), Res("hb%d" % i)) for i in range(2)])
                st_r = Ring([(sb("st%d" % i, [128, 4], F32), Res("st%d" % i)) for i in range(2)])
                ub_r = Ring([(sb("ub%d" % i, [128, 514], F32), Res("ub%d" % i)) for i in range(3)])
                y_r = Ring([(sb("y%d" % i, [128, 512], F32), Res("y%d" % i)) for i in range(4)])
                sg_r = Ring([(sb("sg%d" % i, [128, 512], F32), Res("sg%d" % i)) for i in range(2)])
                ab_r = Ring([(sb("ab%d" % i, [128, 512], BF16), Res("ab%d" % i)) for i in range(3)])
                bank = Ring(PS)
                rings = (junk, R_junk, hb_r, st_r)
                for ti in range(NT):
                    t0 = 512 * ti
                    mT, R_mT = mT_r.next()
                    hT, R_hT = hT_r.next()
                    dma(mT[:], mixT[:, t0:t0 + 512].rearrange("(k p) t -> p k t", p=128), (R_mix,), (R_mT,), q="sp")
                    for b in range(4):
                        r0 = t0 + 128 * b
                        xt, R_xt = xt_r.next()
                        x1, R_x1t = x1_r.next()
                        dma(xt[:], xsrc[r0:r0 + 128, :], (R_xs,), (R_xt,))
                        for hf in range(2):
                            ps, R_ps = bank.next()
                            for k in range(8):
                                mm(ps[:, :], mT[:, k, 128 * b:128 * b + 128], Wo[:, k, 512 * hf:512 * hf + 512],
                                   k == 0, k == 7, (R_mT, R_W), (R_ps,))
                            tt("dve", x1[:, 512 * hf:512 * hf + 512], ps[:, :], xt[:, 512 * hf:512 * hf + 512], ALU.add,
                               (R_ps, R_xt), (R_x1t,))
                        dma(xres1[r0:r0 + 128, :], x1[:], (R_x1t,), (R_x1,))
                        norm_T(rings, x1, R_x1t, hT, R_hT, b, bank)
                    for f in range(22):
                        ys = []
                        for fc in (f, 22 + f):
                            ps, R_ps = bank.next()
                            for k in range(8):
                                mm(ps[:, :], Wup[:, k, 128 * fc:128 * fc + 128], hT[:, k, :], k == 0, k == 7,
                                   (R_W, R_hT), (R_ps,))
                            ub, R_ub = ub_r.next()
                            cp("dve", ub[:, 0:2], halo[:, fc, :], (R_halo,), (R_ub,))
                            cp("act", ub[:, 2:514], ps[:, :], (R_ps,), (R_ub,))
                            cp("dve", halo[:, fc, :], ub[:, 512:514], (R_ub,), (R_halo,))
                            y, R_y_ = y_r.next()
                            ci = l * 132 + fc
                            ts("dve", y[:], ps[:, :], cw[:, ci + 88:ci + 89], cb[:, l * 44 + fc:l * 44 + fc + 1],
                               ALU.mult, ALU.add, (R_ps, R_const), (R_y_,))
                            stt(y[:], ub[:, 1:513], cw[:, ci + 44:ci + 45], y[:], ALU.mult, ALU.add, (R_ub, R_const, R_y_), (R_y_,))
                            stt(y[:], ub[:, 0:512], cw[:, ci:ci + 1], y[:], ALU.mult, ALU.add, (R_ub, R_const, R_y_), (R_y_,))
                            ys.append((y, R_y_))
                        sg, R_sg = sg_r.next()
                        act(sg[:], ys[0][0][:], AF.Silu, (ys[0][1],), (R_sg,))
                        ab, R_ab = ab_r.next()
                        tt("dve", ab[:], sg[:], ys[1][0][:], ALU.mult, (R_sg, ys[1][1]), (R_ab,))
                        dma(actT[128 * f:128 * f + 128, t0:t0 + 512], ab[:], (R_ab,), (R_act,))
                SC.barrier()

        def phase_E2(l, last):
            uid[0] += 1
            with ExitStack() as es:
                def sb(name, shape, dt):
                    return es.enter_context(nc.sbuf_tensor(name + "_E2_%d" % uid[0], shape, dt))
                Wdn = sb("Wdn", [128, 22, D], BF16)
                R_W = Res("W_E2")
                stg = Ring([(sb("stg%d" % i, [128, D], F32), Res("stg%d" % i)) for i in range(2)])
                for f in range(22):
                    st, R_st = stg.next()
                    dma(st[:], w_down[l, 128 * f:128 * f + 128, :], (), (R_st,))
                    cp("act" if f % 2 else "dve", Wdn[:, f, :], st[:], (R_st,), (R_W,))
                gfin = sb("gfin", [128, D], F32)
                R_gf = Res("gfin")
                if last:
                    dma(gfin[:], gfin_d[:, :], (), (R_gf,))
                aT_r = Ring([(sb("aT%d" % i, [128, 22, 512], BF16), Res("aT%d" % i)) for i in range(2)])
                xt_r = Ring([(sb("xt%d" % i, [128, D], F32), Res("xt%d" % i)) for i in range(2)])
                x2_r = Ring([(sb("x2%d" % i, [128, D], F32), Res("x2%d" % i)) for i in range(2)])
                yo_r = Ring([(sb("yo%d" % i, [128, D], F32), Res("yo%d" % i)) for i in range(2)])
                junk = sb("junk", [128, D], BF16)
                R_junk = Res("junk")
                st_r = Ring([(sb("st%d" % i, [128, 4], F32), Res("st%d" % i)) for i in range(2)])
                bank = Ring(PS)
                for ti in range(NT):
                    t0 = 512 * ti
                    aT, R_aT = aT_r.next()
                    dma(aT[:], actT[:, t0:t0 + 512].rearrange("(f p) t -> p f t", p=128), (R_act,), (R_aT,), q="sp")
                    for b in range(4):
                        r0 = t0 + 128 * b
                        xt, R_xt = xt_r.next()
                        x2, R_x2 = x2_r.next()
                        dma(xt[:], xres1[r0:r0 + 128, :], (R_x1,), (R_xt,))
                        for hf in range(2):
                            ps, R_ps = bank.next()
                            for f in range(22):
                                mm(ps[:, :], aT[:, f, 128 * b:128 * b + 128], Wdn[:, f, 512 * hf:512 * hf + 512],
                                   f == 0, f == 21, (R_aT, R_W), (R_ps,))
                            tt("dve", x2[:, 512 * hf:512 * hf + 512], ps[:, :], xt[:, 512 * hf:512 * hf + 512], ALU.add,
                               (R_ps, R_xt), (R_x2,))
                        if not last:
                            dma(xres0[r0:r0 + 128, :], x2[:], (R_x2,), (R_x,))
                        else:
                            stt_, R_st_ = st_r.next()
                            yo, R_yo = yo_r.next()
                            act(junk[:], x2[:], AF.Square, (R_x2,), (R_junk, R_st_), accum_out=stt_[:, 0:1])
                            act(stt_[:, 1:2], stt_[:, 0:1], AF.Sqrt, (R_st_,), (R_st_,), scale=1.0 / D, bias=EPS)
                            SC.op("dve", lambda e, o=stt_[:, 2:3], i=stt_[:, 1:2]: e.reciprocal(out=o, in_=i), (R_st_,), (R_st_,))
                            stt(yo[:], x2[:], stt_[:, 2:3], gfin[:], ALU.mult, ALU.mult, (R_x2, R_st_, R_gf), (R_yo,))
                            dma(y_out[r0:r0 + 128, :], yo[:], (R_yo,), (R_y,))
                SC.barrier()

        for l in range(L):
            xsrc = x_in if l == 0 else xres0
            phase_A(l, xsrc)
            if stop_after == "A":
                break
            if "B" not in skip:
                phase_B()
            if stop_after == "B":
                break
            if "C" not in skip:
                phase_C()
            if stop_after == "C":
                break
            if "D" not in skip:
                phase_D()
            if stop_after == "D":
                break
            phase_E1(l, xsrc, R_x)
            if stop_after == "E1":
                break
            phase_E2(l, l == L - 1)
        SC.barrier()
        print("instructions:", SC.ninst)
    return nc


def make_consts(S):
    bf = ml_dtypes.bfloat16
    c = {}
    c["c_ident"] = np.eye(128, dtype=np.float32).astype(bf)
    c["c_ones"] = np.ones((128, 128), np.float32).astype(bf)
    sel = np.zeros((128, 64), np.float32)
    sel[64, :] = 1.0
    c["c_sel"] = sel
    k = np.arange(128)[:, None]
    q = np.arange(128)[None, :]
    c["c_band"] = np.concatenate([(k <= q), (k >= q)], axis=1).astype(np.float32).astype(bf)
    c["c_negtri"] = np.where(q.T >= k.T, 0.0, NEG).astype(np.float32) if False else \
        np.where(np.arange(128)[None, :] <= np.arange(128)[:, None], 0.0, NEG).astype(np.float32)
    c["c_pw2"] = np.ascontiguousarray(np.broadcast_to(
        (2.0 ** -(np.arange(NBIS + 1, dtype=np.float32) + 1.0)).astype(np.float32)[None, :], (128, NBIS + 1)))
    t = np.arange(S, dtype=np.float32)

    def tables(dim):
        inv = np.power(np.float32(500000.0), -np.arange(0, dim, 2, dtype=np.float32) / np.float32(dim)).astype(np.float32)
        ang = (t[:, None] * inv[None, :]).astype(np.float32)
        return np.cos(ang).astype(np.float32).T, np.sin(ang).astype(np.float32).T
    ch, sh = tables(16)
    ci, si = tables(8)
    ca, sa = tables(32)
    C = np.ones((128, S), np.float32)
    Sn = np.zeros((128, S), np.float32)
    for hh in range(2):
        C[64 * hh:64 * hh + 8] = ch
        C[64 * hh + 8:64 * hh + 16] = ch
        Sn[64 * hh:64 * hh + 8] = sh
        Sn[64 * hh + 8:64 * hh + 16] = sh
    c["tab64"] = np.stack([C, Sn])
    C = np.ones((128, S), np.float32)
    Sn = np.zeros((128, S), np.float32)
    for hh in range(4):
        C[32 * hh:32 * hh + 4] = ci
        C[32 * hh + 4:32 * hh + 8] = ci
        Sn[32 * hh:32 * hh + 4] = si
        Sn[32 * hh + 4:32 * hh + 8] = si
    c["tabidx"] = np.stack([C, Sn])
    C = np.ones((96, S), np.float32)
    Sn = np.zeros((96, S), np.float32)
    C[64:80] = ca
    C[80:96] = ca
    Sn[64:80] = sa
    Sn[80:96] = sa
    c["tabmla"] = np.stack([C, Sn])
    return c


def layout_params(inp, L):
    f = np.float32
    o = {}
    o["gA"] = np.ascontiguousarray(np.asarray(inp["g_attn"], f).reshape(L, 8, 128).transpose(2, 0, 1).reshape(128, L * 8))
    o["gF"] = np.ascontiguousarray(np.asarray(inp["g_ffn"], f).reshape(L, 8, 128).transpose(2, 0, 1).reshape(128, L * 8))
    o["gq"] = np.ascontiguousarray(np.asarray(inp["g_q_lat"], f).reshape(L, 2, 128).transpose(2, 0, 1).reshape(128, L * 2))
    o["gkv"] = np.ascontiguousarray(np.asarray(inp["g_kv_lat"], f).reshape(L, 128).T)
    o["cw"] = np.ascontiguousarray(np.asarray(inp["conv_w"], f).reshape(L, 3, 44, 128).transpose(3, 0, 1, 2).reshape(128, L * 3 * 44))
    o["cb"] = np.ascontiguousarray(np.asarray(inp["conv_b"], f).reshape(L, 44, 128).transpose(2, 0, 1).reshape(128, L * 44))
    o["gfin"] = np.ascontiguousarray(np.broadcast_to(np.asarray(inp["g_final"], f).reshape(1, D), (128, D)))
    for k in ("w_in", "w_uq", "w_ukv", "w_o", "w_up", "w_down"):
        o[k] = np.ascontiguousarray(np.asarray(inp[k], f))
    return o


_CACHE = {}


def kernel(**inputs):
    x = np.asarray(inputs["x"], np.float32)
    B, S, _ = x.shape
    L = inputs["w_in"].shape[0]
    key = (S, L)
    if key not in _CACHE:
        _CACHE[key] = (build(S, L), make_consts(S))
    nc, consts = _CACHE[key]
    shared = layout_params(inputs, L)
    shared.update(consts)
    n = 8
    in_maps = []
    for c in range(n):
        m = dict(shared)
        m["x"] = np.ascontiguousarray(x[c % B])
        in_maps.append(m)
    res = run_bass_kernel_spmd(nc, in_maps, core_ids=list(range(n)))
    return np.stack([res.results[b]["y"] for b in range(B)], axis=0).astype(np.float32)
```
